# Optimizing a Trainium2 kernel written in Bass

```python
import math
import jax, jax.numpy as jnp
from jax import lax
import numpy as np

D_MODEL = 1024
BATCH = 16
SEQ = 4096
DEPTH = 4

GRID_W = 64
CTX_LEN = 256
N_MIXERS = 3
EPS = 1e-6

S5_GROUP = 16
S5_GROUPS = D_MODEL // S5_GROUP
S5_STATE = 64
S5_DT_MIN = 1e-3
S5_DT_MAX = 1e-1

SSD_D_INNER = 2 * D_MODEL
SSD_HEAD_DIM = 64
SSD_HEADS = SSD_D_INNER // SSD_HEAD_DIM
SSD_GROUPS = 8
SSD_STATE = 128
SSD_CONV = 5
SSD_CHUNK = 128
SSD_GN = SSD_GROUPS * SSD_STATE
SSD_IN_DIM = 2 * SSD_D_INNER + 2 * SSD_GN + 2 * SSD_HEADS

DA_HEADS = 8
DA_HEAD_DIM = D_MODEL // (2 * DA_HEADS)
DA_WIDTH = DA_HEADS * 2 * DA_HEAD_DIM
ROPE_BASE = 10000.0
Q_BLOCK = 128

N_EXPERTS = 32
TOP_K = 4
D_FF_EXPERT = D_MODEL
SWIGLU_ALPHA = 1.702
SWIGLU_LIMIT = 7.0
MOE_BLOCK = 256

N_S5_LAYERS = len(range(0, DEPTH, N_MIXERS))
N_SSD_LAYERS = len(range(1, DEPTH, N_MIXERS))
N_DA_LAYERS = len(range(2, DEPTH, N_MIXERS))

kernel_name = 'hybrid_s5_ssd_diffattn_moe_dit'

F32 = jnp.float32


def rmsnorm(x, g):
    xf = x.astype(F32)
    y = xf * lax.rsqrt(jnp.mean(xf * xf, axis=-1, keepdims=True) + EPS)
    return (y * g.astype(F32)).astype(x.dtype)


def modulate(x, g, shift, scale):
    return rmsnorm(x, g) * (1 + scale) + shift


def _flip(t, rev):
    return jnp.flip(t, axis=1) if rev else t


def axial_rope_tables(n_tok):
    rows = n_tok // GRID_W
    row = jnp.repeat(jnp.arange(rows, dtype=F32), GRID_W)
    col = jnp.tile(jnp.arange(GRID_W, dtype=F32), rows)
    n_freq = DA_HEAD_DIM // 4
    inv = ROPE_BASE ** (-jnp.arange(n_freq, dtype=F32) / n_freq)
    ar = row[:, None] * inv
    ac = col[:, None] * inv
    return (jnp.cos(ar), jnp.sin(ar), jnp.cos(ac), jnp.sin(ac))


def _rotate(x, cos, sin):
    x1, x2 = jnp.split(x, 2, axis=-1)
    return jnp.concatenate([x1 * cos - x2 * sin, x2 * cos + x1 * sin], axis=-1)


def axial_rope(x, tabs):
    cr, sr, cc, sc = tabs
    xr, xc = jnp.split(x.astype(F32), 2, axis=-1)
    return jnp.concatenate([_rotate(xr, cr, sr), _rotate(xc, cc, sc)], axis=-1).astype(x.dtype)


def s5_discretize(a_re, a_im, log_dt, b_re, b_im):
    a_re = a_re.astype(F32); a_im = a_im.astype(F32)
    dt = jnp.exp(log_dt.astype(F32))[:, None]
    mag = jnp.exp(dt * a_re)
    ang = dt * a_im
    ab_re = mag * jnp.cos(ang)
    ab_im = mag * jnp.sin(ang)
    den = a_re * a_re + a_im * a_im
    num_re = ab_re - 1.0
    co_re = (num_re * a_re + ab_im * a_im) / den
    co_im = (ab_im * a_re - num_re * a_im) / den
    b_re = b_re.astype(F32); b_im = b_im.astype(F32)
    bb_re = co_re[..., None] * b_re - co_im[..., None] * b_im
    bb_im = co_re[..., None] * b_im + co_im[..., None] * b_re
    return ab_re, ab_im, bb_re, bb_im


def _complex_affine_combine(e1, e2):
    a1r, a1i, b1r, b1i = e1
    a2r, a2i, b2r, b2i = e2
    return (a1r * a2r - a1i * a2i, a1r * a2i + a1i * a2r,
            a2r * b1r - a2i * b1i + b2r, a2r * b1i + a2i * b1r + b2i)


def s5_scan(ab_re, ab_im, bb_re, bb_im, u, h0):
    bu_re = jnp.einsum('btgj,gpj->btgp', u, bb_re)
    bu_im = jnp.einsum('btgj,gpj->btgp', u, bb_im)
    if h0 is not None:
        h_re, h_im = h0
        bu_re = bu_re.at[:, 0].add(ab_re * h_re - ab_im * h_im)
        bu_im = bu_im.at[:, 0].add(ab_re * h_im + ab_im * h_re)
    shape = (1,) + bu_re.shape[1:]
    a_re = jnp.broadcast_to(ab_re, shape)
    a_im = jnp.broadcast_to(ab_im, shape)
    _, _, s_re, s_im = lax.associative_scan(_complex_affine_combine, (a_re, a_im, bu_re, bu_im), axis=1)
    return s_re, s_im


def s5_readout(s_re, s_im, c_re, c_im):
    y = jnp.einsum('btgp,gjp->btgj', s_re, c_re.astype(F32)) - jnp.einsum('btgp,gjp->btgj', s_im, c_im.astype(F32))
    return y.reshape(y.shape[0], y.shape[1], D_MODEL)


def s5_mixer(hc, hl, a_re, a_im, log_dt, b_re, b_im, c_re, c_im, d_skip, glu_w, glu_b, ctx_out):
    bsz, n_ctx, _ = hc.shape
    n_lat = hl.shape[1]
    uc = hc.astype(F32).reshape(bsz, n_ctx, S5_GROUPS, S5_GROUP)
    ul = hl.astype(F32).reshape(bsz, n_lat, S5_GROUPS, S5_GROUP)
    dsk = d_skip.astype(F32)
    yl = dsk * hl.astype(F32)
    yc = dsk * hc.astype(F32)
    for dirn in range(2):
        rev = dirn == 1
        ab_re, ab_im, bb_re, bb_im = s5_discretize(a_re[dirn], a_im[dirn], log_dt[dirn], b_re[dirn], b_im[dirn])
        sc_re, sc_im = s5_scan(ab_re, ab_im, bb_re, bb_im, _flip(uc, rev), None)
        sl_re, sl_im = s5_scan(ab_re, ab_im, bb_re, bb_im, _flip(ul, rev), (sc_re[:, -1], sc_im[:, -1]))
        yl = yl + _flip(s5_readout(sl_re, sl_im, c_re[dirn], c_im[dirn]), rev)
        if ctx_out:
            yc = yc + _flip(s5_readout(sc_re, sc_im, c_re[dirn], c_im[dirn]), rev)

    def glu(y, dtype):
        g = jax.nn.gelu(y.astype(dtype))
        a, b = jnp.split(g @ glu_w + glu_b, 2, axis=-1)
        return a * jax.nn.sigmoid(b)

    return (glu(yc, hc.dtype) if ctx_out else None), glu(yl, hl.dtype)


def depthwise_conv_centred(x, w, b):
    pad = (w.shape[0] - 1) // 2
    y = lax.conv_general_dilated(x, w[:, None, :], window_strides=(1,), padding=[(pad, pad)],
                                 dimension_numbers=('NWC', 'WIO', 'NWC'), feature_group_count=x.shape[-1])
    return y + b


def ssd_inputs(h, in_w, conv_w, conv_b, dt_bias):
    bsz, t, _ = h.shape
    zxbcdt = h @ in_w
    z = zxbcdt[..., :SSD_D_INNER]
    xbc = zxbcdt[..., SSD_D_INNER:2 * SSD_D_INNER + 2 * SSD_GN]
    dt = zxbcdt[..., 2 * SSD_D_INNER + 2 * SSD_GN:]
    xbc = jax.nn.silu(depthwise_conv_centred(xbc, conv_w, conv_b))
    xs = xbc[..., :SSD_D_INNER].reshape(bsz, t, SSD_HEADS, SSD_HEAD_DIM)
    bm = xbc[..., SSD_D_INNER:SSD_D_INNER + SSD_GN].reshape(bsz, t, SSD_GROUPS, SSD_STATE)
    cm = xbc[..., SSD_D_INNER + SSD_GN:].reshape(bsz, t, SSD_GROUPS, SSD_STATE)
    dt = jax.nn.softplus(dt.astype(F32).reshape(bsz, t, 2, SSD_HEADS) + dt_bias.astype(F32))
    return z, xs, bm, cm, dt


def ssd_scan(x, dt, a, bm, cm, h0, need_y):
    bsz, t, _, _ = x.shape
    nc, q, g, r = t // SSD_CHUNK, SSD_CHUNK, SSD_GROUPS, SSD_HEADS // SSD_GROUPS
    xdt = x.astype(F32).reshape(bsz, nc, q, g, r, SSD_HEAD_DIM) * dt.reshape(bsz, nc, q, g, r)[..., None]
    cs = jnp.cumsum((dt * a).reshape(bsz, nc, q, g, r), axis=2)
    bc = bm.astype(F32).reshape(bsz, nc, q, g, SSD_STATE)
    cc = cm.astype(F32).reshape(bsz, nc, q, g, SSD_STATE)
    decay_s = jnp.exp(cs[:, :, -1:] - cs)
    states = jnp.einsum('bcsgn,bcsgr,bcsgrp->bcgrpn', bc, decay_s, xdt)
    chunk_decay = jnp.exp(cs[:, :, -1])

    def step(h, inp):
        dec, st = inp
        return dec[..., None, None] * h + st, h

    h_fin, h_in = lax.scan(step, h0, (jnp.moveaxis(chunk_decay, 1, 0), jnp.moveaxis(states, 1, 0)))
    if not need_y:
        return None, h_fin
    h_in = jnp.moveaxis(h_in, 0, 1)
    mask = jnp.tril(jnp.ones((q, q), dtype=bool))[None, None, :, :, None, None]
    seg = cs[:, :, :, None] - cs[:, :, None, :]
    lmat = jnp.exp(jnp.where(mask, seg, -jnp.inf))
    scores = jnp.einsum('bclgn,bcsgn->bclsg', cc, bc)
    y_diag = jnp.einsum('bclsg,bclsgr,bcsgrp->bclgrp', scores, lmat, xdt)
    y_off = jnp.einsum('bclgn,bcgrpn,bclgr->bclgrp', cc, h_in, jnp.exp(cs))
    return (y_diag + y_off).reshape(bsz, t, SSD_HEADS, SSD_HEAD_DIM), h_fin


def ssd_mixer(hc, hl, in_w, conv_w, conv_b, dt_bias, a_log, d_skip, norm_g, out_w, ctx_out):
    zc, xc, bc, cc, dtc = ssd_inputs(hc, in_w, conv_w, conv_b, dt_bias)
    zl, xl, bl, cl, dtl = ssd_inputs(hl, in_w, conv_w, conv_b, dt_bias)
    dsk = d_skip.astype(F32)[:, None]
    yl = dsk * xl.astype(F32)
    yc = dsk * xc.astype(F32)
    zero = jnp.zeros((hc.shape[0], SSD_GROUPS, SSD_HEADS // SSD_GROUPS, SSD_HEAD_DIM, SSD_STATE), F32)
    for dirn in range(2):
        rev = dirn == 1
        a = -jnp.exp(a_log[dirn].astype(F32))
        yc_d, hc_fin = ssd_scan(_flip(xc, rev), _flip(dtc[:, :, dirn], rev), a, _flip(bc, rev), _flip(cc, rev), zero, ctx_out)
        yl_d, _ = ssd_scan(_flip(xl, rev), _flip(dtl[:, :, dirn], rev), a, _flip(bl, rev), _flip(cl, rev), hc_fin, True)
        yl = yl + _flip(yl_d, rev)
        if ctx_out:
            yc = yc + _flip(yc_d, rev)

    def out(y, z):
        bsz, t = z.shape[:2]
        gy = (y.reshape(bsz, t, SSD_D_INNER).astype(z.dtype) * jax.nn.silu(z)).reshape(bsz, t, SSD_GROUPS, -1)
        gy = rmsnorm(gy, norm_g.reshape(SSD_GROUPS, -1)).reshape(bsz, t, SSD_D_INNER)
        return gy @ out_w

    return (out(yc, zc) if ctx_out else None), out(yl, zl)


def _diff_attend(q, k, v, lam):
    s = jnp.einsum('bchqd,bchkd->bchqk', q, k).astype(F32) * (DA_HEAD_DIM ** -0.5)
    p = jax.nn.softmax(s, axis=-1)
    a = p[:, 0] - lam * p[:, 1]
    return jnp.einsum('bhqk,bhkv->bhqv', a.astype(v.dtype), v)


def diff_attention(hc, hl, qkv_w, q_g, k_g, lam_vec, sub_g, out_w, rope, layer_idx, ctx_out):
    lam_init = 0.8 - 0.6 * math.exp(-0.3 * layer_idx)
    lv = lam_vec.astype(F32)
    lam = jnp.exp(jnp.sum(lv[0] * lv[1])) - jnp.exp(jnp.sum(lv[2] * lv[3])) + lam_init

    def project(h, tabs):
        bsz, t, _ = h.shape
        q, k, v = jnp.split(h @ qkv_w, 3, axis=-1)
        q = rmsnorm(q.reshape(bsz, t, DA_HEADS, 2, DA_HEAD_DIM).transpose(0, 3, 2, 1, 4), q_g)
        k = rmsnorm(k.reshape(bsz, t, DA_HEADS, 2, DA_HEAD_DIM).transpose(0, 3, 2, 1, 4), k_g)
        v = v.reshape(bsz, t, DA_HEADS, 2 * DA_HEAD_DIM).transpose(0, 2, 1, 3)
        if tabs is not None:
            q = axial_rope(q, tabs)
            k = axial_rope(k, tabs)
        return q, k, v

    def finish(o):
        bsz, _, t, _ = o.shape
        o = rmsnorm(o, sub_g) * (1.0 - lam_init)
        return o.transpose(0, 2, 1, 3).reshape(bsz, t, DA_WIDTH) @ out_w

    qc, kc, vc = project(hc, None)
    ql, kl, vl = project(hl, rope)
    k_all = jnp.concatenate([kc, kl], axis=3)
    v_all = jnp.concatenate([vc, vl], axis=2)
    bsz, _, _, n_lat, _ = ql.shape
    nb = n_lat // Q_BLOCK
    qb = ql.reshape(bsz, 2, DA_HEADS, nb, Q_BLOCK, DA_HEAD_DIM).transpose(3, 0, 1, 2, 4, 5)
    ob = lax.map(lambda qq: _diff_attend(qq, k_all, v_all, lam), qb)
    ol = ob.transpose(1, 2, 0, 3, 4).reshape(bsz, DA_HEADS, n_lat, 2 * DA_HEAD_DIM)
    yc = finish(_diff_attend(qc, kc, vc, lam)) if ctx_out else None
    return yc, finish(ol)


def moe(h, router_w, router_b, gu_w, gu_b, dn_w, dn_b):
    n_tok, d = h.shape
    logits = (h @ router_w + router_b).astype(F32)
    top_v, top_i = lax.top_k(logits, TOP_K)
    gates = jax.nn.softmax(top_v, axis=-1)
    n_assign = n_tok * TOP_K
    n_blocks = -(-n_assign // MOE_BLOCK) + N_EXPERTS
    n_slots = n_blocks * MOE_BLOCK
    flat_e = top_i.reshape(-1)
    order = jnp.argsort(flat_e)
    sorted_e = flat_e[order]
    counts = jnp.bincount(flat_e, length=N_EXPERTS)
    padded = ((counts + MOE_BLOCK - 1) // MOE_BLOCK) * MOE_BLOCK
    pad_end = jnp.cumsum(padded)
    pad_start = pad_end - padded
    start = jnp.cumsum(counts) - counts
    dest = pad_start[sorted_e] + jnp.arange(n_assign) - start[sorted_e]
    slot_tok = jnp.full((n_slots,), n_tok, dtype=jnp.int32).at[dest].set((order // TOP_K).astype(jnp.int32))
    slot_gate = jnp.zeros((n_slots,), F32).at[dest].set(gates.reshape(-1)[order])
    block_e = jnp.minimum(jnp.searchsorted(pad_end, jnp.arange(n_blocks) * MOE_BLOCK, side='right'), N_EXPERTS - 1)
    h_pad = jnp.concatenate([h, jnp.zeros((1, d), h.dtype)], axis=0)

    def expert_block(args):
        tok, e = args
        gu = h_pad[tok] @ gu_w[e] + gu_b[e]
        gate, up = jnp.split(gu, 2, axis=-1)
        gate = jnp.minimum(gate, SWIGLU_LIMIT)
        up = jnp.clip(up, -SWIGLU_LIMIT, SWIGLU_LIMIT)
        act = (up + 1) * gate * jax.nn.sigmoid(SWIGLU_ALPHA * gate)
        return act @ dn_w[e] + dn_b[e]

    y = lax.map(expert_block, (slot_tok.reshape(n_blocks, MOE_BLOCK), block_e)).reshape(n_slots, d)
    out = jnp.zeros((n_tok + 1, d), h.dtype).at[slot_tok].add(y * slot_gate[:, None].astype(y.dtype))
    return out[:n_tok]


def setup_inputs(seed: int = 0) -> dict:
    key = jax.random.key(seed)
    ks = iter(jax.random.split(key, 64))

    def nrm(shape, scale):
        return scale * jax.random.normal(next(ks), shape, F32)

    def uni(shape, lo, hi):
        return jax.random.uniform(next(ks), shape, F32, lo, hi)

    D, G, P, J = D_MODEL, S5_GROUPS, S5_STATE, S5_GROUP
    ns5, nsd, nda = N_S5_LAYERS, N_SSD_LAYERS, N_DA_LAYERS
    conv_ch = SSD_D_INNER + 2 * SSD_GN
    ssd_dt = jnp.exp(uni((nsd, 2, SSD_HEADS), math.log(1e-3), math.log(1e-1)))
    return {
        'x': nrm((BATCH, SEQ, D), 1.0),
        'c': nrm((BATCH, D), 1.0),
        'ctx': nrm((BATCH, CTX_LEN, D), 1.0),
        'c_ctx': nrm((D,), 1.0),
        'mod_w': nrm((DEPTH, D, 6 * D), 0.5 * D ** -0.5),
        'mod_b': nrm((DEPTH, 6 * D), 0.02),
        'norm1_g': 1.0 + nrm((DEPTH, D), 0.02),
        'norm2_g': 1.0 + nrm((DEPTH, D), 0.02),
        's5_a_re': -0.5 + nrm((ns5, 2, G, P), 0.01),
        's5_a_im': math.pi * jnp.arange(P, dtype=F32) + nrm((ns5, 2, G, P), 0.01),
        's5_log_dt': uni((ns5, 2, G), math.log(S5_DT_MIN), math.log(S5_DT_MAX)),
        's5_b_re': nrm((ns5, 2, G, P, J), (2 * J) ** -0.5),
        's5_b_im': nrm((ns5, 2, G, P, J), (2 * J) ** -0.5),
        's5_c_re': nrm((ns5, 2, G, J, P), (2 * P) ** -0.5),
        's5_c_im': nrm((ns5, 2, G, J, P), (2 * P) ** -0.5),
        's5_d': nrm((ns5, D), 0.5),
        's5_glu_w': nrm((ns5, D, 2 * D), D ** -0.5),
        's5_glu_b': nrm((ns5, 2 * D), 0.02),
        'ssd_in_w': nrm((nsd, D, SSD_IN_DIM), D ** -0.5),
        'ssd_conv_w': nrm((nsd, SSD_CONV, conv_ch), SSD_CONV ** -0.5),
        'ssd_conv_b': nrm((nsd, conv_ch), 0.02),
        'ssd_dt_bias': ssd_dt + jnp.log(-jnp.expm1(-ssd_dt)),
        'ssd_a_log': jnp.log(uni((nsd, 2, SSD_HEADS), 1.0, 16.0)),
        'ssd_d': 1.0 + nrm((nsd, SSD_HEADS), 0.1),
        'ssd_norm_g': 1.0 + nrm((nsd, SSD_D_INNER), 0.02),
        'ssd_out_w': nrm((nsd, SSD_D_INNER, D), SSD_D_INNER ** -0.5),
        'da_qkv_w': nrm((nda, D, 3 * DA_WIDTH), D ** -0.5),
        'da_q_g': 1.0 + nrm((nda, DA_HEAD_DIM), 0.02),
        'da_k_g': 1.0 + nrm((nda, DA_HEAD_DIM), 0.02),
        'da_lam': nrm((nda, 4, DA_HEAD_DIM), 0.1),
        'da_sub_g': 1.0 + nrm((nda, 2 * DA_HEAD_DIM), 0.02),
        'da_out_w': nrm((nda, DA_WIDTH, D), DA_WIDTH ** -0.5),
        'moe_router_w': nrm((DEPTH, D, N_EXPERTS), D ** -0.5),
        'moe_router_b': nrm((DEPTH, N_EXPERTS), 0.01),
        'moe_gu_w': nrm((DEPTH, N_EXPERTS, D, 2 * D_FF_EXPERT), D ** -0.5),
        'moe_gu_b': nrm((DEPTH, N_EXPERTS, 2 * D_FF_EXPERT), 0.01),
        'moe_dn_w': nrm((DEPTH, N_EXPERTS, D_FF_EXPERT, D), D_FF_EXPERT ** -0.5),
        'moe_dn_b': nrm((DEPTH, N_EXPERTS, D), 0.01),
    }


def reference(x, c, ctx, c_ctx, mod_w, mod_b, norm1_g, norm2_g,
              s5_a_re, s5_a_im, s5_log_dt, s5_b_re, s5_b_im, s5_c_re, s5_c_im, s5_d, s5_glu_w, s5_glu_b,
              ssd_in_w, ssd_conv_w, ssd_conv_b, ssd_dt_bias, ssd_a_log, ssd_d, ssd_norm_g, ssd_out_w,
              da_qkv_w, da_q_g, da_k_g, da_lam, da_sub_g, da_out_w,
              moe_router_w, moe_router_b, moe_gu_w, moe_gu_b, moe_dn_w, moe_dn_b):
    bsz, n_lat, d = x.shape
    n_ctx = ctx.shape[1]
    rope = axial_rope_tables(n_lat)
    xl, xc = x, ctx
    silu_c = jax.nn.silu(c)
    silu_cc = jax.nn.silu(c_ctx)[None]
    for i in range(DEPTH):
        last = i == DEPTH - 1
        kind, j = i % N_MIXERS, i // N_MIXERS
        ml = jnp.split((silu_c @ mod_w[i] + mod_b[i])[:, None, :], 6, axis=-1)
        mc = jnp.split((silu_cc @ mod_w[i] + mod_b[i])[:, None, :], 6, axis=-1)
        hl = modulate(xl, norm1_g[i], ml[0], ml[1])
        hc = modulate(xc, norm1_g[i], mc[0], mc[1])
        if kind == 0:
            yc, yl = s5_mixer(hc, hl, s5_a_re[j], s5_a_im[j], s5_log_dt[j], s5_b_re[j], s5_b_im[j],
                              s5_c_re[j], s5_c_im[j], s5_d[j], s5_glu_w[j], s5_glu_b[j], not last)
        elif kind == 1:
            yc, yl = ssd_mixer(hc, hl, ssd_in_w[j], ssd_conv_w[j], ssd_conv_b[j], ssd_dt_bias[j],
                               ssd_a_log[j], ssd_d[j], ssd_norm_g[j], ssd_out_w[j], not last)
        else:
            yc, yl = diff_attention(hc, hl, da_qkv_w[j], da_q_g[j], da_k_g[j], da_lam[j], da_sub_g[j],
                                    da_out_w[j], rope, i, not last)
        xl = xl + ml[2] * yl
        hl = modulate(xl, norm2_g[i], ml[3], ml[4])
        moe_p = (moe_router_w[i], moe_router_b[i], moe_gu_w[i], moe_gu_b[i], moe_dn_w[i], moe_dn_b[i])
        if last:
            xl = xl + ml[5] * moe(hl.reshape(-1, d), *moe_p).reshape(bsz, n_lat, d)
        else:
            xc = xc + mc[2] * yc
            hc = modulate(xc, norm2_g[i], mc[3], mc[4])
            f = moe(jnp.concatenate([hc.reshape(-1, d), hl.reshape(-1, d)], axis=0), *moe_p)
            xc = xc + mc[5] * f[:bsz * n_ctx].reshape(bsz, n_ctx, d)
            xl = xl + ml[5] * f[bsz * n_ctx:].reshape(bsz, n_lat, d)
    return xl
```

```python
import numpy as np
import ml_dtypes
import concourse.bass as bass
import concourse.mybir as mybir
from concourse.bass_utils import run_bass_kernel_spmd
from contextlib import ExitStack

F32 = mybir.dt.float32
BF16 = mybir.dt.bfloat16
I32 = mybir.dt.int32
AF = mybir.ActivationFunctionType
ALU = mybir.AluOpType
AX = mybir.AxisListType

ENGS = ["sync", "scalar", "vector", "gpsimd", "tensor"]
NSLOT = {"sync": 8, "scalar": 4, "vector": 2, "gpsimd": 8, "tensor": 2}
SAME_ENGINE_SYNC = {"sync": True, "scalar": True, "vector": True, "gpsimd": True, "tensor": False}
EPOCH = 24000

D = 1024
NCH = 8
TCORE = 8704
NT = 17
EPS = 1e-6
NEXP = 32
BLK = 512


class Op:
    __slots__ = ("eng", "fn", "dma", "deps", "sem", "val", "prev")

    def __init__(self, eng, fn, dma):
        self.eng = eng
        self.fn = fn
        self.dma = dma
        self.deps = ()
        self.sem = None
        self.val = 0
        self.prev = None


class Sched:
    def __init__(self, nc):
        self.nc = nc
        self.eng_ops = {e: [] for e in ENGS}
        self.last_w = {}
        self.readers = {}
        self.nsem = 0
        self.csem = {e: [self._newsem(), 0] for e in ENGS}
        self.dslot = {e: [[self._newsem(), 0, None] for _ in range(NSLOT[e])] for e in ENGS}
        self.dma_rr = {e: 0 for e in ENGS}
        self.last_op = {e: None for e in ENGS}

    def _newsem(self):
        self.nsem += 1
        return self.nsem - 1

    def op(self, eng, fn, reads=(), writes=(), dma=False):
        o = Op(eng, fn, dma)
        deps = {}
        for t in reads:
            w = self.last_w.get(t)
            if w is not None:
                deps[id(w)] = w
        for t in writes:
            w = self.last_w.get(t)
            if w is not None:
                deps[id(w)] = w
            for r in self.readers.get(t, ()):
                deps[id(r)] = r
        o.deps = tuple(deps.values())
        for t in writes:
            self.last_w[t] = o
            self.readers[t] = []
        wset = set(writes)
        for t in reads:
            if t not in wset:
                self.readers.setdefault(t, []).append(o)
        if dma:
            s = self.dma_rr[eng]
            self.dma_rr[eng] = (s + 1) % NSLOT[eng]
            slot = self.dslot[eng][s]
            o.prev = slot[2]
            if slot[1] + 16 > EPOCH:
                slot[0] = self._newsem()
                slot[1] = 0
            slot[1] += 16
            o.sem, o.val = slot[0], slot[1]
            slot[2] = o
        else:
            c = self.csem[eng]
            if c[1] + 1 > EPOCH:
                c[0] = self._newsem()
                c[1] = 0
            c[1] += 1
            o.sem, o.val = c[0], c[1]
            self.last_op[eng] = o
        self.eng_ops[eng].append(o)
        return o

    def dma(self, eng, out, in_, reads=(), writes=(), **kw):
        return self.op(eng, lambda e: e.dma_start(out=out, in_=in_, **kw), reads, writes, dma=True)

    def barrier(self):
        deps = []
        for e in ENGS:
            if self.last_op[e] is not None:
                deps.append(self.last_op[e])
            for slot in self.dslot[e]:
                if slot[2] is not None:
                    deps.append(slot[2])
        for e in ENGS:
            b = Op(e, None, False)
            b.deps = tuple(deps)
            self.eng_ops[e].append(b)

    def emit(self):
        nc = self.nc
        with ExitStack() as es:
            sems = [es.enter_context(nc.semaphore("s%d" % i)) for i in range(self.nsem)]
            block = es.enter_context(nc.Block())

            def run(engname, e):
                waited = {}

                def wait(d):
                    if waited.get(d.sem, 0) >= d.val:
                        return
                    waited[d.sem] = d.val
                    e.wait_ge(sems[d.sem], d.val)

                for o in self.eng_ops[engname]:
                    if o.prev is not None:
                        wait(o.prev)
                    for d in o.deps:
                        if d.eng == engname and not d.dma and not SAME_ENGINE_SYNC[engname] and o.fn is not None:
                            continue
                        wait(d)
                    if o.fn is None:
                        continue
                    ins = o.fn(e)
                    ins.then_inc(sems[o.sem], 16 if o.dma else 1)

            @block.sync
            def _(e):
                run("sync", e)

            @block.scalar
            def _(e):
                run("scalar", e)

            @block.vector
            def _(e):
                run("vector", e)

            @block.gpsimd
            def _(e):
                run("gpsimd", e)

            @block.tensor
            def _(e):
                run("tensor", e)


def bc(ap, shape, axis):
    return ap.unsqueeze(axis).to_broadcast(shape)


class Ctx:
    pass


def stream_of_tile(j):
    return 0 if j == 0 else (1 if j <= 8 else 2)


def build_program(layers, n_layers_weights, phases_per_layer=None, dbg=None):
    nc = bass.Bass("TRN2", target_bir_lowering=False)
    S = Sched(nc)
    g = Ctx()
    g.nc, g.S = nc, S
    g.lidx = {l: i for i, l in enumerate(layers)}
    NLW = n_layers_weights

    def din(name, shape, dt=F32):
        return nc.dram_tensor(name, shape, dt, kind="ExternalInput").ap()

    def dscr(name, shape, dt=F32):
        return nc.dram_tensor(name, shape, dt, kind="Internal").ap()

    g.xT0 = din("xT0", [D, TCORE])
    g.cT = din("cT", [128, NCH, 3])
    g.mod_w = din("mod_w", [NLW, D, 6 * D])
    g.mod_bT = din("mod_bT", [NLW, 128, 48])
    g.n1T = din("n1T", [NLW, 128, NCH])
    g.n2T = din("n2T", [NLW, 128, NCH])
    g.rwT = din("rwT", [NLW, 128, NCH, NEXP])
    g.rb_bc = din("rb_bc", [NLW, 128, NEXP])
    if phases_per_layer is not None and "moe" not in phases_per_layer:
        g.gu_w = din("gu_w", [NLW, 8, 2 * D])
        g.dn_w = din("dn_w", [NLW, 8, D])
    else:
        g.gu_w = din("gu_w", [NLW, NEXP * D, 2 * D])
        g.dn_w = din("dn_w", [NLW, NEXP * D, D])
    g.gu_bT = din("gu_bT", [NLW, NEXP * 128, 16])
    g.dn_b = din("dn_b", [NLW, NEXP, D])
    g.cst = din("cst", [128, 6, 128])
    g.cblk = din("cblk", [128, 128])
    g.outT = nc.dram_tensor("outT", [D, 8192], F32, kind="ExternalOutput").ap()

    ns5 = len([l for l in layers if l % 3 == 0])
    if ns5:
        g.s5B = din("s5B", [2, 128, 2, 32, 3])
        g.s5A = din("s5A", [2, 128, 2, 8, 64, 3])
        g.s5BzB = din("s5BzB", [2, 128, 2, 2, 32, 128])
        g.s5CzB = din("s5CzB", [2, 128, 2, 2, 32, 128])
        g.s5BzA = din("s5BzA", [2, 128, 2, 2, 8, 8, 64])
        g.s5d = din("s5d", [2, 128, NCH])
        g.s5gw = din("s5gw", [2, D, 2 * D])
        g.s5gb = din("s5gb", [2, 128, 16])
        g.s5c = din("s5c", [128, 16 + NBK])
    if any(l % 3 == 2 for l in layers):
        g.daqkv = din("daqkv", [D, 3 * D])
        g.daow = din("daow", [D, D])
        g.dagv = din("dagv", [128, 4])
        g.dalam = din("dalam", [64, 4])
        g.dac = din("dac", [128, 3, 128])
        g.ropeC = din("ropeC", [128, 4096])
        g.ropeS = din("ropeS", [128, 4096])
        g.QT = dscr("QT", [D, TCORE], BF16)
        g.KT = dscr("KT", [D, TCORE], BF16)
        g.VTOK = dscr("VTOK", [TCORE, D], BF16)
    if any(l % 3 == 1 for l in layers):
        g.ssd_inw = din("ssd_inw", [D, 6208])
        g.ssd_cw = din("ssd_cw", [128, 32, 6])
        g.ssd_hp = din("ssd_hp", [64, 4])
        g.ssd_mask = din("ssd_mask", [128, 2, 4, 512])
        g.ssd_fv = din("ssd_fv", [128, 16, 2])
        g.ssd_ow = din("ssd_ow", [2 * D, D])
        g.SZ = dscr("SZ", [2 * D, TCORE], BF16)
        g.XBCp = dscr("XBCp", [4 * D, TCORE], BF16)
        g.DTr = dscr("DTr", [64, TCORE], F32)
        g.XFo = dscr("XFo", [2 * D, TCORE], BF16)
        g.BCo = dscr("BCo", [2 * D, TCORE], BF16)
        g.XTOK = dscr("XTOK", [TCORE, 2 * D], BF16)
        g.YT = dscr("YT", [2 * D, TCORE], BF16)
    g.GT2 = dscr("GT2", [2 * D, TCORE], BF16)
    g.HT = dscr("HT", [D, TCORE], BF16)
    g.GT = dscr("GT", [D, TCORE], BF16)
    g.XT = dscr("XT", [D, TCORE])
    g.Htok = dscr("Htok", [TCORE, D], BF16)
    NSLOTS = (TCORE * 4 // BLK + NEXP) * BLK
    g.NB = NSLOTS // BLK
    g.Hs = dscr("Hs", [NSLOTS, D], BF16)
    g.Ys = dscr("Ys", [NSLOTS, D], F32)

    with ExitStack() as top:
        uid = [0]

        def sb(name, shape, dt=F32, stack=top):
            uid[0] += 1
            return stack.enter_context(nc.sbuf_tensor("%s_%d" % (name, uid[0]), shape, dt))

        def ps(name, shape, dt=F32, stack=top):
            uid[0] += 1
            return stack.enter_context(nc.psum_tensor("%s_%d" % (name, uid[0]), shape, dt))

        g.sb, g.ps = sb, ps
        g.cst_t = sb("cst_t", [128, 6, 128])
        g.cblk_t = sb("cblk_t", [128, 128])
        g.ident_bf = sb("ident_bf", [128, 128], BF16)
        g.ones_bf = sb("ones_bf", [128, 128], BF16)
        S.dma("sync", g.cst_t[:], g.cst[:, :, :], writes=["cst"])
        S.dma("sync", g.cblk_t[:], g.cblk[:, :], writes=["cblk"])
        S.op("vector", lambda e: e.tensor_copy(out=g.ident_bf[:], in_=g.cst_t[:, 0, :]), ["cst"], ["ident_bf"])
        S.op("vector", lambda e: e.tensor_copy(out=g.ones_bf[:], in_=g.cst_t[:, 2, :]), ["cst"], ["ones_bf"])
        g.ident_f = g.cst_t[:, 0, :]
        g.tri_f = g.cst_t[:, 1, :]
        g.ones_f = g.cst_t[:, 2, :]
        g.iota_row = g.cst_t[:, 3, :]
        g.iota_p = g.cst_t[:, 4, 0:1]
        g.mods = {}
        g.es1 = {}
        g.es2 = {}
        for l in layers:
            g.mods[l] = sb("mods%d" % l, [128, 6, NCH, 3])
            g.es1[l] = sb("es1_%d" % l, [128, NCH, 3])
            g.es2[l] = sb("es2_%d" % l, [128, NCH, 3])

        prologue(g, layers)
        with ExitStack() as st:
            buf = [sb("cpx%d" % i, [128, NCH, 512], F32, st) for i in range(2)]
            for j in range(NT):
                b = buf[j % 2]
                cols = slice(j * 512, (j + 1) * 512)
                S.dma("sync", b[:], g.xT0.rearrange("(c p) t -> p c t", p=128)[:, :, cols], writes=["cpx%d" % (j % 2)])
                S.dma("scalar", g.XT.rearrange("(c p) t -> p c t", p=128)[:, :, cols], b[:], reads=["cpx%d" % (j % 2)],
                      writes=[("XT", j)])
            S.barrier()

        for l in layers:
            ph = phases_per_layer or ("mix", "moe")
            if "mix" in ph:
                kind = l % 3
                if kind == 0:
                    s5_layer(g, l)
                elif kind == 1:
                    ssd_layer(g, l)
                else:
                    da_layer(g, l)
            if "moe" in ph:
                moe_layer(g, l, last=(l == 3))

        with ExitStack() as st:
            buf = [sb("cpo%d" % i, [128, NCH, 512], F32, st) for i in range(2)]
            outs = []
            for j in range(1, NT):
                b = buf[j % 2]
                cols = slice(j * 512, (j + 1) * 512)
                S.dma("sync", b[:], g.XT.rearrange("(c p) t -> p c t", p=128)[:, :, cols], reads=[("XT", j)],
                      writes=["cpo%d" % (j % 2)])
                S.dma("scalar", g.outT.rearrange("(c p) t -> p c t", p=128)[:, :, (j - 1) * 512:j * 512], b[:],
                      reads=["cpo%d" % (j % 2)], writes=[("out", j)])
            S.barrier()
        S.emit()
    return nc


def prologue(g, layers):
    nc, S = g.nc, g.S
    with ExitStack() as st:
        sb = lambda n, s, d=F32: g.sb(n, s, d, st)
        ct = sb("ct", [128, NCH, 3])
        sc = sb("sc", [128, NCH, 3])
        mw = [sb("mw%d" % i, [128, NCH, 1024]) for i in range(2)]
        mb = sb("mb", [128, 48])
        gn = sb("gn", [128, 2, NCH])
        pm = g.ps("pm", [128, 8, 4], F32, st)
        S.dma("sync", ct[:], g.cT[:, :, :], writes=["ct"])
        S.op("scalar", lambda e: e.activation(out=sc[:], in_=ct[:], func=AF.Silu), ["ct"], ["sc"])
        k = 0
        for li, l in enumerate(layers):
            S.dma("sync", mb[:], g.mod_bT[li, :, :], writes=["mb"])
            S.dma("sync", gn[:, 0, :], g.n1T[li, :, :], writes=["gn0"])
            S.dma("sync", gn[:, 1, :], g.n2T[li, :, :], writes=["gn1"])
            for part in range(6):
                w = mw[k % 2]
                wn = "mw%d" % (k % 2)
                k += 1
                S.dma("sync", w[:], g.mod_w[li].rearrange("(kc p) n -> p kc n", p=128)[:, :, part * 1024:(part + 1) * 1024],
                      writes=[wn])
                for oc in range(8):
                    for kc in range(NCH):
                        S.op("tensor", lambda e, w=w, oc=oc, kc=kc: e.matmul(
                            pm[:, oc, 0:3], lhsT=w[:, kc, oc * 128:(oc + 1) * 128], rhs=sc[:, kc, :],
                            start=(kc == 0), stop=(kc == NCH - 1)), [wn, "sc"], ["pm"])
                S.op("vector", lambda e, l=l, part=part: e.tensor_tensor(
                    out=g.mods[l][:, part, :, :], in0=pm[:, :, 0:3],
                    in1=bc(mb[:, part * 8:(part + 1) * 8], [128, 8, 3], 2), op=ALU.add), ["pm", "mb"], [("mods", l)])
            S.op("vector", lambda e, l=l: e.scalar_tensor_tensor(
                out=g.es1[l][:], in0=g.mods[l][:, 1, :, :], scalar=1.0, in1=bc(gn[:, 0, :], [128, NCH, 3], 2),
                op0=ALU.add, op1=ALU.mult), [("mods", l), "gn0"], [("es1", l)])
            S.op("vector", lambda e, l=l: e.scalar_tensor_tensor(
                out=g.es2[l][:], in0=g.mods[l][:, 4, :, :], scalar=1.0, in1=bc(gn[:, 1, :], [128, NCH, 3], 2),
                op0=ALU.add, op1=ALU.mult), [("mods", l), "gn1"], [("es2", l)])
        S.barrier()


def normmod(g, l, which, xt, xtn, sq, ssp, rstd, tmp, h32, hbf, stream, N=512, tag="", hbfn=None):
    S = g.S
    hbfn = hbfn or ("hbf" + tag)
    es = g.es1[l] if which == 1 else g.es2[l]
    esn = ("es1", l) if which == 1 else ("es2", l)
    shp = 0 if which == 1 else 3
    S.op("scalar", lambda e: e.activation(out=sq[:, :, :N], in_=xt[:, :, :N], func=AF.Square), [xtn], ["sq" + tag])
    for c in range(NCH):
        S.op("tensor", lambda e, c=c: e.matmul(ssp[:, :N], lhsT=g.ones_bf[:], rhs=sq[:, c, :N], start=(c == 0),
                                              stop=(c == NCH - 1)), ["sq" + tag, "ones_bf"], ["ssp" + tag])
    S.op("vector", lambda e: e.tensor_scalar(out=rstd[:, :N], in0=ssp[:, :N], scalar1=1.0 / D, scalar2=EPS, op0=ALU.mult,
                                             op1=ALU.add), ["ssp" + tag], ["rstd" + tag])
    S.op("scalar", lambda e: e.activation(out=rstd[:, :N], in_=rstd[:, :N], func=AF.Sqrt), ["rstd" + tag], ["rstd" + tag])
    S.op("vector", lambda e: e.reciprocal(out=rstd[:, :N], in_=rstd[:, :N]), ["rstd" + tag], ["rstd" + tag])
    for c in range(NCH):
        S.op("vector", lambda e, c=c: e.scalar_tensor_tensor(
            out=tmp[:, c, :N], in0=xt[:, c, :N], scalar=es[:, c, stream:stream + 1], in1=rstd[:, :N], op0=ALU.mult,
            op1=ALU.mult), [xtn, "rstd" + tag, esn], ["nm_tmp" + tag])
        if h32 is not None:
            S.op("scalar", lambda e, c=c: e.activation(out=h32[:, c, :N], in_=tmp[:, c, :N], func=AF.Identity,
                                                       bias=g.mods[l][:, shp, c, stream:stream + 1], scale=1.0),
                 ["nm_tmp" + tag, ("mods", l)], ["h32" + tag])
            if hbf is not None:
                S.op("gpsimd", lambda e, c=c: e.tensor_copy(out=hbf[:, c, :N], in_=h32[:, c, :N]), ["h32" + tag], [hbfn])
        else:
            S.op("scalar", lambda e, c=c: e.activation(out=hbf[:, c, :N], in_=tmp[:, c, :N], func=AF.Identity,
                                                       bias=g.mods[l][:, shp, c, stream:stream + 1], scale=1.0),
                 ["nm_tmp" + tag, ("mods", l)], [hbfn])


def moe_layer(g, l, last):
    nc, S = g.nc, g.S
    li = g.lidx[l]
    j0 = 1 if last else 0
    tiles = list(range(j0, NT))
    nsub = len(tiles) * 4
    NB = (nsub * 128 * 4) // BLK + NEXP
    XTv = g.XT.rearrange("(c p) t -> p c t", p=128)
    with ExitStack() as st:
        sb = lambda n, s, d=F32: g.sb(n, s, d, st)
        lg_all = sb("lg_all", [128, nsub, NEXP])
        rk_all = sb("rk_all", [128, nsub, NEXP])
        t8_all = sb("t8_all", [128, nsub, 8])
        gates = sb("gates", [128, nsub, 4])
        dest_f = sb("dest_f", [128, nsub, 4])
        dest_i = sb("dest_i", [128, nsub, 4], I32)
        macc = sb("macc", [128, NEXP])
        rw = sb("rw", [128, NCH, NEXP])
        rbb = sb("rbb", [128, NEXP])
        idxw = sb("idxw", [128, 128], I32)
        idxb = sb("idxb", [128, 128], I32)
        idxe = sb("idxe", [128, 128], I32)
        S.dma("sync", rw[:], g.rwT[li, :, :, :], writes=["rw"])
        S.dma("sync", rbb[:], g.rb_bc[li, :, :], writes=["rbb"])
        S.op("vector", lambda e: e.memset(macc[:], 0.0), [], ["macc"])

        with ExitStack() as sa:
            sba = lambda n, s, d=F32: g.sb(n, s, d, sa)
            xt = [sba("a_xt%d" % i, [128, NCH, 512]) for i in range(2)]
            sq = sba("a_sq", [128, NCH, 512], BF16)
            tmp = sba("a_tmp", [128, NCH, 512])
            h32 = sba("a_h32", [128, NCH, 512])
            hbf = sba("a_hbf", [128, NCH, 512], BF16)
            rstd = sba("a_rstd", [128, 512])
            hrow = [sba("a_hrow%d" % i, [128, D], BF16) for i in range(2)]
            lgt = sba("a_lgt", [128, NEXP])
            msk = sba("a_msk", [128, NEXP])
            nv0 = sba("a_nv0", [128, 1])
            ex = sba("a_ex", [128, 4])
            sme = sba("a_sme", [128, 1])
            ssp = g.ps("a_ssp", [128, 512], F32, sa)
            lgp = g.ps("a_lgp", [128, NEXP], F32, sa)
            rkp = g.ps("a_rkp", [128, NEXP], F32, sa)
            trp = [g.ps("a_trp%d" % i, [128, D], BF16, sa) for i in range(2)]
            si = 0
            for jn, j in enumerate(tiles):
                x = xt[jn % 2]
                xn = "a_xt%d" % (jn % 2)
                S.dma("sync", x[:], XTv[:, :, j * 512:(j + 1) * 512], reads=[("XT", j)], writes=[xn])
                normmod(g, l, 2, x, xn, sq, ssp, rstd, tmp, h32, hbf, stream_of_tile(j), tag="A")
                for s in range(4):
                    cs = slice(s * 128, (s + 1) * 128)
                    for kc in range(NCH):
                        S.op("tensor", lambda e, kc=kc, cs=cs: e.matmul(lgp[:], lhsT=h32[:, kc, cs], rhs=rw[:, kc, :],
                                                                      start=(kc == 0), stop=(kc == NCH - 1)),
                             ["h32A", "rw"], ["lgp"])
                    S.op("vector", lambda e, si=si: e.tensor_tensor(out=lg_all[:, si, :], in0=lgp[:], in1=rbb[:], op=ALU.add),
                         ["lgp", "rbb"], [("lg", si)])
                    S.op("vector", lambda e, si=si: e.max(out=t8_all[:, si, :], in_=lg_all[:, si, :]), [("lg", si)], [("t8", si)])
                    S.op("vector", lambda e, si=si: e.tensor_scalar(out=nv0[:], in0=t8_all[:, si, 0:1], scalar1=-1.0, scalar2=None,
                                                                    op0=ALU.mult), [("t8", si)], ["nv0"])
                    S.op("scalar", lambda e, si=si: e.activation(out=ex[:], in_=t8_all[:, si, 0:4], func=AF.Exp, bias=nv0[:, 0:1],
                                                                 scale=1.0, accum_out=sme[:, 0:1]), [("t8", si), "nv0"], ["ex", "sme"])
                    S.op("vector", lambda e: e.reciprocal(out=sme[:], in_=sme[:]), ["sme"], ["sme"])
                    S.op("vector", lambda e, si=si: e.tensor_scalar(out=gates[:, si, :], in0=ex[:], scalar1=sme[:, 0:1], scalar2=None,
                                                                    op0=ALU.mult), ["ex", "sme"], [("gates", si)])
                    S.op("vector", lambda e, si=si: e.tensor_scalar(out=msk[:], in0=lg_all[:, si, :], scalar1=t8_all[:, si, 3:4],
                                                                    scalar2=None, op0=ALU.is_ge), [("lg", si), ("t8", si)], ["msk"])
                    S.op("tensor", lambda e: e.matmul(rkp[:], lhsT=g.tri_f, rhs=msk[:], start=True, stop=False), ["msk", "cst"], ["rkp"])
                    S.op("tensor", lambda e: e.matmul(rkp[:], lhsT=g.ones_f, rhs=macc[:], start=False, stop=True), ["macc", "cst"], ["rkp"])
                    S.op("vector", lambda e, si=si: e.tensor_copy(out=rk_all[:, si, :], in_=rkp[:]), ["rkp"], [("rk", si)])
                    S.op("vector", lambda e: e.tensor_tensor(out=macc[:], in0=macc[:], in1=msk[:], op=ALU.add), ["macc", "msk"], ["macc"])
                    tp = trp[si % 2]
                    tpn = "a_trp%d" % (si % 2)
                    hr = hrow[si % 2]
                    hrn = "a_hrow%d" % (si % 2)
                    for c in range(NCH):
                        S.op("tensor", lambda e, c=c, cs=cs, tp=tp: e.transpose(tp[:, c * 128:(c + 1) * 128], hbf[:, c, cs], g.ident_bf[:]),
                             ["hbfA", "ident_bf"], [tpn])
                    S.op("scalar", lambda e, tp=tp, hr=hr: e.activation(out=hr[:], in_=tp[:], func=AF.Copy), [tpn], [hrn])
                    tok0 = j * 512 + s * 128
                    S.dma("sync", g.Htok[tok0:tok0 + 128, :], hr[:], reads=[hrn], writes=[("Htok", si)])
                    si += 1
            cnt = sba("a_cnt", [128, NEXP])
            cmp3 = sba("a_cmp3", [128, NEXP, 18])
            pad = sba("a_pad", [128, NEXP])
            pend = sba("a_pend", [128, NEXP])
            pstart = sba("a_pstart", [128, NEXP])
            onesr = sba("a_onesr", [128, NEXP])
            be = sba("a_be", [128, 128])
            bef = sba("a_bef", [128, 128])
            S.op("tensor", lambda e: e.matmul(rkp[:], lhsT=g.ones_f, rhs=macc[:], start=True, stop=True), ["macc", "cst"], ["rkp"])
            S.op("vector", lambda e: e.tensor_copy(out=cnt[:], in_=rkp[:]), ["rkp"], ["cnt"])
            S.op("vector", lambda e: e.tensor_tensor(out=cmp3[:], in0=bc(cnt[:], [128, NEXP, 18], 2),
                                                     in1=bc(g.cblk_t[:, 100:118], [128, NEXP, 18], 1), op=ALU.is_gt),
                 ["cnt", "cblk"], ["cmp3"])
            S.op("vector", lambda e: e.tensor_reduce(out=pad[:], in_=cmp3[:], axis=AX.X, op=ALU.add), ["cmp3"], ["pad"])
            S.op("vector", lambda e: e.tensor_scalar(out=pad[:], in0=pad[:], scalar1=float(BLK), scalar2=None, op0=ALU.mult), ["pad"], ["pad"])
            S.op("vector", lambda e: e.memset(onesr[:], 1.0), [], ["onesr"])
            S.op("vector", lambda e: e.tensor_tensor_scan(out=pend[:], data0=onesr[:], data1=pad[:], initial=0.0, op0=ALU.mult,
                                                          op1=ALU.add), ["onesr", "pad"], ["pend"])
            S.op("vector", lambda e: e.tensor_tensor(out=pstart[:], in0=pend[:], in1=pad[:], op=ALU.subtract), ["pend", "pad"], ["pstart"])
            S.op("vector", lambda e: e.memset(be[:], 0.0), [], ["be"])
            for ex_ in range(NEXP):
                S.op("vector", lambda e, ex_=ex_: e.scalar_tensor_tensor(out=be[:], in0=g.cblk_t[:], scalar=pend[:, ex_:ex_ + 1], in1=be[:],
                                                                        op0=ALU.is_ge, op1=ALU.add), ["cblk", "pend", "be"], ["be"])
            S.op("vector", lambda e: e.tensor_scalar(out=be[:], in0=be[:], scalar1=float(NEXP - 1), scalar2=None, op0=ALU.min), ["be"], ["be"])
            S.op("vector", lambda e: e.tensor_scalar(out=bef[:], in0=be[:], scalar1=float(D), scalar2=g.iota_p, op0=ALU.mult, op1=ALU.add),
                 ["be", "cst"], ["bef"])
            S.op("vector", lambda e: e.tensor_copy(out=idxw[:], in_=bef[:]), ["bef"], ["idxw"])
            S.op("vector", lambda e: e.tensor_scalar(out=bef[:], in0=be[:], scalar1=128.0, scalar2=g.iota_p, op0=ALU.mult, op1=ALU.add),
                 ["be", "cst"], ["bef"])
            S.op("vector", lambda e: e.tensor_copy(out=idxb[:], in_=bef[:]), ["bef"], ["idxb"])
            S.op("vector", lambda e: e.tensor_copy(out=idxe[:], in_=be[:]), ["be"], ["idxe"])
            dall = sba("a_dall", [128, nsub, NEXP])
            oh = sba("a_oh", [128, nsub, NEXP])
            allsi_lg = [("lg", i) for i in range(nsub)]
            allsi_rk = [("rk", i) for i in range(nsub)]
            allsi_t8 = [("t8", i) for i in range(nsub)]
            S.op("vector", lambda e: e.tensor_tensor(out=dall[:], in0=rk_all[:], in1=bc(pstart[:], [128, nsub, NEXP], 1), op=ALU.add),
                 allsi_rk + ["pstart"], ["dall"])
            for k in range(4):
                S.op("vector", lambda e, k=k: e.tensor_tensor(out=oh[:], in0=lg_all[:], in1=bc(t8_all[:, :, k], [128, nsub, NEXP], 2),
                                                              op=ALU.is_equal), allsi_lg + allsi_t8, ["oh"])
                S.op("vector", lambda e: e.tensor_tensor(out=oh[:], in0=oh[:], in1=dall[:], op=ALU.mult), ["oh", "dall"], ["oh"])
                S.op("vector", lambda e, k=k: e.tensor_reduce(out=dest_f[:, :, k], in_=oh[:], axis=AX.X, op=ALU.add), ["oh"], ["dest_f"])
            S.op("vector", lambda e: e.tensor_copy(out=dest_i[:], in_=dest_f[:]), ["dest_f"], ["dest_i"])
            for si in range(nsub):
                hr = hrow[si % 2]
                hrn = "a_hrow%d" % (si % 2)
                jn, s = divmod(si, 4)
                tok0 = tiles[jn] * 512 + s * 128
                S.dma("sync", hr[:], g.Htok[tok0:tok0 + 128, :], reads=[("Htok", si)], writes=[hrn])
                for k in range(4):
                    S.op("gpsimd", lambda e, hr=hr, si=si, k=k: e.indirect_dma_start(
                        out=g.Hs[:, :], out_offset=bass.IndirectOffsetOnAxis(ap=dest_i[:, si, k:k + 1], axis=0), in_=hr[:],
                        in_offset=None), [hrn, "dest_i"], [("Hs", si, k)], dma=True)
            S.barrier()

        with ExitStack() as sd:
            sbd = lambda n, s, d=F32: g.sb(n, s, d, sd)
            guw = [sbd("d_guw%d" % i, [128, NCH, 2 * D], BF16) for i in range(2)]
            dnw = [sbd("d_dnw%d" % i, [128, NCH, D], BF16) for i in range(2)]
            gub = [sbd("d_gub%d" % i, [128, 16]) for i in range(2)]
            dnb = [sbd("d_dnb%d" % i, [128, D]) for i in range(2)]
            hs = [sbd("d_hs%d" % i, [128, 4, D], BF16) for i in range(2)]
            hsT = sbd("d_hsT", [128, NCH, BLK], BF16)
            gt = [sbd("d_gt%d" % i, [128, BLK]) for i in range(2)]
            sg = [sbd("d_sg%d" % i, [128, BLK]) for i in range(2)]
            ut = [sbd("d_ut%d" % i, [128, BLK]) for i in range(2)]
            act = sbd("d_act", [128, NCH, BLK], BF16)
            yt = [sbd("d_yt%d" % i, [128, D]) for i in range(2)]
            trp = [g.ps("d_trp%d" % i, [128, BLK], BF16, sd) for i in range(2)]
            pg = [g.ps("d_pg%d" % i, [128, BLK], F32, sd) for i in range(2)]
            pu = [g.ps("d_pu%d" % i, [128, BLK], F32, sd) for i in range(2)]
            py = [g.ps("d_py%d" % i, [128, BLK], F32, sd) for i in range(2)]
            guv = g.gu_w.rearrange("l r c -> (l r) c")
            dnv = g.dn_w.rearrange("l r c -> (l r) c")
            gbv = g.gu_bT.rearrange("l r c -> (l r) c")
            dbv = g.dn_b.rearrange("l r c -> (l r) c")
            yk = [0]
            hsT2 = [hsT, sbd("d_hsT1", [128, NCH, BLK], BF16)]

            def names(b):
                p = b % 2
                return p, "d_guw%d" % p, "d_dnw%d" % p, "d_gub%d" % p, "d_dnb%d" % p, "d_hs%d" % p

            def emitLoad(b):
                p, wn, dn_, gbn, dbn, hsn = names(b)
                for kc in range(NCH):
                    S.op("gpsimd", lambda e, b=b, kc=kc, p=p: e.indirect_dma_start(
                        out=guw[p][:, kc, :], out_offset=None, in_=guv[:, :],
                        in_offset=bass.IndirectOffsetOnAxis(ap=idxw[:, b:b + 1], axis=0), element_offset=(li * NEXP * D + kc * 128) * 2 * D),
                        ["idxw"], [(wn, kc)], dma=True)
                for kc in range(NCH):
                    S.op("gpsimd", lambda e, b=b, kc=kc, p=p: e.indirect_dma_start(
                        out=dnw[p][:, kc, :], out_offset=None, in_=dnv[:, :],
                        in_offset=bass.IndirectOffsetOnAxis(ap=idxw[:, b:b + 1], axis=0), element_offset=(li * NEXP * D + kc * 128) * D),
                        ["idxw"], [(dn_, kc)], dma=True)
                S.op("gpsimd", lambda e, b=b, p=p: e.indirect_dma_start(
                    out=gub[p][:], out_offset=None, in_=gbv[:, :],
                    in_offset=bass.IndirectOffsetOnAxis(ap=idxb[:, b:b + 1], axis=0), element_offset=li * NEXP * 128 * 16), ["idxb"], [gbn], dma=True)
                S.op("gpsimd", lambda e, b=b, p=p: e.indirect_dma_start(
                    out=dnb[p][:], out_offset=None, in_=dbv[:, :],
                    in_offset=bass.IndirectOffsetOnAxis(ap=idxe[:, b:b + 1], axis=0), element_offset=li * NEXP * D), ["idxe"], [dbn], dma=True)
                S.dma("sync", hs[p][:], g.Hs[b * BLK:(b + 1) * BLK, :].rearrange("(s p) f -> p s f", p=128), reads=[], writes=[hsn])

            def emitT(b):
                p, wn, dn_, gbn, dbn, hsn = names(b)
                hT = hsT2[p]
                for kc in range(NCH):
                    tp = trp[kc % 2]
                    tpn = "d_trp%d" % (kc % 2)
                    for s_ in range(4):
                        S.op("tensor", lambda e, tp=tp, s_=s_, kc=kc, p=p: e.transpose(
                            tp[:, s_ * 128:(s_ + 1) * 128], hs[p][:, s_, kc * 128:(kc + 1) * 128], g.ident_bf[:]),
                            [hsn, "ident_bf"], [tpn])
                    S.op("scalar", lambda e, tp=tp, kc=kc, hT=hT: e.activation(out=hT[:, kc, :], in_=tp[:], func=AF.Copy), [tpn], [("hsT", p, kc)])

            def emitGU(b):
                p, wn, dn_, gbn, dbn, hsn = names(b)
                hT = hsT2[p]
                for fc in range(NCH):
                    q = fc % 2
                    for kc in range(NCH):
                        S.op("tensor", lambda e, fc=fc, kc=kc, p=p, q=q, hT=hT: e.matmul(
                            pg[q][:], lhsT=guw[p][:, kc, fc * 128:(fc + 1) * 128], rhs=hT[:, kc, :], start=(kc == 0),
                            stop=(kc == NCH - 1)), [(wn, kc), ("hsT", p, kc)], ["d_pg%d" % q])
                    for kc in range(NCH):
                        S.op("tensor", lambda e, fc=fc, kc=kc, p=p, q=q, hT=hT: e.matmul(
                            pu[q][:], lhsT=guw[p][:, kc, D + fc * 128:D + (fc + 1) * 128], rhs=hT[:, kc, :], start=(kc == 0),
                            stop=(kc == NCH - 1)), [(wn, kc), ("hsT", p, kc)], ["d_pu%d" % q])
                    gtn, sgn, utn = "d_gt%d" % q, "d_sg%d" % q, "d_ut%d" % q
                    S.op("vector", lambda e, fc=fc, p=p, q=q: e.tensor_scalar(
                        out=gt[q][:], in0=pg[q][:], scalar1=gub[p][:, fc:fc + 1], scalar2=7.0, op0=ALU.add, op1=ALU.min),
                        ["d_pg%d" % q, gbn], [gtn])
                    S.op("scalar", lambda e, q=q: e.activation(out=sg[q][:], in_=gt[q][:], func=AF.Sigmoid, scale=1.702), [gtn], [sgn])
                    S.op("vector", lambda e, fc=fc, p=p, q=q: e.tensor_scalar(
                        out=ut[q][:], in0=pu[q][:], scalar1=gub[p][:, 8 + fc:9 + fc], scalar2=7.0, op0=ALU.add, op1=ALU.min),
                        ["d_pu%d" % q, gbn], [utn])
                    S.op("gpsimd", lambda e, q=q: e.tensor_scalar(out=ut[q][:], in0=ut[q][:], scalar1=-7.0, scalar2=1.0, op0=ALU.max,
                                                                  op1=ALU.add), [utn], [utn])
                    S.op("gpsimd", lambda e, q=q: e.tensor_tensor(out=gt[q][:], in0=gt[q][:], in1=sg[q][:], op=ALU.mult), [gtn, sgn], [gtn])
                    S.op("vector", lambda e, fc=fc, q=q: e.tensor_tensor(out=act[:, fc, :], in0=gt[q][:], in1=ut[q][:], op=ALU.mult),
                         [gtn, utn], [("act", fc)])

            def emitDN(b):
                p, wn, dn_, gbn, dbn, hsn = names(b)
                for s_ in range(4):
                    y = yt[yk[0] % 2]
                    yn = "d_yt%d" % (yk[0] % 2)
                    yk[0] += 1
                    for half in range(2):
                        for fc in range(NCH):
                            S.op("tensor", lambda e, s_=s_, half=half, fc=fc, p=p: e.matmul(
                                py[half][:], lhsT=act[:, fc, s_ * 128:(s_ + 1) * 128], rhs=dnw[p][:, fc, half * 512:(half + 1) * 512],
                                start=(fc == 0), stop=(fc == NCH - 1)), [("act", fc), (dn_, fc)], ["d_py%d" % half])
                        S.op("vector", lambda e, half=half, y=y, p=p: e.tensor_tensor(
                            out=y[:, half * 512:(half + 1) * 512], in0=py[half][:], in1=dnb[p][:, half * 512:(half + 1) * 512], op=ALU.add),
                            ["d_py%d" % half, dbn], [(yn, half)])
                    r0 = b * BLK + s_ * 128
                    S.dma("sync", g.Ys[r0:r0 + 128, :], y[:], reads=[(yn, 0), (yn, 1)], writes=[("Ys", b, s_)])

            emitLoad(0)
            emitT(0)
            for b in range(NB):
                if b + 1 < NB:
                    emitLoad(b + 1)
                emitGU(b)
                if b + 1 < NB:
                    emitT(b + 1)
                emitDN(b)
            S.barrier()

        with ExitStack() as se:
            sbe = lambda n, s, d=F32: g.sb(n, s, d, se)
            xt = [sbe("e_xt%d" % i, [128, NCH, 512]) for i in range(2)]
            yk_t = [sbe("e_yk%d" % i, [128, 4, D]) for i in range(2)]
            dg = [sbe("e_dg%d" % i, [128, 4, 128]) for i in range(2)]
            fp = [g.ps("e_fp%d" % i, [128, NCH, 128], F32, se) for i in range(2)]
            si = 0
            for jn, j in enumerate(tiles):
                x = xt[jn % 2]
                xn = "e_xt%d" % (jn % 2)
                stream = stream_of_tile(j)
                S.dma("sync", x[:], XTv[:, :, j * 512:(j + 1) * 512], reads=[("XT", j)], writes=[xn])
                for s in range(4):
                    q = si % 2
                    for k in range(4):
                        S.op("gpsimd", lambda e, q=q, si=si, k=k: e.indirect_dma_start(
                            out=yk_t[q][:, k, :], out_offset=None, in_=g.Ys[:, :],
                            in_offset=bass.IndirectOffsetOnAxis(ap=dest_i[:, si, k:k + 1], axis=0)), ["dest_i"],
                            [("e_yk%d" % q, k)], dma=True)
                        S.op("vector", lambda e, q=q, si=si, k=k: e.tensor_scalar(
                            out=dg[q][:, k, :], in0=g.ident_f, scalar1=gates[:, si, k:k + 1], scalar2=None, op0=ALU.mult),
                            [("gates", si), "cst"], [("e_dg%d" % q, k)])
                    for c in range(NCH):
                        for k in range(4):
                            S.op("tensor", lambda e, q=q, c=c, k=k: e.matmul(
                                fp[q][:, c, :], lhsT=yk_t[q][:, k, c * 128:(c + 1) * 128], rhs=dg[q][:, k, :], start=(k == 0), stop=(k == 3)),
                                [("e_yk%d" % q, k), ("e_dg%d" % q, k)], ["e_fp%d" % q])
                    for c in range(NCH):
                        S.op("vector", lambda e, q=q, c=c, x=x, s=s, stream=stream: e.scalar_tensor_tensor(
                            out=x[:, c, s * 128:(s + 1) * 128], in0=fp[q][:, c, :], scalar=g.mods[l][:, 5, c, stream:stream + 1],
                            in1=x[:, c, s * 128:(s + 1) * 128], op0=ALU.mult, op1=ALU.add), ["e_fp%d" % q, xn, ("mods", l)], [xn])
                    si += 1
                S.dma("scalar", XTv[:, :, j * 512:(j + 1) * 512], x[:], reads=[xn], writes=[("XT", j)])
            S.barrier()


TWO_PI = 6.283185307179586
NBK = 544


def mkap(base, step, cnt):
    return bass.AP(tensor=base.tensor, offset=base.offset, ap=[list(base.ap[0]), [step, cnt]])


def s5_layer(g, l):
    nc, S = g.nc, g.S
    jj = l // 3
    last = (l == 3)
    XTv = g.XT.rearrange("(c p) t -> p c t", p=128)
    HTv = g.HT.rearrange("(c p) t -> p c t", p=128)

    def V(fn, r, w, eng="vector"):
        S.op(eng, fn, r, w)

    def tt(out, a, b, op, r, w, eng="vector"):
        S.op(eng, lambda e: e.tensor_tensor(out=out, in0=a, in1=b, op=op), r, w)

    def ts(out, a, s1, s2, op0, op1, r, w):
        if s2 is None:
            S.op("vector", lambda e: e.tensor_scalar(out=out, in0=a, scalar1=s1, scalar2=None, op0=op0), r, w)
        else:
            S.op("vector", lambda e: e.tensor_scalar(out=out, in0=a, scalar1=s1, scalar2=s2, op0=op0, op1=op1), r, w)

    def act(out, in_, func, r, w, **kw):
        S.op("scalar", lambda e: e.activation(out=out, in_=in_, func=func, **kw), r, w)

    def sinred(y, out, ki, kf, fr, quarter, rd, tagw, names=("sr_ki", "sr_kf", "sr_fr")):
        nki, nkf, nfr = names
        if quarter:
            ts(fr, y, 0.25, None, ALU.add, None, rd, [nfr])
            src, rsrc = fr, [nfr]
        else:
            src, rsrc = y, rd
        V(lambda e: e.tensor_copy(out=ki, in_=src), rsrc, [nki])
        V(lambda e: e.tensor_copy(out=kf, in_=ki), [nki], [nkf])
        tt(fr, src, kf, ALU.subtract, rsrc + [nkf], [nfr])
        ts(kf, fr, 0.5, None, ALU.is_gt, None, [nfr], [nkf])
        tt(fr, fr, kf, ALU.subtract, [nfr, nkf], [nfr])
        act(out, fr, AF.Sin, [nfr], tagw, scale=TWO_PI)

    def discretize(tag, pv, Fn, Pre, Pim, co, fr8, svals, st2):
        sb2 = lambda n, s_, d=F32: g.sb("z" + tag + n, s_, d, st2)
        dt = sb2("dt", [128, Fn]); dta = sb2("dta", [128, Fn]); ang = sb2("ang", [128, Fn])
        eS = sb2("eS", [128, Fn, 9]); yS = sb2("yS", [128, Fn, 9])
        ki = sb2("ki", [128, Fn, 9], I32); kf = sb2("kf", [128, Fn, 9]); fr = sb2("fr", [128, Fn, 9])
        t1 = sb2("t1", [128, Fn]); t2 = sb2("t2", [128, Fn]); den = sb2("den", [128, Fn])
        pn = "s5prm" + tag
        act(dt[:], pv[:, :, 2], AF.Exp, [pn], ["dz_dt"])
        tt(dta[:], dt[:], pv[:, :, 0], ALU.mult, ["dz_dt", pn], ["dz_dta"])
        tt(ang[:], dt[:], pv[:, :, 1], ALU.mult, ["dz_dt", pn], ["dz_ang"])
        tt(eS[:], bc(dta[:], [128, Fn, 9], 2), bc(svals[:], [128, Fn, 9], 1), ALU.mult, ["dz_dta", "svals"], ["dz_eS"])
        act(eS[:], eS[:], AF.Exp, ["dz_eS"], ["dz_eS"])
        tt(yS[:], bc(ang[:], [128, Fn, 9], 2), bc(svals[:], [128, Fn, 9], 1), ALU.mult, ["dz_ang", "svals"], ["dz_yS"])
        ts(yS[:], yS[:], 1.0 / TWO_PI, None, ALU.mult, None, ["dz_yS"], ["dz_yS"])
        sinred(yS[:], Pim, ki[:], kf[:], fr[:], False, ["dz_yS"], [tag + "P"])
        V(lambda e: e.tensor_copy(out=fr8, in_=fr[:, :, 8]), ["sr_fr"], [tag + "fr8"])
        sinred(yS[:], Pre, ki[:], kf[:], fr[:], True, ["dz_yS"], [tag + "P"])
        tt(Pre, Pre, eS[:], ALU.mult, ["dz_eS", tag + "P"], [tag + "P"])
        tt(Pim, Pim, eS[:], ALU.mult, ["dz_eS", tag + "P"], [tag + "P"])
        Pre1, Pim1 = Pre[:, :, 1], Pim[:, :, 1]
        are, aim = pv[:, :, 0], pv[:, :, 1]
        tt(den[:], are, are, ALU.mult, [pn], ["dz_den"])
        tt(t1[:], aim, aim, ALU.mult, [pn], ["dz_t1"])
        tt(den[:], den[:], t1[:], ALU.add, ["dz_den", "dz_t1"], ["dz_den"])
        V(lambda e: e.reciprocal(out=den[:], in_=den[:]), ["dz_den"], ["dz_den"])
        ts(t1[:], Pre1, -1.0, None, ALU.add, None, [tag + "P"], ["dz_t1"])
        tt(t2[:], t1[:], are, ALU.mult, ["dz_t1", pn], ["dz_t2"])
        tt(dt[:], Pim1, aim, ALU.mult, [tag + "P", pn], ["dz_dt"])
        tt(t2[:], t2[:], dt[:], ALU.add, ["dz_t2", "dz_dt"], ["dz_t2"])
        tt(co[:, 0, :], t2[:], den[:], ALU.mult, ["dz_t2", "dz_den"], [tag + "co"])
        tt(t2[:], Pim1, are, ALU.mult, [tag + "P", pn], ["dz_t2"])
        tt(dt[:], t1[:], aim, ALU.mult, ["dz_t1", pn], ["dz_dt"])
        tt(t2[:], t2[:], dt[:], ALU.subtract, ["dz_t2", "dz_dt"], ["dz_t2"])
        tt(co[:, 1, :], t2[:], den[:], ALU.mult, ["dz_t2", "dz_den"], [tag + "co"])

    with ExitStack() as st:
        sb = lambda n, s_, d=F32: g.sb(n, s_, d, st)
        xt = [sb("n_xt%d" % i, [128, NCH, 512]) for i in range(2)]
        sq = sb("n_sq", [128, NCH, 512], BF16)
        tmp = sb("n_tmp", [128, NCH, 512])
        hbf = [sb("n_hbf%d" % i, [128, NCH, 512], BF16) for i in range(2)]
        rstd = sb("n_rstd", [128, 512])
        ssp = g.ps("n_ssp", [128, 512], F32, st)
        for j in range(NT):
            x, xn = xt[j % 2], "n_xt%d" % (j % 2)
            S.dma("sync", x[:], XTv[:, :, j * 512:(j + 1) * 512], reads=[("XT", j)], writes=[xn])
            normmod(g, l, 1, x, xn, sq, ssp, rstd, tmp, None, hbf[j % 2], stream_of_tile(j), tag="N", hbfn="hbfN%d" % (j % 2))
            S.dma("sync", HTv[:, :, j * 512:(j + 1) * 512], hbf[j % 2][:], reads=["hbfN%d" % (j % 2)], writes=[("HT", j)])
        S.barrier()

    with ExitStack() as st:
        sb = lambda n, s_, d=F32: g.sb(n, s_, d, st)
        svals = sb("s_svals", [128, 9])
        irow = sb("s_irow", [128, NBK])
        S.dma("sync", svals[:], g.s5c[:, 0:9], writes=["svals"])
        S.dma("sync", irow[:], g.s5c[:, 16:16 + NBK], writes=["irow"])
        PB_re = sb("s_PBre", [128, 64, 9]); PB_im = sb("s_PBim", [128, 64, 9]); coB = sb("s_coB", [128, 2, 64]); fr8B = sb("s_fr8B", [128, 64])
        with ExitStack() as st2:
            prmB = g.sb("s_prmB", [128, 64, 3], F32, st2)
            S.dma("sync", prmB[:], g.s5B[jj].rearrange("p d q k -> p (d q) k"), writes=["s5prmB"])
            discretize("B", prmB, 64, PB_re[:], PB_im[:], coB, fr8B[:], svals, st2)
            S.barrier()
        PBr = PB_re[:].rearrange("p (d q) k -> p d q k", d=2); PBi = PB_im[:].rearrange("p (d q) k -> p d q k", d=2)
        coBv = coB[:].rearrange("p r (d q) -> p r d q", d=2)
        fr8Bv = fr8B[:].rearrange("p (d q) -> p d q", d=2)
        rho = sb("s_rho", [128, 2, 32])
        t_r = sb("s_tr", [128, 2, 32]); t_i = sb("s_ti", [128, 2, 32])
        tt(t_r[:], PBr[:, :, :, 8], PBr[:, :, :, 8], ALU.mult, ["BP"], ["s_tr"])
        tt(t_i[:], PBi[:, :, :, 8], PBi[:, :, :, 8], ALU.mult, ["BP"], ["s_ti"])
        tt(t_r[:], t_r[:], t_i[:], ALU.add, ["s_tr", "s_ti"], ["s_tr"])
        act(rho[:], t_r[:], AF.Sqrt, ["s_tr"], ["rho"])
        dsk = sb("s_dsk", [128, NCH])
        S.dma("sync", dsk[:], g.s5d[jj, :, :], writes=["dsk"])

        Vw = sb("s_Vw", [128, 2, 8, 2, 512], BF16)
        Y1 = sb("s_Y1", [128, 2, 8, 2, 4, 128], BF16)
        Kw = sb("s_Kw", [128, 15, 128], BF16)
        XS = [sb("s_XS%d" % d, [128, 2, 4, NBK + 1], BF16) for d in range(2)]
        for d in range(2):
            V(lambda e, d=d: e.memset(XS[d][:], 0.0), [], ["XS%d" % d])
        kps = g.ps("s_kps", [128, 128], F32, st)
        vps_c = [g.ps("s_vpc%d" % i, [128, 32], F32, st) for i in range(2)]
        vps_l = [g.ps("s_vpl%d" % i, [128, 512], F32, st) for i in range(2)]

        for ct in range(NCH):
            with ExitStack() as spa:
                PA_re = g.sb("p_PAre", [128, 128, 9], F32, spa); PA_im = g.sb("p_PAim", [128, 128, 9], F32, spa)
                coA = g.sb("p_coA", [128, 2, 128], F32, spa); fr8A = g.sb("p_fr8A", [128, 128], F32, spa)
                with ExitStack() as st2:
                    prmA4 = g.sb("p_prmA", [128, 2, 64, 3], F32, st2)
                    S.dma("sync", prmA4[:], g.s5A[jj][:, :, ct, :, :], writes=["s5prmA"])
                    prmA = prmA4[:].rearrange("p d q k -> p (d q) k")
                    discretize("A", prmA, 128, PA_re[:], PA_im[:], coA, fr8A[:], svals, st2)
                    S.barrier()
                PAr = PA_re[:].rearrange("p (d q) k -> p d q k", d=2); PAi = PA_im[:].rearrange("p (d q) k -> p d q k", d=2)
                coAv = coA[:].rearrange("p r (d q) -> p r d q", d=2)
                with ExitStack() as sp:
                    sbp = lambda n, s_, d=F32: g.sb(n, s_, d, sp)
                    BzB = sbp("p_BzB", [128, 2, 2, 4, 128]); CzB = sbp("p_CzB", [128, 2, 2, 4, 128]); bbz = sbp("p_bbz", [128, 2, 2, 4, 128])
                    BzA = sbp("p_BzA", [128, 2, 2, 8, 64]); bbA = sbp("p_bbA", [128, 2, 2, 8, 64])
                    u1 = sbp("p_u1", [128, 2, 4, 128]); u2 = sbp("p_u2", [128, 2, 4, 128])
                    cp = sbp("p_cp", [128, 2, 4, 128])
                    w1 = sbp("p_w1", [128, 8, 64]); w2 = sbp("p_w2", [128, 8, 64])
                    dgd = sbp("p_dgd", [128, 128])
                    S.dma("sync", BzB[:], g.s5BzB[jj][:, :, :, 4 * ct:4 * ct + 4, :], writes=["BzB"])
                    S.dma("sync", CzB[:], g.s5CzB[jj][:, :, :, 4 * ct:4 * ct + 4, :], writes=["CzB"])
                    S.dma("sync", BzA[:], g.s5BzA[jj][:, :, :, ct, :, :], writes=["BzA"])
                    co_re = bc(coBv[:, 0, :, 4 * ct:4 * ct + 4], [128, 2, 4, 128], 3)
                    co_im = bc(coBv[:, 1, :, 4 * ct:4 * ct + 4], [128, 2, 4, 128], 3)
                    tt(u1[:], BzB[:, :, 0], co_re, ALU.mult, ["BzB", "Bco"], ["u1"])
                    tt(u2[:], BzB[:, :, 1], co_im, ALU.mult, ["BzB", "Bco"], ["u2"])
                    tt(bbz[:, :, 0], u1[:], u2[:], ALU.subtract, ["u1", "u2"], ["bbz"])
                    tt(u1[:], BzB[:, :, 1], co_re, ALU.mult, ["BzB", "Bco"], ["u1"])
                    tt(u2[:], BzB[:, :, 0], co_im, ALU.mult, ["BzB", "Bco"], ["u2"])
                    tt(bbz[:, :, 1], u1[:], u2[:], ALU.add, ["u1", "u2"], ["bbz"])
                    for d in range(2):
                        cr = bc(coAv[:, 0, d, :], [128, 8, 64], 1)
                        ci = bc(coAv[:, 1, d, :], [128, 8, 64], 1)
                        tt(w1[:], BzA[:, d, 0], cr, ALU.mult, ["BzA", "Aco"], ["w1"])
                        tt(w2[:], BzA[:, d, 1], ci, ALU.mult, ["BzA", "Aco"], ["w2"])
                        tt(bbA[:, d, 0], w1[:], w2[:], ALU.subtract, ["w1", "w2"], ["bbA"])
                        tt(w1[:], BzA[:, d, 1], cr, ALU.mult, ["BzA", "Aco"], ["w1"])
                        tt(w2[:], BzA[:, d, 0], ci, ALU.mult, ["BzA", "Aco"], ["w2"])
                        tt(bbA[:, d, 1], w1[:], w2[:], ALU.add, ["w1", "w2"], ["bbA"])
                    for d in range(2):
                        for s in range(8):
                            pw = 7 - s if d == 0 else s
                            pr = bc(PAr[:, d, :, pw], [128, 8, 64], 1)
                            pi = bc(PAi[:, d, :, pw], [128, 8, 64], 1)
                            o_re = Vw[:, d, s, 0, :].rearrange("p (a b) -> p a b", a=8)
                            o_im = Vw[:, d, s, 1, :].rearrange("p (a b) -> p a b", a=8)
                            tt(w1[:], bbA[:, d, 0], pr, ALU.mult, ["bbA", "AP"], ["w1"])
                            tt(w2[:], bbA[:, d, 1], pi, ALU.mult, ["bbA", "AP"], ["w2"])
                            tt(o_re, w1[:], w2[:], ALU.subtract, ["w1", "w2"], ["Vw"])
                            tt(w1[:], bbA[:, d, 1], pr, ALU.mult, ["bbA", "AP"], ["w1"])
                            tt(w2[:], bbA[:, d, 0], pi, ALU.mult, ["bbA", "AP"], ["w2"])
                            tt(o_im, w1[:], w2[:], ALU.add, ["w1", "w2"], ["Vw"])
                    ts(dgd[:], g.ident_f, dsk[:, ct:ct + 1], None, ALU.mult, None, ["cst", "dsk"], ["dgd"])
                    for k in range(9):
                        for d in range(2):
                            pr = bc(PBr[:, d, 4 * ct:4 * ct + 4, k], [128, 4, 128], 2)
                            pi = bc(PBi[:, d, 4 * ct:4 * ct + 4, k], [128, 4, 128], 2)
                            tt(u1[:, 0], CzB[:, d, 0], pr, ALU.mult, ["CzB", "BP"], ["u1"])
                            tt(u2[:, 0], CzB[:, d, 1], pi, ALU.mult, ["CzB", "BP"], ["u2"])
                            tt(cp[:, 0], u1[:, 0], u2[:, 0], ALU.subtract, ["u1", "u2"], ["cp"])
                            tt(u1[:, 0], CzB[:, d, 0], pi, ALU.mult, ["CzB", "BP"], ["u1"])
                            tt(u2[:, 0], CzB[:, d, 1], pr, ALU.mult, ["CzB", "BP"], ["u2"])
                            V(lambda e: e.scalar_tensor_tensor(out=cp[:, 1], in0=u1[:, 0], scalar=-1.0, in1=u2[:, 0], op0=ALU.mult, op1=ALU.subtract),
                              ["u1", "u2"], ["cp"])
                            if k >= 1:
                                S.op("gpsimd", lambda e, d=d, k=k: e.tensor_copy(out=Y1[:, d, k - 1], in_=cp[:]), ["cp"], ["Y1"])
                            if k <= 7:
                                n = 0
                                for q in range(4):
                                    for ri in range(2):
                                        st_ = (n == 0) and (k > 0 or d == 0)
                                        sp_ = (n == 7) and (k > 0)
                                        S.op("tensor", lambda e, d=d, ri=ri, q=q, st_=st_, sp_=sp_: e.matmul(
                                            kps[:], lhsT=bbz[:, d, ri, q, :], rhs=cp[:, ri, q, :], start=st_, stop=sp_), ["bbz", "cp"], ["kps"])
                                        n += 1
                                if k == 0 and d == 1:
                                    S.op("tensor", lambda e: e.matmul(kps[:], lhsT=g.ident_f, rhs=dgd[:], start=False, stop=True), ["cst", "dgd"], ["kps"])
                                if k > 0 or d == 1:
                                    slot = 0 if k == 0 else (k if d == 0 else 7 + k)
                                    act(Kw[:, slot, :], kps[:], AF.Copy, ["kps"], ["Kw"])
                    S.barrier()

            with ExitStack() as sd:
                sbd = lambda n, s_, d=F32: g.sb(n, s_, d, sd)
                Us = sbd("d_U", [128, 4352], BF16)
                Ys = sbd("d_Y", [128, 4352], BF16)
                Vb = sbd("d_V", [128, 2, 2, NBK]); Wb = sbd("d_W", [128, 2, 2, NBK]); Tb = sbd("d_T", [128, 2, NBK])
                cosT = sbd("d_cosT", [128, 2, NBK]); sinT = sbd("d_sinT", [128, 2, NBK]); tki = sbd("d_tki", [128, 2, NBK], I32)
                gq = sbd("d_gq", [128, 512]); gt2 = sbd("d_gt2", [128, 512]); gsg = sbd("d_gsg", [128, 512])
                Un, Yn = "d_U", "d_Y"
                for sq_ in range(2):
                    cc = slice(sq_ * 256, sq_ * 256 + 256)
                    lc = slice(512 + sq_ * 4096, 512 + (sq_ + 1) * 4096)
                    S.dma("sync", Us[:, 0:256], g.HT[ct * 128:(ct + 1) * 128, cc], reads=[], writes=[Un])
                    S.dma("sync", Us[:, 256:4352], g.HT[ct * 128:(ct + 1) * 128, lc], reads=[], writes=[Un])

                    def ucols(start, step, cnt):
                        return mkap(Us[:, start:start + 1], step, cnt)

                    for d in range(2):
                        for qh in range(2):
                            q0 = 4 * ct + 2 * qh
                            tt(Tb[:], bc(fr8Bv[:, d, q0:q0 + 2], [128, 2, NBK], 2), bc(irow[:], [128, 2, NBK], 1), ALU.mult, ["Bfr8", "irow"], ["Tb"])
                            sinred(Tb[:], sinT[:], tki[:], Wb[:, 0], Wb[:, 1], False, ["Tb"], ["sinT"], names=("tki", "Wb", "Wb"))
                            sinred(Tb[:], cosT[:], tki[:], Wb[:, 0], Wb[:, 1], True, ["Tb"], ["cosT"], names=("tki", "Wb", "Wb"))
                            n = 0
                            for ql in range(2):
                                q = 2 * qh + ql
                                for ri in range(2):
                                    pc, pl = vps_c[n % 2], vps_l[n % 2]
                                    pcn, pln = "s_vpc%d" % (n % 2), "s_vpl%d" % (n % 2)
                                    n += 1
                                    for s in range(8):
                                        if d == 0:
                                            rc, rl = ucols(s, 8, 32), ucols(256 + s, 8, 512)
                                        else:
                                            rc, rl = ucols(31 * 8 + s, -8, 32), ucols(256 + 511 * 8 + s, -8, 512)
                                        S.op("tensor", lambda e, d=d, s=s, ri=ri, q=q, pc=pc, rc=rc: e.matmul(
                                            pc[:], lhsT=Vw[:, d, s, ri, q * 128:(q + 1) * 128], rhs=rc, start=(s == 0), stop=(s == 7)), ["Vw", Un], [pcn])
                                        S.op("tensor", lambda e, d=d, s=s, ri=ri, q=q, pl=pl, rl=rl: e.matmul(
                                            pl[:], lhsT=Vw[:, d, s, ri, q * 128:(q + 1) * 128], rhs=rl, start=(s == 0), stop=(s == 7)), ["Vw", Un], [pln])
                                    act(Vb[:, ri, ql, 0:32], pc[:], AF.Copy, [pcn], ["Vb"])
                                    act(Vb[:, ri, ql, 32:NBK], pl[:], AF.Copy, [pln], ["Vb"])
                            tt(Wb[:, 0], Vb[:, 0], cosT[:], ALU.mult, ["Vb", "cosT"], ["Wb"])
                            tt(Tb[:], Vb[:, 1], sinT[:], ALU.mult, ["Vb", "sinT"], ["Tb"])
                            tt(Wb[:, 0], Wb[:, 0], Tb[:], ALU.add, ["Wb", "Tb"], ["Wb"])
                            tt(Wb[:, 1], Vb[:, 1], cosT[:], ALU.mult, ["Vb", "cosT"], ["Wb"])
                            tt(Tb[:], Vb[:, 0], sinT[:], ALU.mult, ["Vb", "sinT"], ["Tb"])
                            tt(Wb[:, 1], Wb[:, 1], Tb[:], ALU.subtract, ["Wb", "Tb"], ["Wb"])
                            for ql in range(2):
                                for ri in range(2):
                                    V(lambda e, d=d, ql=ql, ri=ri, q0=q0: e.tensor_tensor_scan(
                                        out=Vb[:, ri, ql, :], data0=rho[:, d, q0 + ql:q0 + ql + 1].to_broadcast([128, NBK]),
                                        data1=Wb[:, ri, ql, :], initial=0.0, op0=ALU.mult, op1=ALU.add), ["Wb", "rho", "Vb"], ["Vb"])
                            xs, xn = XS[d], "XS%d" % d
                            qs = slice(2 * qh, 2 * qh + 2)
                            tt(Wb[:, 0], Vb[:, 0], cosT[:], ALU.mult, ["Vb", "cosT"], ["Wb"])
                            tt(Tb[:], Vb[:, 1], sinT[:], ALU.mult, ["Vb", "sinT"], ["Tb"])
                            tt(xs[:, 0, qs, 1:NBK + 1], Wb[:, 0], Tb[:], ALU.subtract, ["Wb", "Tb"], [xn])
                            tt(Wb[:, 1], Vb[:, 1], cosT[:], ALU.mult, ["Vb", "cosT"], ["Wb"])
                            tt(Tb[:], Vb[:, 0], sinT[:], ALU.mult, ["Vb", "sinT"], ["Tb"])
                            tt(xs[:, 1, qs, 1:NBK + 1], Wb[:, 1], Tb[:], ALU.add, ["Wb", "Tb"], [xn])

                    def xcols(d, ri, q, start, step, cnt):
                        return mkap(XS[d][:, ri, q, start:start + 1], step, cnt)

                    def ycols(start, step, cnt):
                        return mkap(Ys[:, start:start + 1], step, cnt)

                    for s in range(8):
                        pc, pl = vps_c[s % 2], vps_l[s % 2]
                        pcn, pln = "s_vpc%d" % (s % 2), "s_vpl%d" % (s % 2)
                        mm = []
                        for q in range(4):
                            for ri in range(2):
                                mm.append((Y1[:, 0, s, ri, q, :], xcols(0, ri, q, 0, 1, 32), xcols(0, ri, q, 32, 1, 512), ["Y1", "XS0"]))
                                mm.append((Y1[:, 1, 7 - s, ri, q, :], xcols(1, ri, q, 31, -1, 32), xcols(1, ri, q, 543, -1, 512), ["Y1", "XS1"]))
                        for s2 in range(8):
                            slot = 0 if s2 == s else ((s - s2) if s2 < s else 7 + (s2 - s))
                            mm.append((Kw[:, slot, :], ucols(s2, 8, 32), ucols(256 + s2, 8, 512), ["Kw", Un]))
                        for i, (lw, rc, rl, rd) in enumerate(mm):
                            S.op("tensor", lambda e, lw=lw, rc=rc, pc=pc, i=i, nm=len(mm): e.matmul(pc[:], lhsT=lw, rhs=rc, start=(i == 0), stop=(i == nm - 1)), rd, [pcn])
                            S.op("tensor", lambda e, lw=lw, rl=rl, pl=pl, i=i, nm=len(mm): e.matmul(pl[:], lhsT=lw, rhs=rl, start=(i == 0), stop=(i == nm - 1)), rd, [pln])
                        for (pp, ppn, N, oc) in ((pc, pcn, 32, ycols(s, 8, 32)), (pl, pln, 512, ycols(256 + s, 8, 512))):
                            act(gq[:, :N], pp[:], AF.Square, [ppn], ["gq"])
                            ts(gq[:, :N], gq[:, :N], 0.044715, 1.0, ALU.mult, ALU.add, ["gq"], ["gq"])
                            tt(gt2[:, :N], gq[:, :N], pp[:], ALU.mult, ["gq", ppn], ["gt2"])
                            act(gsg[:, :N], gt2[:, :N], AF.Sigmoid, ["gt2"], ["gsg"], scale=1.5957691216057308)
                            tt(oc, gsg[:, :N], pp[:], ALU.mult, ["gsg", ppn], [Yn])
                    S.dma("sync", g.GT[ct * 128:(ct + 1) * 128, cc], Ys[:, 0:256], reads=[Yn], writes=[("GT", ct, sq_, 0)])
                    S.dma("sync", g.GT[ct * 128:(ct + 1) * 128, lc], Ys[:, 256:4352], reads=[Yn], writes=[("GT", ct, sq_, 1)])
                S.barrier()

    with ExitStack() as st:
        sb = lambda n, s_, d=F32: g.sb(n, s_, d, st)
        GTv = g.GT.rearrange("(c p) t -> p c t", p=128)
        gw = sb("g_w", [128, NCH, 2 * D], BF16)
        gb = sb("g_b", [128, 16])
        S.op("gpsimd", lambda e: e.dma_start(out=gw[:], in_=g.s5gw[jj].rearrange("(kc p) n -> p kc n", p=128)), [], ["g_w"], dma=True)
        S.dma("sync", gb[:], g.s5gb[jj, :, :], writes=["g_b"])
        xt = [sb("g_xt%d" % i, [128, NCH, 512]) for i in range(2)]
        gi = [sb("g_gi%d" % i, [128, NCH, 512], BF16) for i in range(2)]
        sgm = [sb("g_sg%d" % i, [128, 512]) for i in range(2)]
        yl = [sb("g_yl%d" % i, [128, 512]) for i in range(2)]
        pa = [g.ps("g_pa%d" % i, [128, 512], F32, st) for i in range(2)]
        pb = [g.ps("g_pb%d" % i, [128, 512], F32, st) for i in range(2)]
        tiles = list(range(1 if last else 0, NT))
        for jn, j in enumerate(tiles):
            x, xn = xt[jn % 2], "g_xt%d" % (jn % 2)
            gg, ggn = gi[jn % 2], "g_gi%d" % (jn % 2)
            stream = stream_of_tile(j)
            S.dma("sync", x[:], XTv[:, :, j * 512:(j + 1) * 512], reads=[("XT", j)], writes=[xn])
            S.dma("sync", gg[:], GTv[:, :, j * 512:(j + 1) * 512], reads=[], writes=[ggn])
            for oc in range(NCH):
                q = oc % 2
                for kc in range(NCH):
                    S.op("tensor", lambda e, oc=oc, kc=kc, q=q, gg=gg: e.matmul(pa[q][:], lhsT=gw[:, kc, oc * 128:(oc + 1) * 128], rhs=gg[:, kc, :],
                                                                               start=(kc == 0), stop=(kc == NCH - 1)), ["g_w", ggn], ["g_pa%d" % q])
                for kc in range(NCH):
                    S.op("tensor", lambda e, oc=oc, kc=kc, q=q, gg=gg: e.matmul(pb[q][:], lhsT=gw[:, kc, D + oc * 128:D + (oc + 1) * 128], rhs=gg[:, kc, :],
                                                                               start=(kc == 0), stop=(kc == NCH - 1)), ["g_w", ggn], ["g_pb%d" % q])
                act(sgm[q][:], pb[q][:], AF.Sigmoid, ["g_pb%d" % q, "g_b"], ["g_sg%d" % q], bias=gb[:, 8 + oc:9 + oc], scale=1.0)
                V(lambda e, oc=oc, q=q: e.scalar_tensor_tensor(out=yl[q][:], in0=pa[q][:], scalar=gb[:, oc:oc + 1], in1=sgm[q][:], op0=ALU.add, op1=ALU.mult),
                  ["g_pa%d" % q, "g_sg%d" % q, "g_b"], ["g_yl%d" % q])
                V(lambda e, oc=oc, q=q, x=x, stream=stream: e.scalar_tensor_tensor(
                    out=x[:, oc, :], in0=yl[q][:], scalar=g.mods[l][:, 2, oc, stream:stream + 1], in1=x[:, oc, :], op0=ALU.mult, op1=ALU.add),
                  ["g_yl%d" % q, xn, ("mods", l)], [xn])
            S.dma("sync", XTv[:, :, j * 512:(j + 1) * 512], x[:], reads=[xn], writes=[("XT", j)])
        S.barrier()


def ssd_layer(g, l):
    nc, S = g.nc, g.S
    last = (l == 3)
    XTv = g.XT.rearrange("(c p) t -> p c t", p=128)
    DI = 2 * D

    def V(fn, r, w, eng="vector"):
        S.op(eng, fn, r, w)

    def tt(out, a, b, op, r, w, eng="vector"):
        S.op(eng, lambda e: e.tensor_tensor(out=out, in0=a, in1=b, op=op), r, w)

    def act(out, in_, func, r, w, **kw):
        S.op("scalar", lambda e: e.activation(out=out, in_=in_, func=func, **kw), r, w)

    with ExitStack() as st:
        sb = lambda n, s_, d=F32: g.sb(n, s_, d, st)
        w = sb("m_w", [128, NCH, 6208], BF16)
        Wv = g.ssd_inw.rearrange("(kc p) n -> p kc n", p=128)
        for i in range(4):
            c0, c1 = i * 1552, (i + 1) * 1552
            S.op("gpsimd", lambda e, c0=c0, c1=c1: e.dma_start(out=w[:, :, c0:c1], in_=Wv[:, :, c0:c1]), [], [("m_w", i)], dma=True)
        wtok = [("m_w", i) for i in range(4)]
        xt = [sb("m_xt%d" % i, [128, NCH, 512]) for i in range(2)]
        sq = sb("m_sq", [128, NCH, 512], BF16)
        tmp = sb("m_tmp", [128, NCH, 512])
        hbf = sb("m_hbf", [128, NCH, 512], BF16)
        rstd = sb("m_rstd", [128, 512])
        ob = [sb("m_ob%d" % i, [128, 8, 512], BF16) for i in range(2)]
        dtb = sb("m_dtb", [64, 512])
        ssp = g.ps("m_ssp", [128, 512], F32, st)
        pp = [g.ps("m_pp%d" % i, [128, 512], F32, st) for i in range(3)]
        n = 0
        nb = 0
        for j in range(NT):
            x, xn = xt[j % 2], "m_xt%d" % (j % 2)
            cols = slice(j * 512, (j + 1) * 512)
            S.dma("sync", x[:], XTv[:, :, cols], reads=[("XT", j)], writes=[xn])
            normmod(g, l, 1, x, xn, sq, ssp, rstd, tmp, None, hbf, stream_of_tile(j), tag="M")
            for grp in range(6):
                o_, on = ob[nb % 2], "m_ob%d" % (nb % 2)
                nb += 1
                for ci in range(8):
                    oc = grp * 8 + ci
                    p_, pn = pp[n % 3], "m_pp%d" % (n % 3)
                    n += 1
                    for kc in range(NCH):
                        S.op("tensor", lambda e, kc=kc, oc=oc, p_=p_: e.matmul(p_[:], lhsT=w[:, kc, oc * 128:(oc + 1) * 128], rhs=hbf[:, kc, :],
                                                                           start=(kc == 0), stop=(kc == NCH - 1)), wtok + ["hbfM"], [pn])
                    act(o_[:, ci, :], p_[:], AF.Silu if grp < 2 else AF.Copy, [pn], [(on, ci)])
                rd = [(on, ci) for ci in range(8)]
                if grp < 2:
                    dstv = g.SZ.rearrange("(c p) t -> p c t", p=128)[:, grp * 8:(grp + 1) * 8, cols]
                else:
                    dstv = g.XBCp.rearrange("(c p) t -> p c t", p=128)[:, (grp - 2) * 8:(grp - 1) * 8, cols]
                S.dma("sync", dstv, o_[:], reads=rd, writes=[("inproj", j, grp)])
            p_, pn = pp[n % 3], "m_pp%d" % (n % 3)
            n += 1
            for kc in range(NCH):
                S.op("tensor", lambda e, kc=kc, p_=p_: e.matmul(p_[0:64, :], lhsT=w[:, kc, 6144:6208], rhs=hbf[:, kc, :], start=(kc == 0),
                                                             stop=(kc == NCH - 1)), wtok + ["hbfM"], [pn])
            act(dtb[:], p_[0:64, :], AF.Copy, [pn], ["dtb"])
            S.dma("sync", g.DTr[:, cols], dtb[:], reads=["dtb"], writes=[("dtr", j)])
        S.barrier()

    PBW = TCORE + 16
    seg = [(0, 256), (256, 256), (512, 4096), (4608, 4096)]
    segp = [2, 2 + 260, 2 + 520, 2 + 520 + 4100]
    with ExitStack() as st:
        sb = lambda n, s_, d=F32: g.sb(n, s_, d, st)
        pb_ = [sb("c_pb%d" % i, [128, PBW], BF16) for i in range(2)]
        acc = sb("c_acc", [128, PBW])
        cob = [sb("c_co%d" % i, [128, PBW], BF16) for i in range(2)]
        cw = sb("c_w", [128, 32, 6])
        tr_sb = [sb("c_tr%d" % i, [128, 4, 128], BF16) for i in range(2)]
        trp = [g.ps("c_trp%d" % i, [128, 4, 128], BF16, st) for i in range(2)]
        S.dma("sync", cw[:], g.ssd_cw[:, :, :], writes=["c_w"])
        for i in range(2):
            V(lambda e, i=i: e.memset(pb_[i][:], 0.0), [], ["c_pb%d" % i])
        L = PBW - 4
        ntr = 0
        for c in range(32):
            p_, pn = pb_[c % 2], "c_pb%d" % (c % 2)
            o_, on = cob[c % 2], "c_co%d" % (c % 2)
            for si, (gc, ln) in enumerate(seg):
                S.dma("sync", p_[:, segp[si]:segp[si] + ln], g.XBCp[c * 128:(c + 1) * 128, gc:gc + ln], writes=[pn])
            V(lambda e, c=c, p_=p_: e.tensor_scalar(out=acc[:, 0:L], in0=p_[:, 0:L], scalar1=cw[:, c, 0:1], scalar2=None, op0=ALU.mult), [pn, "c_w"], ["c_acc"])
            for k in range(1, 5):
                V(lambda e, c=c, k=k, p_=p_: e.scalar_tensor_tensor(out=acc[:, 0:L], in0=p_[:, k:k + L], scalar=cw[:, c, k:k + 1], in1=acc[:, 0:L],
                                                                   op0=ALU.mult, op1=ALU.add), [pn, "c_w", "c_acc"], ["c_acc"])
            act(o_[:, 2:2 + L], acc[:, 0:L], AF.Silu, ["c_acc", "c_w"], [on], bias=cw[:, c, 5:6], scale=1.0)
            for si, (gc, ln) in enumerate(seg):
                src = o_[:, segp[si]:segp[si] + ln]
                if c < 16:
                    S.dma("sync", g.XFo[c * 128:(c + 1) * 128, gc:gc + ln], src, reads=[on], writes=[("xfo", c, si)])
                else:
                    S.dma("sync", g.BCo[(c - 16) * 128:(c - 15) * 128, gc:gc + ln], src, reads=[on], writes=[("bco", c, si)])
            if c < 16:
                for si, (gc, ln) in enumerate(seg):
                    for t4 in range(ln // 512 if ln >= 512 else 1):
                        nsub = 4 if ln >= 512 else ln // 128
                        tp, tpn = trp[ntr % 2], "c_trp%d" % (ntr % 2)
                        ts_, tsn = tr_sb[ntr % 2], "c_tr%d" % (ntr % 2)
                        ntr += 1
                        for u in range(nsub):
                            a0 = segp[si] + t4 * 512 + u * 128
                            S.op("tensor", lambda e, tp=tp, u=u, a0=a0, o_=o_: e.transpose(tp[:, u, :], o_[:, a0:a0 + 128], g.ident_bf[:]),
                                 [on, "ident_bf"], [tpn])
                        act(ts_[:, 0:nsub, :], tp[:, 0:nsub, :], AF.Copy, [tpn], [tsn])
                        r0 = gc + t4 * 512
                        S.dma("sync", g.XTOK[r0:r0 + nsub * 128, c * 128:(c + 1) * 128].rearrange("(u p) v -> p u v", p=128), ts_[:, 0:nsub, :],
                              reads=[tsn], writes=[("xtok", c, si, t4)])
        S.barrier()

    with ExitStack() as st:
        sb = lambda n, s_, d=F32: g.sb(n, s_, d, st)
        hp = sb("k_hp", [64, 4])
        mk = sb("k_mk", [128, 2, 4, 512], BF16)
        S.dma("sync", hp[:], g.ssd_hp[:, :], writes=["k_hp"])
        S.op("gpsimd", lambda e: e.dma_start(out=mk[:], in_=g.ssd_mask[:, :, :, :]), [], ["k_mk"], dma=True)
        aneg = sb("k_aneg", [64, 1])
        act(aneg[:], hp[:, 1:2], AF.Exp, ["k_hp"], ["k_aneg"])
        V(lambda e: e.tensor_scalar(out=aneg[:], in0=aneg[:], scalar1=-1.0, scalar2=None, op0=ALU.mult), ["k_aneg"], ["k_aneg"])
        dt = sb("k_dt", [64, 4352]); psi = sb("k_psi", [64, 4352]); dta = sb("k_dta", [64, 4352])
        psiT = sb("k_psiT", [128, 34, 64]); dtT = sb("k_dtT", [128, 34, 64])
        sel = sb("k_sel", [64, 128])
        bcs = sb("k_bcs", [128, 8, 512])
        Bg = sb("k_B", [128, 4352], BF16); Cg = sb("k_C", [128, 4352], BF16)
        Xg = sb("k_X", [128, 34, 256], BF16)
        xdt = sb("k_xdt", [128, 34, 2, 256], BF16)
        Gs = [sb("k_G%d" % i, [128, 512], BF16) for i in range(2)]
        Gm = [sb("k_Gm%d" % i, [128, 512], BF16) for i in range(2)]
        Et = [sb("k_E%d" % i, [128, 512], BF16) for i in range(3)]
        Wt = [sb("k_W%d" % i, [128, 512], BF16) for i in range(3)]
        arg = sb("k_arg", [128, 512])
        yo = [sb("k_yo%d" % i, [64, 512], BF16) for i in range(2)]
        tps = g.ps("k_tps", [128, 4, 64], F32, st)
        bps = g.ps("k_bps", [128, 512], F32, st)
        gps = [g.ps("k_gps%d" % i, [128, 512], F32, st) for i in range(2)]
        yps = [g.ps("k_yps%d" % i, [64, 512], F32, st) for i in range(4)]
        ei = 0
        gi_ = 0
        yi = 0
        for sq_ in range(2):
            cc = slice(sq_ * 256, sq_ * 256 + 256)
            lc = slice(512 + sq_ * 4096, 512 + (sq_ + 1) * 4096)
            S.dma("sync", dt[:, 0:256], g.DTr[:, cc], writes=["k_dt"])
            S.dma("sync", dt[:, 256:4352], g.DTr[:, lc], writes=["k_dt"])
            act(dt[:], dt[:], AF.Exp, ["k_dt", "k_hp"], ["k_dt"], bias=hp[:, 0:1], scale=1.0)
            V(lambda e: e.tensor_scalar(out=dt[:], in0=dt[:], scalar1=1.0, scalar2=None, op0=ALU.add), ["k_dt"], ["k_dt"])
            act(dt[:], dt[:], AF.Ln, ["k_dt"], ["k_dt"])
            V(lambda e: e.tensor_scalar(out=dta[:], in0=dt[:], scalar1=aneg[:, 0:1], scalar2=None, op0=ALU.mult), ["k_dt", "k_aneg"], ["k_dta"])
            V(lambda e: e.tensor_tensor_scan(out=psi[0:32, :], data0=g.cst_t[0:32, 2, 0:1].to_broadcast([32, 4352]), data1=dta[0:32, :], initial=0.0, op0=ALU.mult, op1=ALU.add),
              ["k_dta", "cst"], ["k_psi"])
            V(lambda e: e.tensor_tensor_scan(out=mkap(psi[32:64, 255:256], -1, 256), data0=g.cst_t[32:64, 2, 0:1].to_broadcast([32, 256]),
                                             data1=mkap(dta[32:64, 255:256], -1, 256), initial=0.0, op0=ALU.mult, op1=ALU.add),
              ["k_dta", "cst"], ["k_psi"])
            V(lambda e: e.tensor_tensor_scan(out=mkap(psi[32:64, 4351:4352], -1, 4096), data0=g.cst_t[32:64, 2, 0:1].to_broadcast([32, 4096]),
                                             data1=mkap(dta[32:64, 4351:4352], -1, 4096), initial=0.0, op0=ALU.mult, op1=ALU.add),
              ["k_dta", "cst"], ["k_psi"])
            V(lambda e: e.tensor_scalar(out=psi[32:64, 256:4352], in0=psi[32:64, 256:4352], scalar1=psi[32:64, 0:1], scalar2=None, op0=ALU.add),
              ["k_psi"], ["k_psi"])
            for which, (src, dst, dn, scl) in enumerate(((psi, psiT, "k_psiT", -1.0), (dt, dtT, "k_dtT", 1.0))):
                srcn = "k_psi" if which == 0 else "k_dt"
                for k4 in range(9):
                    nk = 4 if k4 < 8 else 2
                    for u in range(nk):
                        kt = k4 * 4 + u
                        S.op("tensor", lambda e, u=u, kt=kt, src=src: e.transpose(tps[:, u, :], src[:, kt * 128:(kt + 1) * 128], g.cst_t[0:64, 0, 0:64]),
                             [srcn, "cst"], ["k_tps"])
                    act(dst[:, k4 * 4:k4 * 4 + nk, :], tps[:, 0:nk, :], AF.Copy, ["k_tps"], [dn], scale=scl)
            for grp in range(8):
                S.dma("sync", Bg[:, 0:256], g.BCo[grp * 128:(grp + 1) * 128, cc], writes=["k_B"])
                S.dma("sync", Bg[:, 256:4352], g.BCo[grp * 128:(grp + 1) * 128, lc], writes=["k_B"])
                S.dma("sync", Cg[:, 0:256], g.BCo[D + grp * 128:D + (grp + 1) * 128, cc], writes=["k_C"])
                S.dma("sync", Cg[:, 256:4352], g.BCo[D + grp * 128:D + (grp + 1) * 128, lc], writes=["k_C"])
                S.dma("sync", Xg[:, 0:2, :], g.XTOK[cc, grp * 256:(grp + 1) * 256].rearrange("(kt p) v -> p kt v", p=128), writes=["k_X"])
                S.dma("sync", Xg[:, 2:34, :], g.XTOK[lc, grp * 256:(grp + 1) * 256].rearrange("(kt p) v -> p kt v", p=128), writes=["k_X"])
                for d in range(2):
                    V(lambda e, d=d, grp=grp: e.tensor_tensor(
                        out=xdt[:, :, d, :].rearrange("p k (h v) -> p k h v", h=4), in0=Xg[:].rearrange("p k (h v) -> p k h v", h=4),
                        in1=bc(dtT[:, :, d * 32 + 4 * grp:d * 32 + 4 * grp + 4], [128, 34, 4, 64], 3), op=ALU.mult), ["k_X", "k_dtT"], ["k_xdt"])
                for qt in range(9):
                    if qt == 0:
                        q0, N, ktq = 0, 256, 0
                    else:
                        q0, N, ktq = 256 + (qt - 1) * 512, 512, 2 + 4 * (qt - 1)
                    nq = N // 128
                    for d in range(2):
                        for h in range(4):
                            dh = d * 32 + 4 * grp + h
                            V(lambda e, dh=dh: e.tensor_copy(out=sel[:], in_=g.cst_t[0:64, 0, dh:dh + 1].to_broadcast([64, 128])), ["cst"], ["k_sel"])
                            S.op("tensor", lambda e, q0=q0, N=N: e.matmul(bps[:, :N], lhsT=sel[:], rhs=psi[:, q0:q0 + N], start=True, stop=True),
                                 ["k_sel", "k_psi"], ["k_bps"])
                            act(bcs[:, d * 4 + h, :N], bps[:, :N], AF.Copy, ["k_bps"], [("k_bcs", d * 4 + h)])
                    work = []
                    for kt in range(34):
                        inq = ktq <= kt < ktq + nq
                        o = kt - ktq
                        if qt == 0:
                            if kt < 2:
                                work.append((kt, True, True, o))
                        else:
                            if kt < 2:
                                work.append((kt, True, True, None))
                            elif inq:
                                work.append((kt, True, True, o))
                            elif kt < ktq:
                                work.append((kt, True, False, None))
                            else:
                                work.append((kt, False, True, None))
                    nf = sum(1 for w_ in work if w_[1])
                    nbk = sum(1 for w_ in work if w_[2])
                    tot = nf + nbk
                    cnt = [0, 0, 0, 0]
                    ginfo = {}

                    def emitG(wi):
                        nonlocal gi_
                        kt = work[wi][0]
                        gp_, gpn = gps[gi_ % 2], "k_gps%d" % (gi_ % 2)
                        G_, Gn = Gs[gi_ % 2], "k_G%d" % (gi_ % 2)
                        gi_ += 1
                        S.op("tensor", lambda e, gp_=gp_, kt=kt, q0=q0, N=N: e.matmul(gp_[:, :N], lhsT=Bg[:, kt * 128:(kt + 1) * 128], rhs=Cg[:, q0:q0 + N],
                                                                                   start=True, stop=True), ["k_B", "k_C"], [gpn])
                        act(G_[:, :N], gp_[:, :N], AF.Copy, [gpn], [Gn])
                        ginfo[wi] = (G_, Gn)

                    def emitRest(wi):
                        nonlocal ei
                        (kt, fw, bw, o) = work[wi]
                        G_, Gn = ginfo.pop(wi)
                        for d in range(2):
                            if not (fw if d == 0 else bw):
                                continue
                            if o is not None:
                                S.op("gpsimd", lambda e, d=d, o=o, G_=G_, N=N: e.tensor_tensor(out=Gm[d][:, :N], in0=G_[:, :N], in1=mk[:, d, o, :N], op=ALU.mult),
                                     [Gn, "k_mk"], ["k_Gm%d" % d])
                                Gsrc, Gsn = Gm[d], "k_Gm%d" % d
                            else:
                                Gsrc, Gsn = G_, Gn
                            for h in range(4):
                                dh = d * 32 + 4 * grp + h
                                E_, En = Et[ei % 3], "k_E%d" % (ei % 3)
                                W_, Wn = Wt[ei % 3], "k_W%d" % (ei % 3)
                                ei += 1
                                if o is not None:
                                    V(lambda e, d=d, h=h, kt=kt, dh=dh, N=N: e.tensor_scalar(out=arg[:, :N], in0=bcs[:, d * 4 + h, :N], scalar1=psiT[:, kt, dh:dh + 1],
                                                                                    scalar2=0.0, op0=ALU.add, op1=ALU.min),
                                      [("k_bcs", d * 4 + h), "k_psiT"], ["k_arg"])
                                    act(E_[:, :N], arg[:, :N], AF.Exp, ["k_arg"], [En])
                                else:
                                    act(E_[:, :N], bcs[:, d * 4 + h, :N], AF.Exp, [("k_bcs", d * 4 + h), "k_psiT"], [En], bias=psiT[:, kt, dh:dh + 1], scale=1.0)
                                weng = "gpsimd" if (ei % 3 == 0) else "vector"
                                S.op(weng, lambda e, W_=W_, E_=E_, Gsrc=Gsrc, N=N: e.tensor_tensor(out=W_[:, :N], in0=Gsrc[:, :N], in1=E_[:, :N], op=ALU.mult),
                                     [Gsn, En], [Wn])
                                S.op("tensor", lambda e, h=h, kt=kt, d=d, W_=W_, N=N, st_=(cnt[h] == 0), sp_=(cnt[h] == tot - 1): e.matmul(
                                    yps[h][:, :N], lhsT=xdt[:, kt, d, h * 64:(h + 1) * 64], rhs=W_[:, :N], start=st_, stop=sp_), ["k_xdt", Wn], ["k_yps%d" % h])
                                cnt[h] += 1

                    for wi in range(len(work) + 1):
                        if wi < len(work):
                            emitG(wi)
                        if wi >= 1:
                            emitRest(wi - 1)
                    for h in range(4):
                        y_, yn = yo[yi % 2], "k_yo%d" % (yi % 2)
                        yi += 1
                        act(y_[:, :N], yps[h][:, :N], AF.Copy, ["k_yps%d" % h], [yn])
                        r0 = (4 * grp + h) * 64
                        c0 = (sq_ * 256) if qt == 0 else (512 + sq_ * 4096 + (qt - 1) * 512)
                        S.dma("sync", g.YT[r0:r0 + 64, c0:c0 + N], y_[:, :N], reads=[yn], writes=[("yt", sq_, grp, qt, h)])
        S.barrier()

    with ExitStack() as st:
        sb = lambda n, s_, d=F32: g.sb(n, s_, d, st)
        fv = sb("f_v", [128, 16, 2])
        S.dma("sync", fv[:], g.ssd_fv[:, :, :], writes=["f_v"])
        yb = [sb("f_y%d" % i, [128, 16, 512], BF16) for i in range(2)]
        xb = [sb("f_x%d" % i, [128, 16, 512], BF16) for i in range(2)]
        zb = [sb("f_z%d" % i, [128, 16, 512], BF16) for i in range(2)]
        gy = sb("f_gy", [128, 16, 512])
        gsq = sb("f_gsq", [128, 16, 512], BF16)
        rr = sb("f_rr", [128, 512])
        go = [sb("f_go%d" % i, [128, 16, 512], BF16) for i in range(2)]
        nps = [g.ps("f_nps%d" % i, [128, 512], F32, st) for i in range(2)]
        YTv = g.YT.rearrange("(c p) t -> p c t", p=128)
        XFv = g.XFo.rearrange("(c p) t -> p c t", p=128)
        SZv = g.SZ.rearrange("(c p) t -> p c t", p=128)
        G2v = g.GT2.rearrange("(c p) t -> p c t", p=128)
        tiles = list(range(1 if last else 0, NT))
        for jn, j in enumerate(tiles):
            q = jn % 2
            cols = slice(j * 512, (j + 1) * 512)
            S.dma("sync", yb[q][:], YTv[:, :, cols], writes=["f_y%d" % q])
            S.dma("sync", xb[q][:], XFv[:, :, cols], writes=["f_x%d" % q])
            S.dma("sync", zb[q][:], SZv[:, :, cols], writes=["f_z%d" % q])
            for c in range(16):
                V(lambda e, c=c, q=q: e.scalar_tensor_tensor(out=gy[:, c, :], in0=xb[q][:, c, :], scalar=fv[:, c, 0:1], in1=yb[q][:, c, :],
                                                             op0=ALU.mult, op1=ALU.add), ["f_x%d" % q, "f_y%d" % q, "f_v"], [("f_gy", c)])
                S.op("gpsimd", lambda e, c=c, q=q: e.tensor_tensor(out=gy[:, c, :], in0=gy[:, c, :], in1=zb[q][:, c, :], op=ALU.mult),
                     [("f_gy", c), "f_z%d" % q], [("f_gy", c)])
                act(gsq[:, c, :], gy[:, c, :], AF.Square, [("f_gy", c)], [("f_gsq", c)])
            for grp in range(8):
                np_, npn = nps[grp % 2], "f_nps%d" % (grp % 2)
                for u in range(2):
                    S.op("tensor", lambda e, grp=grp, u=u, np_=np_: e.matmul(np_[:], lhsT=g.ones_bf[:], rhs=gsq[:, 2 * grp + u, :], start=(u == 0), stop=(u == 1)),
                         [("f_gsq", 2 * grp + u), "ones_bf"], [npn])
                V(lambda e, np_=np_: e.tensor_scalar(out=rr[:], in0=np_[:], scalar1=1.0 / 256, scalar2=EPS, op0=ALU.mult, op1=ALU.add), [npn], ["f_rr"])
                act(rr[:], rr[:], AF.Sqrt, ["f_rr"], ["f_rr"])
                V(lambda e: e.reciprocal(out=rr[:], in_=rr[:]), ["f_rr"], ["f_rr"])
                for u in range(2):
                    c = 2 * grp + u
                    V(lambda e, c=c, q=q: e.scalar_tensor_tensor(out=go[q][:, c, :], in0=gy[:, c, :], scalar=fv[:, c, 1:2], in1=rr[:],
                                                                 op0=ALU.mult, op1=ALU.mult), [("f_gy", c), "f_rr", "f_v"], [("f_go%d" % q, c)])
            S.dma("sync", G2v[:, :, cols], go[q][:], reads=[("f_go%d" % q, c) for c in range(16)], writes=[("gt2", j)])
        S.barrier()
    out_proj_residual(g, l, g.ssd_ow, DI, last, "so")


def da_layer(g, l):
    import math
    nc, S = g.nc, g.S
    last = (l == 3)
    lam_init = 0.8 - 0.6 * math.exp(-0.3 * l)
    XTv = g.XT.rearrange("(c p) t -> p c t", p=128)
    GTv = g.GT.rearrange("(c p) t -> p c t", p=128)

    def V(fn, r, w, eng="vector"):
        S.op(eng, fn, r, w)

    def tt(out, a, b, op, r, w, eng="vector"):
        S.op(eng, lambda e: e.tensor_tensor(out=out, in0=a, in1=b, op=op), r, w)

    def act(out, in_, func, r, w, **kw):
        S.op("scalar", lambda e: e.activation(out=out, in_=in_, func=func, **kw), r, w)

    with ExitStack() as sl:
        dac = g.sb("a_dac", [128, 3, 128], F32, sl)
        Rm = g.sb("a_Rm", [128, 128], BF16, sl)
        bones = g.sb("a_bones", [128, 128], BF16, sl)
        gv = g.sb("a_gv", [128, 4], F32, sl)
        lamt = g.sb("a_lamt", [64, 4], F32, sl)
        lpr = g.sb("a_lpr", [64, 2], F32, sl)
        lbc = g.sb("a_lbc", [128, 2], F32, sl)
        nlam = g.sb("a_nlam", [128, 1], F32, sl)
        S.dma("sync", dac[:], g.dac[:, :, :], writes=["dac"])
        S.dma("sync", gv[:], g.dagv[:, :], writes=["gv"])
        S.dma("sync", lamt[:], g.dalam[:, :], writes=["lamt"])
        V(lambda e: e.tensor_copy(out=Rm[:], in_=dac[:, 0, :]), ["dac"], ["Rm"])
        V(lambda e: e.tensor_copy(out=bones[:], in_=dac[:, 1, :]), ["dac"], ["bones"])
        V(lambda e: e.tensor_scalar(out=gv[:, 0:1], in0=gv[:, 0:1], scalar1=0.125, scalar2=None, op0=ALU.mult), ["gv"], ["gv"])
        V(lambda e: e.tensor_scalar(out=gv[:, 2:3], in0=gv[:, 2:3], scalar1=1.0 - lam_init, scalar2=None, op0=ALU.mult), ["gv"], ["gv"])
        tt(lpr[:, 0:1], lamt[:, 0:1], lamt[:, 1:2], ALU.mult, ["lamt"], ["lpr"])
        tt(lpr[:, 1:2], lamt[:, 2:3], lamt[:, 3:4], ALU.mult, ["lamt"], ["lpr"])
        with ExitStack() as s0:
            lps = g.ps("a_lps", [128, 2], F32, s0)
            S.op("tensor", lambda e: e.matmul(lps[:], lhsT=g.cst_t[0:64, 2, :], rhs=lpr[:], start=True, stop=True), ["lpr", "cst"], ["lps"])
            act(lbc[:], lps[:], AF.Exp, ["lps"], ["lbc"])
            V(lambda e: e.scalar_tensor_tensor(out=nlam[:], in0=lbc[:, 1:2], scalar=-lam_init, in1=lbc[:, 0:1], op0=ALU.add, op1=ALU.subtract),
              ["lbc"], ["nlam"])
            S.barrier()

        with ExitStack() as st:
            sb = lambda n, s_, d=F32: g.sb(n, s_, d, st)
            w = sb("a_w", [128, NCH, 3 * D], BF16)
            S.op("gpsimd", lambda e: e.dma_start(out=w[:, :, 0:1536], in_=g.daqkv.rearrange("(kc p) n -> p kc n", p=128)[:, :, 0:1536]), [], ["a_w0"], dma=True)
            S.op("gpsimd", lambda e: e.dma_start(out=w[:, :, 1536:3072], in_=g.daqkv.rearrange("(kc p) n -> p kc n", p=128)[:, :, 1536:3072]), [], ["a_w1"], dma=True)
            xt = [sb("a_xt%d" % i, [128, NCH, 512]) for i in range(2)]
            sq = sb("a_sq", [128, NCH, 512], BF16)
            tmp = sb("a_tmp", [128, NCH, 512])
            hbf = sb("a_hbf", [128, NCH, 512], BF16)
            rstd = sb("a_rstd", [128, 512])
            rc = sb("a_rc", [128, 512]); rs = sb("a_rs", [128, 512])
            qsq = sb("a_qsq", [128, 512], BF16)
            qr = sb("a_qr", [128, 512]); qn = sb("a_qn", [128, 512]); qnb = sb("a_qnb", [128, 512], BF16)
            t1 = sb("a_t1", [128, 512]); t2 = sb("a_t2", [128, 512])
            qo = [sb("a_qo%d" % i, [128, 512], BF16) for i in range(2)]
            vt = [sb("a_vt%d" % i, [128, D], BF16) for i in range(2)]
            ssp = g.ps("a_ssp", [128, 512], F32, st)
            pq = [g.ps("a_pq%d" % i, [128, 512], F32, st) for i in range(2)]
            pss = g.ps("a_pss", [128, 512], F32, st)
            prq = g.ps("a_prq", [128, 512], F32, st)
            pv = [g.ps("a_pv%d" % i, [128, 512], F32, st) for i in range(2)]
            n = 0
            nv = 0
            for j in range(NT):
                x, xn = xt[j % 2], "a_xt%d" % (j % 2)
                S.dma("sync", x[:], XTv[:, :, j * 512:(j + 1) * 512], reads=[("XT", j)], writes=[xn])
                normmod(g, l, 1, x, xn, sq, ssp, rstd, tmp, None, hbf, stream_of_tile(j), tag="DA")
                if j >= 1:
                    pos0 = ((j - 1) % 8) * 512
                    S.dma("sync", rc[:], g.ropeC[:, pos0:pos0 + 512], writes=["rc"])
                    S.dma("sync", rs[:], g.ropeS[:, pos0:pos0 + 512], writes=["rs"])
                for which in range(2):
                    dst = g.QT if which == 0 else g.KT
                    for hd in range(8):
                        p_, pn = pq[n % 2], "a_pq%d" % (n % 2)
                        o_, on = qo[n % 2], "a_qo%d" % (n % 2)
                        n += 1
                        c0 = which * D + hd * 128
                        for kc in range(NCH):
                            S.op("tensor", lambda e, kc=kc, c0=c0, p_=p_: e.matmul(p_[:], lhsT=w[:, kc, c0:c0 + 128], rhs=hbf[:, kc, :], start=(kc == 0),
                                                                               stop=(kc == NCH - 1)), ["a_w0", "a_w1", "hbfDA"], [pn])
                        act(qsq[:], p_[:], AF.Square, [pn], ["qsq"])
                        S.op("tensor", lambda e: e.matmul(pss[:], lhsT=bones[:], rhs=qsq[:], start=True, stop=True), ["qsq", "bones"], ["pss"])
                        V(lambda e: e.tensor_scalar(out=qr[:], in0=pss[:], scalar1=1.0 / 64, scalar2=EPS, op0=ALU.mult, op1=ALU.add), ["pss"], ["qr"])
                        act(qr[:], qr[:], AF.Sqrt, ["qr"], ["qr"])
                        V(lambda e: e.reciprocal(out=qr[:], in_=qr[:]), ["qr"], ["qr"])
                        if j == 0:
                            V(lambda e, which=which, p_=p_, o_=o_: e.scalar_tensor_tensor(out=o_[:], in0=p_[:], scalar=gv[:, which:which + 1], in1=qr[:],
                                                                                      op0=ALU.mult, op1=ALU.mult), [pn, "qr", "gv"], [on])
                        else:
                            V(lambda e, which=which, p_=p_: e.scalar_tensor_tensor(out=qn[:], in0=p_[:], scalar=gv[:, which:which + 1], in1=qr[:],
                                                                               op0=ALU.mult, op1=ALU.mult), [pn, "qr", "gv"], ["qn"])
                            S.op("gpsimd", lambda e: e.tensor_copy(out=qnb[:], in_=qn[:]), ["qn"], ["qnb"])
                            S.op("tensor", lambda e: e.matmul(prq[:], lhsT=Rm[:], rhs=qnb[:], start=True, stop=True), ["qnb", "Rm"], ["prq"])
                            S.op("gpsimd", lambda e: e.tensor_tensor(out=t1[:], in0=qn[:], in1=rc[:], op=ALU.mult), ["qn", "rc"], ["t1"])
                            tt(t2[:], prq[:], rs[:], ALU.mult, ["prq", "rs"], ["t2"])
                            S.op("gpsimd", lambda e, o_=o_: e.tensor_tensor(out=o_[:], in0=t1[:], in1=t2[:], op=ALU.add), ["t1", "t2"], [on])
                        S.dma("sync", dst[hd * 128:(hd + 1) * 128, j * 512:(j + 1) * 512], o_[:], reads=[on], writes=[("qk", which, hd, j)])
                for s_ in range(4):
                    v_, vn = vt[nv % 2], "a_vt%d" % (nv % 2)
                    nv += 1
                    for half in range(2):
                        for kc in range(NCH):
                            S.op("tensor", lambda e, kc=kc, half=half, s_=s_: e.matmul(
                                pv[half][:], lhsT=hbf[:, kc, s_ * 128:(s_ + 1) * 128], rhs=w[:, kc, 2 * D + half * 512:2 * D + (half + 1) * 512],
                                start=(kc == 0), stop=(kc == NCH - 1)), ["a_w0", "a_w1", "hbfDA"], ["a_pv%d" % half])
                        act(v_[:, half * 512:(half + 1) * 512], pv[half][:], AF.Copy, ["a_pv%d" % half], [(vn, half)])
                    r0 = j * 512 + s_ * 128
                    S.dma("sync", g.VTOK[r0:r0 + 128, :], v_[:], reads=[(vn, 0), (vn, 1)], writes=[("vtok", j, s_)])
            S.barrier()

        with ExitStack() as st:
            sb = lambda n, s_, d=F32: g.sb(n, s_, d, st)
            Kh = [sb("b_K%d" % i, [128, 4352], BF16) for i in range(2)]
            Qh = [sb("b_Q%d" % i, [128, 4352], BF16) for i in range(2)]
            Vh = [sb("b_V%d" % i, [128, 34, 128], BF16) for i in range(2)]
            pt = [sb("b_pt%d" % i, [128, 512], BF16) for i in range(3)]
            rsum = sb("b_rsum", [128, 512])
            oc_ = [sb("b_oc%d" % i, [128, 512]) for i in range(2)]
            osq = sb("b_osq", [128, 512], BF16)
            orr = sb("b_orr", [128, 512])
            ao = [sb("b_ao%d" % i, [128, 512], BF16) for i in range(2)]
            sps = [g.ps("b_sps%d" % i, [128, 512], F32, st) for i in range(3)]
            ops_ = [g.ps("b_ops%d" % i, [128, 512], F32, st) for i in range(2)]
            sums = [g.ps("b_sum%d" % i, [128, 512], F32, st) for i in range(2)]
            oss = g.ps("b_oss", [128, 512], F32, st)
            hn = 0
            ei = 0
            an = 0
            for sq_ in range(2):
                cc = slice(sq_ * 256, sq_ * 256 + 256)
                lc = slice(512 + sq_ * 4096, 512 + (sq_ + 1) * 4096)
                for hd in range(8):
                    K_, Q_, V_ = Kh[hn % 2], Qh[hn % 2], Vh[hn % 2]
                    Kn, Qn, Vn = "b_K%d" % (hn % 2), "b_Q%d" % (hn % 2), "b_V%d" % (hn % 2)
                    hn += 1
                    hr = slice(hd * 128, (hd + 1) * 128)
                    S.dma("sync", K_[:, 0:256], g.KT[hr, cc], writes=[Kn])
                    S.dma("sync", K_[:, 256:4352], g.KT[hr, lc], writes=[Kn])
                    S.dma("sync", Q_[:, 0:256], g.QT[hr, cc], writes=[Qn])
                    S.dma("sync", Q_[:, 256:4352], g.QT[hr, lc], writes=[Qn])
                    S.dma("sync", V_[:, 0:2, :], g.VTOK[cc, hr].rearrange("(kt p) v -> p kt v", p=128), writes=[Vn])
                    S.dma("sync", V_[:, 2:34, :], g.VTOK[lc, hr].rearrange("(kt p) v -> p kt v", p=128), writes=[Vn])
                    for qt in range(9):
                        if qt == 0:
                            q0, N, nkt = 0, 256, 2
                        else:
                            q0, N, nkt = 256 + (qt - 1) * 512, 512, 34
                        for comp in range(2):
                            cr = slice(comp * 64, (comp + 1) * 64)
                            o_ps, o_n = ops_[comp], "b_ops%d" % comp
                            s_ps, s_n = sums[comp], "b_sum%d" % comp
                            pend = {}
                            for it in range(nkt + 2):
                                if it < nkt:
                                    kt = it
                                    sp_, spn = sps[ei % 3], "b_sps%d" % (ei % 3)
                                    p_, ptn = pt[ei % 3], "b_pt%d" % (ei % 3)
                                    ei += 1
                                    S.op("tensor", lambda e, sp_=sp_, K_=K_, Q_=Q_, cr=cr, kt=kt, q0=q0, N=N: e.matmul(
                                        sp_[:, :N], lhsT=K_[cr, kt * 128:(kt + 1) * 128], rhs=Q_[cr, q0:q0 + N], start=True, stop=True), [Kn, Qn], [spn])
                                    act(p_[:, :N], sp_[:, :N], AF.Exp, [spn], [ptn])
                                    pend[kt] = (p_, ptn)
                                if it >= 2:
                                    kt = it - 2
                                    p_, ptn = pend.pop(kt)
                                    S.op("tensor", lambda e, o_ps=o_ps, V_=V_, kt=kt, p_=p_, N=N, nkt=nkt: e.matmul(
                                        o_ps[:, :N], lhsT=V_[:, kt, :], rhs=p_[:, :N], start=(kt == 0), stop=(kt == nkt - 1)), [Vn, ptn], [o_n])
                                    S.op("tensor", lambda e, s_ps=s_ps, p_=p_, N=N, kt=kt, nkt=nkt: e.matmul(
                                        s_ps[:, :N], lhsT=g.ones_bf[:], rhs=p_[:, :N], start=(kt == 0), stop=(kt == nkt - 1)), ["ones_bf", ptn], [s_n])
                            V(lambda e, s_ps=s_ps, N=N: e.reciprocal(out=rsum[:, :N], in_=s_ps[:, :N]), [s_n], ["rsum"])
                            tt(oc_[comp][:, :N], o_ps[:, :N], rsum[:, :N], ALU.mult, [o_n, "rsum"], ["b_oc%d" % comp])
                        V(lambda e, N=N: e.scalar_tensor_tensor(out=oc_[0][:, :N], in0=oc_[1][:, :N], scalar=nlam[:, 0:1], in1=oc_[0][:, :N],
                                                                op0=ALU.mult, op1=ALU.add), ["b_oc0", "b_oc1", "nlam"], ["b_oc0"])
                        act(osq[:, :N], oc_[0][:, :N], AF.Square, ["b_oc0"], ["osq"])
                        S.op("tensor", lambda e, N=N: e.matmul(oss[:, :N], lhsT=g.ones_bf[:], rhs=osq[:, :N], start=True, stop=True), ["osq", "ones_bf"], ["oss"])
                        V(lambda e, N=N: e.tensor_scalar(out=orr[:, :N], in0=oss[:, :N], scalar1=1.0 / 128, scalar2=EPS, op0=ALU.mult, op1=ALU.add),
                          ["oss"], ["orr"])
                        act(orr[:, :N], orr[:, :N], AF.Sqrt, ["orr"], ["orr"])
                        V(lambda e, N=N: e.reciprocal(out=orr[:, :N], in_=orr[:, :N]), ["orr"], ["orr"])
                        a_, a_n = ao[an % 2], "b_ao%d" % (an % 2)
                        an += 1
                        V(lambda e, N=N, a_=a_: e.scalar_tensor_tensor(out=a_[:, :N], in0=oc_[0][:, :N], scalar=gv[:, 2:3], in1=orr[:, :N],
                                                                      op0=ALU.mult, op1=ALU.mult), ["b_oc0", "orr", "gv"], [a_n])
                        if qt == 0:
                            S.dma("sync", g.GT[hr, cc], a_[:, :256], reads=[a_n], writes=[("AT", sq_, hd, qt)])
                        else:
                            c0 = 512 + sq_ * 4096 + (qt - 1) * 512
                            S.dma("sync", g.GT[hr, c0:c0 + 512], a_[:, :512], reads=[a_n], writes=[("AT", sq_, hd, qt)])
            S.barrier()

        out_proj_residual(g, l, g.daow, D, last, "o")


def out_proj_residual(g, l, wdram, kdim, last, tag):
    nc, S = g.nc, g.S
    XTv = g.XT.rearrange("(c p) t -> p c t", p=128)
    nk = kdim // 128
    src = g.GT if kdim == D else g.GT2
    SRCv = src.rearrange("(c p) t -> p c t", p=128)
    with ExitStack() as st:
        sb = lambda n, s_, d=F32: g.sb(n, s_, d, st)
        w = sb(tag + "_w", [128, nk, D], BF16)
        S.op("gpsimd", lambda e: e.dma_start(out=w[:], in_=wdram.rearrange("(kc p) n -> p kc n", p=128)), [], [tag + "_w"], dma=True)
        xt = [sb(tag + "_xt%d" % i, [128, NCH, 512]) for i in range(2)]
        ai = [sb(tag + "_ai%d" % i, [128, nk, 512], BF16) for i in range(2)]
        po = [g.ps(tag + "_po%d" % i, [128, 512], F32, st) for i in range(2)]
        tiles = list(range(1 if last else 0, NT))
        for jn, j in enumerate(tiles):
            x, xn = xt[jn % 2], tag + "_xt%d" % (jn % 2)
            a, an = ai[jn % 2], tag + "_ai%d" % (jn % 2)
            stream = stream_of_tile(j)
            S.dma("sync", x[:], XTv[:, :, j * 512:(j + 1) * 512], reads=[("XT", j)], writes=[xn])
            S.dma("sync", a[:], SRCv[:, :, j * 512:(j + 1) * 512], reads=[], writes=[an])
            for oc in range(NCH):
                q = oc % 2
                for kc in range(nk):
                    S.op("tensor", lambda e, oc=oc, kc=kc, q=q, a=a: e.matmul(po[q][:], lhsT=w[:, kc, oc * 128:(oc + 1) * 128], rhs=a[:, kc, :],
                                                                             start=(kc == 0), stop=(kc == nk - 1)), [tag + "_w", an], [tag + "_po%d" % q])
                S.op("vector", lambda e, oc=oc, q=q, x=x, stream=stream: e.scalar_tensor_tensor(
                    out=x[:, oc, :], in0=po[q][:], scalar=g.mods[l][:, 2, oc, stream:stream + 1], in1=x[:, oc, :], op0=ALU.mult, op1=ALU.add),
                    [tag + "_po%d" % q, xn, ("mods", l)], [xn])
            S.dma("sync", XTv[:, :, j * 512:(j + 1) * 512], x[:], reads=[xn], writes=[("XT", j)])
        S.barrier()


def host_consts():
    cst = np.zeros((128, 6, 128), np.float32)
    cst[:, 0, :] = np.eye(128)
    cst[:, 1, :] = np.triu(np.ones((128, 128)), 1)
    cst[:, 2, :] = 1.0
    cst[:, 3, :] = np.arange(128)[None, :]
    cst[:, 4, :] = np.arange(128)[:, None]
    cblk = np.zeros((128, 128), np.float32)
    cblk[:, :] = (np.arange(128) * BLK)[None, :]
    cblk[:, 100:118] = (np.arange(18) * BLK)[None, :]
    return cst, cblk


def fm(v):
    v = np.asarray(v, np.float32)
    lead = v.shape[:-1]
    n = v.shape[-1] // 128
    return np.ascontiguousarray(np.swapaxes(v.reshape(lead + (n, 128)), -1, -2))


def make_s5_inputs(inp):
    out = {}
    are, aim, ldt = inp["s5_a_re"], inp["s5_a_im"], inp["s5_log_dt"]
    ldtb = np.broadcast_to(ldt[..., None], are.shape)
    prm = np.stack([are, aim, ldtb], -1).astype(np.float32)
    pB = prm.reshape(2, 2, 32, 2, 64, 3).transpose(0, 3, 4, 1, 2, 5).reshape(2, 128, 2, 32, 3)
    out["s5B"] = np.ascontiguousarray(pB)
    pA = prm.reshape(2, 2, 8, 8, 64, 3)
    pA = np.broadcast_to(pA[:, :, :, :, None], (2, 2, 8, 8, 16, 64, 3))
    out["s5A"] = np.ascontiguousarray(pA.transpose(0, 3, 4, 1, 2, 5, 6).reshape(2, 128, 2, 8, 64, 3))
    b = np.stack([inp["s5_b_re"], inp["s5_b_im"]], 2).astype(np.float32)
    c = np.stack([inp["s5_c_re"], inp["s5_c_im"]], 2).astype(np.float32)
    BzB = np.zeros((2, 2, 64, 2, 2, 32, 128), np.float32)
    CzB = np.zeros_like(BzB)
    for q in range(32):
        for gp in range(2):
            c0 = 32 * (q % 4) + 16 * gp
            BzB[:, gp, :, :, :, q, c0:c0 + 16] = b[:, :, :, 2 * q + gp].transpose(0, 3, 1, 2, 4)
            CzB[:, gp, :, :, :, q, c0:c0 + 16] = c[:, :, :, 2 * q + gp].transpose(0, 4, 1, 2, 3)
    out["s5BzB"] = BzB.reshape(2, 128, 2, 2, 32, 128)
    out["s5CzB"] = CzB.reshape(2, 128, 2, 2, 32, 128)
    BzA = np.zeros((2, 8, 16, 2, 2, 8, 8, 64), np.float32)
    for ct in range(8):
        for g8 in range(8):
            BzA[:, g8, :, :, :, ct, g8, :] = b[:, :, :, 8 * ct + g8].transpose(0, 4, 1, 2, 3)
    out["s5BzA"] = BzA.reshape(2, 128, 2, 2, 8, 8, 64)
    out["s5d"] = fm(inp["s5_d"])
    out["s5gw"] = np.ascontiguousarray(inp["s5_glu_w"], dtype=np.float32)
    out["s5gb"] = fm(inp["s5_glu_b"])
    s5c = np.zeros((128, 16 + NBK), np.float32)
    s5c[:, 0:9] = np.arange(9)[None, :]
    s5c[:, 16:] = np.arange(NBK)[None, :]
    out["s5c"] = s5c
    return out


def make_ssd_inputs(inp):
    out = {}
    out["ssd_inw"] = np.ascontiguousarray(inp["ssd_in_w"][0], dtype=np.float32)
    cw = np.zeros((128, 32, 6), np.float32)
    w = inp["ssd_conv_w"][0]
    b = inp["ssd_conv_b"][0]
    cw[:, :, 0:5] = w.T.reshape(32, 128, 5).transpose(1, 0, 2)
    cw[:, :, 5] = b.reshape(32, 128).T
    out["ssd_cw"] = cw
    hp = np.zeros((64, 4), np.float32)
    hp[:, 0] = inp["ssd_dt_bias"][0].reshape(64)
    hp[:, 1] = inp["ssd_a_log"][0].reshape(64)
    out["ssd_hp"] = hp
    m = np.zeros((128, 2, 4, 512), np.float32)
    s_ = np.arange(128)[:, None]
    t = np.arange(512)[None, :]
    for o in range(4):
        m[:, 0, o, :] = (t >= 128 * o + s_)
        m[:, 1, o, :] = (t <= 128 * o + s_)
    out["ssd_mask"] = m
    fv = np.zeros((128, 16, 2), np.float32)
    dsk = np.repeat(inp["ssd_d"][0], 64)
    fv[:, :, 0] = dsk.reshape(16, 128).T
    fv[:, :, 1] = inp["ssd_norm_g"][0].reshape(16, 128).T
    out["ssd_fv"] = fv
    out["ssd_ow"] = np.ascontiguousarray(inp["ssd_out_w"][0], dtype=np.float32)
    return out


def make_da_inputs(inp):
    out = {}
    out["daqkv"] = np.ascontiguousarray(inp["da_qkv_w"][0], dtype=np.float32)
    out["daow"] = np.ascontiguousarray(inp["da_out_w"][0], dtype=np.float32)
    gv = np.zeros((128, 4), np.float32)
    gv[:, 0] = np.tile(inp["da_q_g"][0], 2)
    gv[:, 1] = np.tile(inp["da_k_g"][0], 2)
    gv[:, 2] = inp["da_sub_g"][0]
    out["dagv"] = gv
    out["dalam"] = np.ascontiguousarray(inp["da_lam"][0].T, dtype=np.float32)
    dac = np.zeros((128, 3, 128), np.float32)
    for m in range(128):
        if (m % 32) < 16:
            dac[m + 16, 0, m] = -1.0
        else:
            dac[m - 16, 0, m] = 1.0
    dac[:, 1, :] = (np.arange(128)[:, None] // 64 == np.arange(128)[None, :] // 64)
    out["dac"] = dac
    t = np.arange(4096)
    row, col = (t // 64).astype(np.float64), (t % 64).astype(np.float64)
    inv = (np.float32(10000.0) ** (-np.arange(16, dtype=np.float32) / np.float32(16))).astype(np.float64)
    ang = np.zeros((128, 4096))
    for p in range(128):
        dd = p % 64
        f = dd % 16
        ang[p] = (row if dd < 32 else col) * inv[f]
    out["ropeC"] = np.cos(ang).astype(np.float32)
    out["ropeS"] = np.sin(ang).astype(np.float32)
    return out


def make_in_maps(inp, layers, core_ids=range(8), stub_moe=False):
    L = list(layers)
    cst, cblk = host_consts()
    shared = {
        "mod_w": np.ascontiguousarray(inp["mod_w"][L]),
        "mod_bT": fm(inp["mod_b"][L]),
        "n1T": fm(inp["norm1_g"][L]),
        "n2T": fm(inp["norm2_g"][L]),
        "rwT": np.ascontiguousarray(inp["moe_router_w"][L].reshape(len(L), NCH, 128, NEXP).transpose(0, 2, 1, 3)),
        "rb_bc": np.ascontiguousarray(np.broadcast_to(inp["moe_router_b"][L][:, None, :], (len(L), 128, NEXP))),
        "gu_w": np.ascontiguousarray(inp["moe_gu_w"][L].reshape(len(L), NEXP * D, 2 * D)),
        "dn_w": np.ascontiguousarray(inp["moe_dn_w"][L].reshape(len(L), NEXP * D, D)),
        "gu_bT": np.ascontiguousarray(inp["moe_gu_b"][L].reshape(len(L), NEXP, 16, 128).transpose(0, 1, 3, 2).reshape(len(L), NEXP * 128, 16)),
        "dn_b": np.ascontiguousarray(inp["moe_dn_b"][L]),
        "cst": cst, "cblk": cblk,
    }
    if any(l % 3 == 0 for l in L):
        shared.update(make_s5_inputs(inp))
    if any(l % 3 == 2 for l in L):
        shared.update(make_da_inputs(inp))
    if any(l % 3 == 1 for l in L):
        shared.update(make_ssd_inputs(inp))
    if stub_moe:
        shared["gu_w"] = shared["gu_w"][:, :8].copy()
        shared["dn_w"] = shared["dn_w"][:, :8].copy()
    maps = []
    for c in core_ids:
        b0, b1 = 2 * c, 2 * c + 1
        x = inp["x"]
        ctx = inp["ctx"]
        xT = np.concatenate([ctx[b0].T, ctx[b1].T, x[b0].T, x[b1].T], axis=1)
        cvec = np.stack([inp["c_ctx"], inp["c"][b0], inp["c"][b1]], axis=-1)
        cT = np.ascontiguousarray(cvec.reshape(NCH, 128, 3).transpose(1, 0, 2))
        m = dict(shared)
        m["xT0"] = np.ascontiguousarray(xT, dtype=np.float32)
        m["cT"] = cT.astype(np.float32)
        maps.append(m)
    return maps


def kernel(**inputs):
    inp = {k: np.asarray(v) for k, v in inputs.items()}
    layers = [0, 1, 2, 3]
    nc = build_program(layers, 4)
    maps = make_in_maps(inp, layers)
    res = run_bass_kernel_spmd(nc, maps, core_ids=list(range(8)))
    out = np.zeros((16, 4096, D), np.float32)
    for c in range(8):
        o = res.results[c]["outT"]
        out[2 * c] = o[:, :4096].T
        out[2 * c + 1] = o[:, 4096:].T
    return out
```

```python
import numpy as np
import ml_dtypes
import concourse.bass as bass
import concourse.mybir as mybir
from concourse.bass_utils import run_bass_kernel_spmd
from contextlib import ExitStack

F32 = mybir.dt.float32
BF16 = mybir.dt.bfloat16
I32 = mybir.dt.int32
AF = mybir.ActivationFunctionType
ALU = mybir.AluOpType
AX = mybir.AxisListType

ENGS = ["sync", "scalar", "vector", "gpsimd", "tensor"]
NSLOT = {"sync": 8, "scalar": 4, "vector": 2, "gpsimd": 8, "tensor": 2}
SAME_ENGINE_SYNC = {"sync": True, "scalar": True, "vector": True, "gpsimd": True, "tensor": False}
EPOCH = 24000

D = 1024
NCH = 8
TCORE = 8704
NT = 17
EPS = 1e-6
NEXP = 32
BLK = 512


class Op:
    __slots__ = ("eng", "fn", "dma", "deps", "sem", "val", "prev")

    def __init__(self, eng, fn, dma):
        self.eng = eng
        self.fn = fn
        self.dma = dma
        self.deps = ()
        self.sem = None
        self.val = 0
        self.prev = None


class Sched:
    def __init__(self, nc):
        self.nc = nc
        self.eng_ops = {e: [] for e in ENGS}
        self.last_w = {}
        self.readers = {}
        self.nsem = 0
        self.csem = {e: [self._newsem(), 0] for e in ENGS}
        self.dslot = {e: [[self._newsem(), 0, None] for _ in range(NSLOT[e])] for e in ENGS}
        self.dma_rr = {e: 0 for e in ENGS}
        self.last_op = {e: None for e in ENGS}

    def _newsem(self):
        self.nsem += 1
        return self.nsem - 1

    def op(self, eng, fn, reads=(), writes=(), dma=False):
        o = Op(eng, fn, dma)
        deps = {}
        for t in reads:
            w = self.last_w.get(t)
            if w is not None:
                deps[id(w)] = w
        for t in writes:
            w = self.last_w.get(t)
            if w is not None:
                deps[id(w)] = w
            for r in self.readers.get(t, ()):
                deps[id(r)] = r
        o.deps = tuple(deps.values())
        for t in writes:
            self.last_w[t] = o
            self.readers[t] = []
        wset = set(writes)
        for t in reads:
            if t not in wset:
                self.readers.setdefault(t, []).append(o)
        if dma:
            s = self.dma_rr[eng]
            self.dma_rr[eng] = (s + 1) % NSLOT[eng]
            slot = self.dslot[eng][s]
            o.prev = slot[2]
            if slot[1] + 16 > EPOCH:
                slot[0] = self._newsem()
                slot[1] = 0
            slot[1] += 16
            o.sem, o.val = slot[0], slot[1]
            slot[2] = o
        else:
            c = self.csem[eng]
            if c[1] + 1 > EPOCH:
                c[0] = self._newsem()
                c[1] = 0
            c[1] += 1
            o.sem, o.val = c[0], c[1]
            self.last_op[eng] = o
        self.eng_ops[eng].append(o)
        return o

    def dma(self, eng, out, in_, reads=(), writes=(), **kw):
        return self.op(eng, lambda e: e.dma_start(out=out, in_=in_, **kw), reads, writes, dma=True)

    def barrier(self):
        deps = []
        for e in ENGS:
            if self.last_op[e] is not None:
                deps.append(self.last_op[e])
            for slot in self.dslot[e]:
                if slot[2] is not None:
                    deps.append(slot[2])
        for e in ENGS:
            b = Op(e, None, False)
            b.deps = tuple(deps)
            self.eng_ops[e].append(b)

    def emit(self):
        nc = self.nc
        with ExitStack() as es:
            sems = [es.enter_context(nc.semaphore("s%d" % i)) for i in range(self.nsem)]
            block = es.enter_context(nc.Block())

            def run(engname, e):
                waited = {}

                def wait(d):
                    if waited.get(d.sem, 0) >= d.val:
                        return
                    waited[d.sem] = d.val
                    e.wait_ge(sems[d.sem], d.val)

                for o in self.eng_ops[engname]:
                    if o.prev is not None:
                        wait(o.prev)
                    for d in o.deps:
                        if d.eng == engname and not d.dma and not SAME_ENGINE_SYNC[engname] and o.fn is not None:
                            continue
                        wait(d)
                    if o.fn is None:
                        continue
                    ins = o.fn(e)
                    ins.then_inc(sems[o.sem], 16 if o.dma else 1)

            @block.sync
            def _(e):
                run("sync", e)

            @block.scalar
            def _(e):
                run("scalar", e)

            @block.vector
            def _(e):
                run("vector", e)

            @block.gpsimd
            def _(e):
                run("gpsimd", e)

            @block.tensor
            def _(e):
                run("tensor", e)


def bc(ap, shape, axis):
    return ap.unsqueeze(axis).to_broadcast(shape)


class Ctx:
    pass


def stream_of_tile(j):
    return 0 if j == 0 else (1 if j <= 8 else 2)


def build_program(layers, n_layers_weights, phases_per_layer=None, dbg=None):
    nc = bass.Bass("TRN2", target_bir_lowering=False)
    S = Sched(nc)
    g = Ctx()
    g.nc, g.S = nc, S
    g.lidx = {l: i for i, l in enumerate(layers)}
    NLW = n_layers_weights

    def din(name, shape, dt=F32):
        return nc.dram_tensor(name, shape, dt, kind="ExternalInput").ap()

    def dscr(name, shape, dt=F32):
        return nc.dram_tensor(name, shape, dt, kind="Internal").ap()

    g.xT0 = din("xT0", [D, TCORE])
    g.cT = din("cT", [128, NCH, 3])
    g.mod_w = din("mod_w", [NLW, D, 6 * D])
    g.mod_bT = din("mod_bT", [NLW, 128, 48])
    g.n1T = din("n1T", [NLW, 128, NCH])
    g.n2T = din("n2T", [NLW, 128, NCH])
    g.rwT = din("rwT", [NLW, 128, NCH, NEXP])
    g.rb_bc = din("rb_bc", [NLW, 128, NEXP])
    if phases_per_layer is not None and "moe" not in phases_per_layer:
        g.gu_w = din("gu_w", [NLW, 8, 2 * D])
        g.dn_w = din("dn_w", [NLW, 8, D])
    else:
        g.gu_w = din("gu_w", [NLW, NEXP * D, 2 * D])
        g.dn_w = din("dn_w", [NLW, NEXP * D, D])
    g.gu_bT = din("gu_bT", [NLW, NEXP * 128, 16])
    g.dn_b = din("dn_b", [NLW, NEXP, D])
    g.cst = din("cst", [128, 6, 128])
    g.cblk = din("cblk", [128, 128])
    g.outT = nc.dram_tensor("outT", [D, 8192], F32, kind="ExternalOutput").ap()

    ns5 = len([l for l in layers if l % 3 == 0])
    if ns5:
        g.s5B = din("s5B", [2, 128, 2, 32, 3])
        g.s5A = din("s5A", [2, 128, 2, 8, 64, 3])
        g.s5BzB = din("s5BzB", [2, 128, 2, 2, 32, 128])
        g.s5CzB = din("s5CzB", [2, 128, 2, 2, 32, 128])
        g.s5BzA = din("s5BzA", [2, 128, 2, 2, 8, 8, 64])
        g.s5d = din("s5d", [2, 128, NCH])
        g.s5gw = din("s5gw", [2, D, 2 * D])
        g.s5gb = din("s5gb", [2, 128, 16])
        g.s5c = din("s5c", [128, 16 + NBK])
    if any(l % 3 == 2 for l in layers):
        g.daqkv = din("daqkv", [D, 3 * D])
        g.daow = din("daow", [D, D])
        g.dagv = din("dagv", [128, 4])
        g.dalam = din("dalam", [64, 4])
        g.dac = din("dac", [128, 3, 128])
        g.ropeC = din("ropeC", [128, 4096])
        g.ropeS = din("ropeS", [128, 4096])
        g.QT = dscr("QT", [D, TCORE], BF16)
        g.KT = dscr("KT", [D, TCORE], BF16)
        g.VTOK = dscr("VTOK", [TCORE, D], BF16)
    if any(l % 3 == 1 for l in layers):
        g.ssd_inw = din("ssd_inw", [D, 6208])
        g.ssd_cw = din("ssd_cw", [128, 32, 6])
        g.ssd_hp = din("ssd_hp", [64, 4])
        g.ssd_mask = din("ssd_mask", [128, 2, 4, 512])
        g.ssd_fv = din("ssd_fv", [128, 16, 2])
        g.ssd_ow = din("ssd_ow", [2 * D, D])
        g.SZ = dscr("SZ", [2 * D, TCORE], BF16)
        g.XBCp = dscr("XBCp", [4 * D, TCORE], BF16)
        g.DTr = dscr("DTr", [64, TCORE], F32)
        g.XFo = dscr("XFo", [2 * D, TCORE], BF16)
        g.BCo = dscr("BCo", [2 * D, TCORE], BF16)
        g.XTOK = dscr("XTOK", [TCORE, 2 * D], BF16)
        g.YT = dscr("YT", [2 * D, TCORE], BF16)
    g.GT2 = dscr("GT2", [2 * D, TCORE], BF16)
    g.HT = dscr("HT", [D, TCORE], BF16)
    g.GT = dscr("GT", [D, TCORE], BF16)
    g.XT = dscr("XT", [D, TCORE])
    g.Htok = dscr("Htok", [TCORE, D], BF16)
    NSLOTS = (TCORE * 4 // BLK + NEXP) * BLK
    g.NB = NSLOTS // BLK
    g.Hs = dscr("Hs", [NSLOTS, D], BF16)
    g.Ys = dscr("Ys", [NSLOTS, D], F32)

    with ExitStack() as top:
        uid = [0]

        def sb(name, shape, dt=F32, stack=top):
            uid[0] += 1
            return stack.enter_context(nc.sbuf_tensor("%s_%d" % (name, uid[0]), shape, dt))

        def ps(name, shape, dt=F32, stack=top):
            uid[0] += 1
            return stack.enter_context(nc.psum_tensor("%s_%d" % (name, uid[0]), shape, dt))

        g.sb, g.ps = sb, ps
        g.cst_t = sb("cst_t", [128, 6, 128])
        g.cblk_t = sb("cblk_t", [128, 128])
        g.ident_bf = sb("ident_bf", [128, 128], BF16)
        g.ones_bf = sb("ones_bf", [128, 128], BF16)
        S.dma("sync", g.cst_t[:], g.cst[:, :, :], writes=["cst"])
        S.dma("sync", g.cblk_t[:], g.cblk[:, :], writes=["cblk"])
        S.op("vector", lambda e: e.tensor_copy(out=g.ident_bf[:], in_=g.cst_t[:, 0, :]), ["cst"], ["ident_bf"])
        S.op("vector", lambda e: e.tensor_copy(out=g.ones_bf[:], in_=g.cst_t[:, 2, :]), ["cst"], ["ones_bf"])
        g.ident_f = g.cst_t[:, 0, :]
        g.tri_f = g.cst_t[:, 1, :]
        g.ones_f = g.cst_t[:, 2, :]
        g.iota_row = g.cst_t[:, 3, :]
        g.iota_p = g.cst_t[:, 4, 0:1]
        g.mods = {}
        g.es1 = {}
        g.es2 = {}
        for l in layers:
            g.mods[l] = sb("mods%d" % l, [128, 6, NCH, 3])
            g.es1[l] = sb("es1_%d" % l, [128, NCH, 3])
            g.es2[l] = sb("es2_%d" % l, [128, NCH, 3])

        prologue(g, layers)
        with ExitStack() as st:
            buf = [sb("cpx%d" % i, [128, NCH, 512], F32, st) for i in range(2)]
            for j in range(NT):
                b = buf[j % 2]
                cols = slice(j * 512, (j + 1) * 512)
                S.dma("sync", b[:], g.xT0.rearrange("(c p) t -> p c t", p=128)[:, :, cols], writes=["cpx%d" % (j % 2)])
                S.dma("scalar", g.XT.rearrange("(c p) t -> p c t", p=128)[:, :, cols], b[:], reads=["cpx%d" % (j % 2)],
                      writes=[("XT", j)])
            S.barrier()

        for l in layers:
            ph = phases_per_layer or ("mix", "moe")
            if "mix" in ph:
                kind = l % 3
                if kind == 0:
                    s5_layer(g, l)
                elif kind == 1:
                    ssd_layer(g, l)
                else:
                    da_layer(g, l)
            if "moe" in ph:
                moe_layer(g, l, last=(l == 3))

        with ExitStack() as st:
            buf = [sb("cpo%d" % i, [128, NCH, 512], F32, st) for i in range(2)]
            outs = []
            for j in range(1, NT):
                b = buf[j % 2]
                cols = slice(j * 512, (j + 1) * 512)
                S.dma("sync", b[:], g.XT.rearrange("(c p) t -> p c t", p=128)[:, :, cols], reads=[("XT", j)],
                      writes=["cpo%d" % (j % 2)])
                S.dma("scalar", g.outT.rearrange("(c p) t -> p c t", p=128)[:, :, (j - 1) * 512:j * 512], b[:],
                      reads=["cpo%d" % (j % 2)], writes=[("out", j)])
            S.barrier()
        S.emit()
    return nc


def prologue(g, layers):
    nc, S = g.nc, g.S
    with ExitStack() as st:
        sb = lambda n, s, d=F32: g.sb(n, s, d, st)
        ct = sb("ct", [128, NCH, 3])
        sc = sb("sc", [128, NCH, 3])
        mw = [sb("mw%d" % i, [128, NCH, 1024]) for i in range(2)]
        mb = sb("mb", [128, 48])
        gn = sb("gn", [128, 2, NCH])
        pm = g.ps("pm", [128, 8, 4], F32, st)
        S.dma("sync", ct[:], g.cT[:, :, :], writes=["ct"])
        S.op("scalar", lambda e: e.activation(out=sc[:], in_=ct[:], func=AF.Silu), ["ct"], ["sc"])
        k = 0
        for li, l in enumerate(layers):
            S.dma("sync", mb[:], g.mod_bT[li, :, :], writes=["mb"])
            S.dma("sync", gn[:, 0, :], g.n1T[li, :, :], writes=["gn0"])
            S.dma("sync", gn[:, 1, :], g.n2T[li, :, :], writes=["gn1"])
            for part in range(6):
                w = mw[k % 2]
                wn = "mw%d" % (k % 2)
                k += 1
                S.dma("sync", w[:], g.mod_w[li].rearrange("(kc p) n -> p kc n", p=128)[:, :, part * 1024:(part + 1) * 1024],
                      writes=[wn])
                for oc in range(8):
                    for kc in range(NCH):
                        S.op("tensor", lambda e, w=w, oc=oc, kc=kc: e.matmul(
                            pm[:, oc, 0:3], lhsT=w[:, kc, oc * 128:(oc + 1) * 128], rhs=sc[:, kc, :],
                            start=(kc == 0), stop=(kc == NCH - 1)), [wn, "sc"], ["pm"])
                S.op("vector", lambda e, l=l, part=part: e.tensor_tensor(
                    out=g.mods[l][:, part, :, :], in0=pm[:, :, 0:3],
                    in1=bc(mb[:, part * 8:(part + 1) * 8], [128, 8, 3], 2), op=ALU.add), ["pm", "mb"], [("mods", l)])
            S.op("vector", lambda e, l=l: e.scalar_tensor_tensor(
                out=g.es1[l][:], in0=g.mods[l][:, 1, :, :], scalar=1.0, in1=bc(gn[:, 0, :], [128, NCH, 3], 2),
                op0=ALU.add, op1=ALU.mult), [("mods", l), "gn0"], [("es1", l)])
            S.op("vector", lambda e, l=l: e.scalar_tensor_tensor(
                out=g.es2[l][:], in0=g.mods[l][:, 4, :, :], scalar=1.0, in1=bc(gn[:, 1, :], [128, NCH, 3], 2),
                op0=ALU.add, op1=ALU.mult), [("mods", l), "gn1"], [("es2", l)])
        S.barrier()


def normmod(g, l, which, xt, xtn, sq, ssp, rstd, tmp, h32, hbf, stream, N=512, tag="", hbfn=None):
    S = g.S
    hbfn = hbfn or ("hbf" + tag)
    es = g.es1[l] if which == 1 else g.es2[l]
    esn = ("es1", l) if which == 1 else ("es2", l)
    shp = 0 if which == 1 else 3
    S.op("scalar", lambda e: e.activation(out=sq[:, :, :N], in_=xt[:, :, :N], func=AF.Square), [xtn], ["sq" + tag])
    for c in range(NCH):
        S.op("tensor", lambda e, c=c: e.matmul(ssp[:, :N], lhsT=g.ones_bf[:], rhs=sq[:, c, :N], start=(c == 0),
                                              stop=(c == NCH - 1)), ["sq" + tag, "ones_bf"], ["ssp" + tag])
    S.op("vector", lambda e: e.tensor_scalar(out=rstd[:, :N], in0=ssp[:, :N], scalar1=1.0 / D, scalar2=EPS, op0=ALU.mult,
                                             op1=ALU.add), ["ssp" + tag], ["rstd" + tag])
    S.op("scalar", lambda e: e.activation(out=rstd[:, :N], in_=rstd[:, :N], func=AF.Sqrt), ["rstd" + tag], ["rstd" + tag])
    S.op("vector", lambda e: e.reciprocal(out=rstd[:, :N], in_=rstd[:, :N]), ["rstd" + tag], ["rstd" + tag])
    for c in range(NCH):
        S.op("vector", lambda e, c=c: e.scalar_tensor_tensor(
            out=tmp[:, c, :N], in0=xt[:, c, :N], scalar=es[:, c, stream:stream + 1], in1=rstd[:, :N], op0=ALU.mult,
            op1=ALU.mult), [xtn, "rstd" + tag, esn], ["nm_tmp" + tag])
        if h32 is not None:
            S.op("scalar", lambda e, c=c: e.activation(out=h32[:, c, :N], in_=tmp[:, c, :N], func=AF.Identity,
                                                       bias=g.mods[l][:, shp, c, stream:stream + 1], scale=1.0),
                 ["nm_tmp" + tag, ("mods", l)], ["h32" + tag])
            if hbf is not None:
                S.op("gpsimd", lambda e, c=c: e.tensor_copy(out=hbf[:, c, :N], in_=h32[:, c, :N]), ["h32" + tag], [hbfn])
        else:
            S.op("scalar", lambda e, c=c: e.activation(out=hbf[:, c, :N], in_=tmp[:, c, :N], func=AF.Identity,
                                                       bias=g.mods[l][:, shp, c, stream:stream + 1], scale=1.0),
                 ["nm_tmp" + tag, ("mods", l)], [hbfn])


def moe_layer(g, l, last):
    nc, S = g.nc, g.S
    li = g.lidx[l]
    j0 = 1 if last else 0
    tiles = list(range(j0, NT))
    nsub = len(tiles) * 4
    NB = (nsub * 128 * 4) // BLK + NEXP
    XTv = g.XT.rearrange("(c p) t -> p c t", p=128)
    with ExitStack() as st:
        sb = lambda n, s, d=F32: g.sb(n, s, d, st)
        lg_all = sb("lg_all", [128, nsub, NEXP])
        rk_all = sb("rk_all", [128, nsub, NEXP])
        t8_all = sb("t8_all", [128, nsub, 8])
        gates = sb("gates", [128, nsub, 4])
        dest_f = sb("dest_f", [128, nsub, 4])
        dest_i = sb("dest_i", [128, nsub, 4], I32)
        macc = sb("macc", [128, NEXP])
        rw = sb("rw", [128, NCH, NEXP])
        rbb = sb("rbb", [128, NEXP])
        idxw = sb("idxw", [128, 128], I32)
        idxb = sb("idxb", [128, 128], I32)
        idxe = sb("idxe", [128, 128], I32)
        S.dma("sync", rw[:], g.rwT[li, :, :, :], writes=["rw"])
        S.dma("sync", rbb[:], g.rb_bc[li, :, :], writes=["rbb"])
        S.op("vector", lambda e: e.memset(macc[:], 0.0), [], ["macc"])

        with ExitStack() as sa:
            sba = lambda n, s, d=F32: g.sb(n, s, d, sa)
            xt = [sba("a_xt%d" % i, [128, NCH, 512]) for i in range(2)]
            sq = sba("a_sq", [128, NCH, 512], BF16)
            tmp = sba("a_tmp", [128, NCH, 512])
            h32 = sba("a_h32", [128, NCH, 512])
            hbf = sba("a_hbf", [128, NCH, 512], BF16)
            rstd = sba("a_rstd", [128, 512])
            hrow = [sba("a_hrow%d" % i, [128, D], BF16) for i in range(2)]
            lgt = sba("a_lgt", [128, NEXP])
            msk = sba("a_msk", [128, NEXP])
            nv0 = sba("a_nv0", [128, 1])
            ex = sba("a_ex", [128, 4])
            sme = sba("a_sme", [128, 1])
            ssp = g.ps("a_ssp", [128, 512], F32, sa)
            lgp = g.ps("a_lgp", [128, NEXP], F32, sa)
            rkp = g.ps("a_rkp", [128, NEXP], F32, sa)
            trp = [g.ps("a_trp%d" % i, [128, D], BF16, sa) for i in range(2)]
            si = 0
            for jn, j in enumerate(tiles):
                x = xt[jn % 2]
                xn = "a_xt%d" % (jn % 2)
                S.dma("sync", x[:], XTv[:, :, j * 512:(j + 1) * 512], reads=[("XT", j)], writes=[xn])
                normmod(g, l, 2, x, xn, sq, ssp, rstd, tmp, h32, hbf, stream_of_tile(j), tag="A")
                for s in range(4):
                    cs = slice(s * 128, (s + 1) * 128)
                    for kc in range(NCH):
                        S.op("tensor", lambda e, kc=kc, cs=cs: e.matmul(lgp[:], lhsT=h32[:, kc, cs], rhs=rw[:, kc, :],
                                                                      start=(kc == 0), stop=(kc == NCH - 1)),
                             ["h32A", "rw"], ["lgp"])
                    S.op("vector", lambda e, si=si: e.tensor_tensor(out=lg_all[:, si, :], in0=lgp[:], in1=rbb[:], op=ALU.add),
                         ["lgp", "rbb"], [("lg", si)])
                    S.op("vector", lambda e, si=si: e.max(out=t8_all[:, si, :], in_=lg_all[:, si, :]), [("lg", si)], [("t8", si)])
                    S.op("vector", lambda e, si=si: e.tensor_scalar(out=nv0[:], in0=t8_all[:, si, 0:1], scalar1=-1.0, scalar2=None,
                                                                    op0=ALU.mult), [("t8", si)], ["nv0"])
                    S.op("scalar", lambda e, si=si: e.activation(out=ex[:], in_=t8_all[:, si, 0:4], func=AF.Exp, bias=nv0[:, 0:1],
                                                                 scale=1.0, accum_out=sme[:, 0:1]), [("t8", si), "nv0"], ["ex", "sme"])
                    S.op("vector", lambda e: e.reciprocal(out=sme[:], in_=sme[:]), ["sme"], ["sme"])
                    S.op("vector", lambda e, si=si: e.tensor_scalar(out=gates[:, si, :], in0=ex[:], scalar1=sme[:, 0:1], scalar2=None,
                                                                    op0=ALU.mult), ["ex", "sme"], [("gates", si)])
                    S.op("vector", lambda e, si=si: e.tensor_scalar(out=msk[:], in0=lg_all[:, si, :], scalar1=t8_all[:, si, 3:4],
                                                                    scalar2=None, op0=ALU.is_ge), [("lg", si), ("t8", si)], ["msk"])
                    S.op("tensor", lambda e: e.matmul(rkp[:], lhsT=g.tri_f, rhs=msk[:], start=True, stop=False), ["msk", "cst"], ["rkp"])
                    S.op("tensor", lambda e: e.matmul(rkp[:], lhsT=g.ones_f, rhs=macc[:], start=False, stop=True), ["macc", "cst"], ["rkp"])
                    S.op("vector", lambda e, si=si: e.tensor_copy(out=rk_all[:, si, :], in_=rkp[:]), ["rkp"], [("rk", si)])
                    S.op("vector", lambda e: e.tensor_tensor(out=macc[:], in0=macc[:], in1=msk[:], op=ALU.add), ["macc", "msk"], ["macc"])
                    tp = trp[si % 2]
                    tpn = "a_trp%d" % (si % 2)
                    hr = hrow[si % 2]
                    hrn = "a_hrow%d" % (si % 2)
                    for c in range(NCH):
                        S.op("tensor", lambda e, c=c, cs=cs, tp=tp: e.transpose(tp[:, c * 128:(c + 1) * 128], hbf[:, c, cs], g.ident_bf[:]),
                             ["hbfA", "ident_bf"], [tpn])
                    S.op("scalar", lambda e, tp=tp, hr=hr: e.activation(out=hr[:], in_=tp[:], func=AF.Copy), [tpn], [hrn])
                    tok0 = j * 512 + s * 128
                    S.dma("sync", g.Htok[tok0:tok0 + 128, :], hr[:], reads=[hrn], writes=[("Htok", si)])
                    si += 1
            cnt = sba("a_cnt", [128, NEXP])
            cmp3 = sba("a_cmp3", [128, NEXP, 18])
            pad = sba("a_pad", [128, NEXP])
            pend = sba("a_pend", [128, NEXP])
            pstart = sba("a_pstart", [128, NEXP])
            onesr = sba("a_onesr", [128, NEXP])
            be = sba("a_be", [128, 128])
            bef = sba("a_bef", [128, 128])
            S.op("tensor", lambda e: e.matmul(rkp[:], lhsT=g.ones_f, rhs=macc[:], start=True, stop=True), ["macc", "cst"], ["rkp"])
            S.op("vector", lambda e: e.tensor_copy(out=cnt[:], in_=rkp[:]), ["rkp"], ["cnt"])
            S.op("vector", lambda e: e.tensor_tensor(out=cmp3[:], in0=bc(cnt[:], [128, NEXP, 18], 2),
                                                     in1=bc(g.cblk_t[:, 100:118], [128, NEXP, 18], 1), op=ALU.is_gt),
                 ["cnt", "cblk"], ["cmp3"])
            S.op("vector", lambda e: e.tensor_reduce(out=pad[:], in_=cmp3[:], axis=AX.X, op=ALU.add), ["cmp3"], ["pad"])
            S.op("vector", lambda e: e.tensor_scalar(out=pad[:], in0=pad[:], scalar1=float(BLK), scalar2=None, op0=ALU.mult), ["pad"], ["pad"])
            S.op("vector", lambda e: e.memset(onesr[:], 1.0), [], ["onesr"])
            S.op("vector", lambda e: e.tensor_tensor_scan(out=pend[:], data0=onesr[:], data1=pad[:], initial=0.0, op0=ALU.mult,
                                                          op1=ALU.add), ["onesr", "pad"], ["pend"])
            S.op("vector", lambda e: e.tensor_tensor(out=pstart[:], in0=pend[:], in1=pad[:], op=ALU.subtract), ["pend", "pad"], ["pstart"])
            S.op("vector", lambda e: e.memset(be[:], 0.0), [], ["be"])
            for ex_ in range(NEXP):
                S.op("vector", lambda e, ex_=ex_: e.scalar_tensor_tensor(out=be[:], in0=g.cblk_t[:], scalar=pend[:, ex_:ex_ + 1], in1=be[:],
                                                                        op0=ALU.is_ge, op1=ALU.add), ["cblk", "pend", "be"], ["be"])
            S.op("vector", lambda e: e.tensor_scalar(out=be[:], in0=be[:], scalar1=float(NEXP - 1), scalar2=None, op0=ALU.min), ["be"], ["be"])
            S.op("vector", lambda e: e.tensor_scalar(out=bef[:], in0=be[:], scalar1=float(D), scalar2=g.iota_p, op0=ALU.mult, op1=ALU.add),
                 ["be", "cst"], ["bef"])
            S.op("vector", lambda e: e.tensor_copy(out=idxw[:], in_=bef[:]), ["bef"], ["idxw"])
            S.op("vector", lambda e: e.tensor_scalar(out=bef[:], in0=be[:], scalar1=128.0, scalar2=g.iota_p, op0=ALU.mult, op1=ALU.add),
                 ["be", "cst"], ["bef"])
            S.op("vector", lambda e: e.tensor_copy(out=idxb[:], in_=bef[:]), ["bef"], ["idxb"])
            S.op("vector", lambda e: e.tensor_copy(out=idxe[:], in_=be[:]), ["be"], ["idxe"])
            dall = sba("a_dall", [128, nsub, NEXP])
            oh = sba("a_oh", [128, nsub, NEXP])
            allsi_lg = [("lg", i) for i in range(nsub)]
            allsi_rk = [("rk", i) for i in range(nsub)]
            allsi_t8 = [("t8", i) for i in range(nsub)]
            S.op("vector", lambda e: e.tensor_tensor(out=dall[:], in0=rk_all[:], in1=bc(pstart[:], [128, nsub, NEXP], 1), op=ALU.add),
                 allsi_rk + ["pstart"], ["dall"])
            for k in range(4):
                S.op("vector", lambda e, k=k: e.tensor_tensor(out=oh[:], in0=lg_all[:], in1=bc(t8_all[:, :, k], [128, nsub, NEXP], 2),
                                                              op=ALU.is_equal), allsi_lg + allsi_t8, ["oh"])
                S.op("vector", lambda e: e.tensor_tensor(out=oh[:], in0=oh[:], in1=dall[:], op=ALU.mult), ["oh", "dall"], ["oh"])
                S.op("vector", lambda e, k=k: e.tensor_reduce(out=dest_f[:, :, k], in_=oh[:], axis=AX.X, op=ALU.add), ["oh"], ["dest_f"])
            S.op("vector", lambda e: e.tensor_copy(out=dest_i[:], in_=dest_f[:]), ["dest_f"], ["dest_i"])
            for si in range(nsub):
                hr = hrow[si % 2]
                hrn = "a_hrow%d" % (si % 2)
                jn, s = divmod(si, 4)
                tok0 = tiles[jn] * 512 + s * 128
                S.dma("sync", hr[:], g.Htok[tok0:tok0 + 128, :], reads=[("Htok", si)], writes=[hrn])
                for k in range(4):
                    S.op("gpsimd", lambda e, hr=hr, si=si, k=k: e.indirect_dma_start(
                        out=g.Hs[:, :], out_offset=bass.IndirectOffsetOnAxis(ap=dest_i[:, si, k:k + 1], axis=0), in_=hr[:],
                        in_offset=None), [hrn, "dest_i"], [("Hs", si, k)], dma=True)
            S.barrier()

        with ExitStack() as sd:
            sbd = lambda n, s, d=F32: g.sb(n, s, d, sd)
            guw = [sbd("d_guw%d" % i, [128, NCH, 2 * D], BF16) for i in range(2)]
            dnw = [sbd("d_dnw%d" % i, [128, NCH, D], BF16) for i in range(2)]
            gub = [sbd("d_gub%d" % i, [128, 16]) for i in range(2)]
            dnb = [sbd("d_dnb%d" % i, [128, D]) for i in range(2)]
            hs = [sbd("d_hs%d" % i, [128, 4, D], BF16) for i in range(2)]
            hsT = sbd("d_hsT", [128, NCH, BLK], BF16)
            gt = [sbd("d_gt%d" % i, [128, BLK]) for i in range(2)]
            sg = [sbd("d_sg%d" % i, [128, BLK]) for i in range(2)]
            ut = [sbd("d_ut%d" % i, [128, BLK]) for i in range(2)]
            act = sbd("d_act", [128, NCH, BLK], BF16)
            yt = [sbd("d_yt%d" % i, [128, D]) for i in range(2)]
            trp = [g.ps("d_trp%d" % i, [128, BLK], BF16, sd) for i in range(2)]
            pg = [g.ps("d_pg%d" % i, [128, BLK], F32, sd) for i in range(2)]
            pu = [g.ps("d_pu%d" % i, [128, BLK], F32, sd) for i in range(2)]
            py = [g.ps("d_py%d" % i, [128, BLK], F32, sd) for i in range(2)]
            guv = g.gu_w.rearrange("l r c -> (l r) c")
            dnv = g.dn_w.rearrange("l r c -> (l r) c")
            gbv = g.gu_bT.rearrange("l r c -> (l r) c")
            dbv = g.dn_b.rearrange("l r c -> (l r) c")
            yk = [0]
            hsT2 = [hsT, sbd("d_hsT1", [128, NCH, BLK], BF16)]

            def names(b):
                p = b % 2
                return p, "d_guw%d" % p, "d_dnw%d" % p, "d_gub%d" % p, "d_dnb%d" % p, "d_hs%d" % p

            def emitLoad(b):
                p, wn, dn_, gbn, dbn, hsn = names(b)
                for kc in range(NCH):
                    S.op("gpsimd", lambda e, b=b, kc=kc, p=p: e.indirect_dma_start(
                        out=guw[p][:, kc, :], out_offset=None, in_=guv[:, :],
                        in_offset=bass.IndirectOffsetOnAxis(ap=idxw[:, b:b + 1], axis=0), element_offset=(li * NEXP * D + kc * 128) * 2 * D),
                        ["idxw"], [(wn, kc)], dma=True)
                for kc in range(NCH):
                    S.op("gpsimd", lambda e, b=b, kc=kc, p=p: e.indirect_dma_start(
                        out=dnw[p][:, kc, :], out_offset=None, in_=dnv[:, :],
                        in_offset=bass.IndirectOffsetOnAxis(ap=idxw[:, b:b + 1], axis=0), element_offset=(li * NEXP * D + kc * 128) * D),
                        ["idxw"], [(dn_, kc)], dma=True)
                S.op("gpsimd", lambda e, b=b, p=p: e.indirect_dma_start(
                    out=gub[p][:], out_offset=None, in_=gbv[:, :],
                    in_offset=bass.IndirectOffsetOnAxis(ap=idxb[:, b:b + 1], axis=0), element_offset=li * NEXP * 128 * 16), ["idxb"], [gbn], dma=True)
                S.op("gpsimd", lambda e, b=b, p=p: e.indirect_dma_start(
                    out=dnb[p][:], out_offset=None, in_=dbv[:, :],
                    in_offset=bass.IndirectOffsetOnAxis(ap=idxe[:, b:b + 1], axis=0), element_offset=li * NEXP * D), ["idxe"], [dbn], dma=True)
                S.dma("sync", hs[p][:], g.Hs[b * BLK:(b + 1) * BLK, :].rearrange("(s p) f -> p s f", p=128), reads=[], writes=[hsn])

            def emitT(b):
                p, wn, dn_, gbn, dbn, hsn = names(b)
                hT = hsT2[p]
                for kc in range(NCH):
                    tp = trp[kc % 2]
                    tpn = "d_trp%d" % (kc % 2)
                    for s_ in range(4):
                        S.op("tensor", lambda e, tp=tp, s_=s_, kc=kc, p=p: e.transpose(
                            tp[:, s_ * 128:(s_ + 1) * 128], hs[p][:, s_, kc * 128:(kc + 1) * 128], g.ident_bf[:]),
                            [hsn, "ident_bf"], [tpn])
                    S.op("scalar", lambda e, tp=tp, kc=kc, hT=hT: e.activation(out=hT[:, kc, :], in_=tp[:], func=AF.Copy), [tpn], [("hsT", p, kc)])

            def emitGU(b):
                p, wn, dn_, gbn, dbn, hsn = names(b)
                hT = hsT2[p]
                for fc in range(NCH):
                    q = fc % 2
                    for kc in range(NCH):
                        S.op("tensor", lambda e, fc=fc, kc=kc, p=p, q=q, hT=hT: e.matmul(
                            pg[q][:], lhsT=guw[p][:, kc, fc * 128:(fc + 1) * 128], rhs=hT[:, kc, :], start=(kc == 0),
                            stop=(kc == NCH - 1)), [(wn, kc), ("hsT", p, kc)], ["d_pg%d" % q])
                    for kc in range(NCH):
                        S.op("tensor", lambda e, fc=fc, kc=kc, p=p, q=q, hT=hT: e.matmul(
                            pu[q][:], lhsT=guw[p][:, kc, D + fc * 128:D + (fc + 1) * 128], rhs=hT[:, kc, :], start=(kc == 0),
                            stop=(kc == NCH - 1)), [(wn, kc), ("hsT", p, kc)], ["d_pu%d" % q])
                    gtn, sgn, utn = "d_gt%d" % q, "d_sg%d" % q, "d_ut%d" % q
                    S.op("vector", lambda e, fc=fc, p=p, q=q: e.tensor_scalar(
                        out=gt[q][:], in0=pg[q][:], scalar1=gub[p][:, fc:fc + 1], scalar2=7.0, op0=ALU.add, op1=ALU.min),
                        ["d_pg%d" % q, gbn], [gtn])
                    S.op("scalar", lambda e, q=q: e.activation(out=sg[q][:], in_=gt[q][:], func=AF.Sigmoid, scale=1.702), [gtn], [sgn])
                    S.op("vector", lambda e, fc=fc, p=p, q=q: e.tensor_scalar(
                        out=ut[q][:], in0=pu[q][:], scalar1=gub[p][:, 8 + fc:9 + fc], scalar2=7.0, op0=ALU.add, op1=ALU.min),
                        ["d_pu%d" % q, gbn], [utn])
                    S.op("vector", lambda e, q=q: e.tensor_scalar(out=ut[q][:], in0=ut[q][:], scalar1=-7.0, scalar2=1.0, op0=ALU.max,
                                                                  op1=ALU.add), [utn], [utn])
                    S.op("vector", lambda e, q=q: e.tensor_tensor(out=gt[q][:], in0=gt[q][:], in1=sg[q][:], op=ALU.mult), [gtn, sgn], [gtn])
                    S.op("vector", lambda e, fc=fc, q=q: e.tensor_tensor(out=act[:, fc, :], in0=gt[q][:], in1=ut[q][:], op=ALU.mult),
                         [gtn, utn], [("act", fc)])

            def emitDN(b):
                p, wn, dn_, gbn, dbn, hsn = names(b)
                for s_ in range(4):
                    y = yt[yk[0] % 2]
                    yn = "d_yt%d" % (yk[0] % 2)
                    yk[0] += 1
                    for half in range(2):
                        for fc in range(NCH):
                            S.op("tensor", lambda e, s_=s_, half=half, fc=fc, p=p: e.matmul(
                                py[half][:], lhsT=act[:, fc, s_ * 128:(s_ + 1) * 128], rhs=dnw[p][:, fc, half * 512:(half + 1) * 512],
                                start=(fc == 0), stop=(fc == NCH - 1)), [("act", fc), (dn_, fc)], ["d_py%d" % half])
                        S.op("vector", lambda e, half=half, y=y, p=p: e.tensor_tensor(
                            out=y[:, half * 512:(half + 1) * 512], in0=py[half][:], in1=dnb[p][:, half * 512:(half + 1) * 512], op=ALU.add),
                            ["d_py%d" % half, dbn], [(yn, half)])
                    r0 = b * BLK + s_ * 128
                    S.dma("sync", g.Ys[r0:r0 + 128, :], y[:], reads=[(yn, 0), (yn, 1)], writes=[("Ys", b, s_)])

            emitLoad(0)
            emitT(0)
            for b in range(NB):
                if b + 1 < NB:
                    emitLoad(b + 1)
                emitGU(b)
                if b + 1 < NB:
                    emitT(b + 1)
                emitDN(b)
            S.barrier()

        with ExitStack() as se:
            sbe = lambda n, s, d=F32: g.sb(n, s, d, se)
            xt = [sbe("e_xt%d" % i, [128, NCH, 512]) for i in range(2)]
            yk_t = [sbe("e_yk%d" % i, [128, 4, D]) for i in range(2)]
            dg = [sbe("e_dg%d" % i, [128, 4, 128]) for i in range(2)]
            fp = [g.ps("e_fp%d" % i, [128, NCH, 128], F32, se) for i in range(2)]
            si = 0
            for jn, j in enumerate(tiles):
                x = xt[jn % 2]
                xn = "e_xt%d" % (jn % 2)
                stream = stream_of_tile(j)
                S.dma("sync", x[:], XTv[:, :, j * 512:(j + 1) * 512], reads=[("XT", j)], writes=[xn])
                for s in range(4):
                    q = si % 2
                    for k in range(4):
                        S.op("gpsimd", lambda e, q=q, si=si, k=k: e.indirect_dma_start(
                            out=yk_t[q][:, k, :], out_offset=None, in_=g.Ys[:, :],
                            in_offset=bass.IndirectOffsetOnAxis(ap=dest_i[:, si, k:k + 1], axis=0)), ["dest_i"],
                            [("e_yk%d" % q, k)], dma=True)
                        S.op("vector", lambda e, q=q, si=si, k=k: e.tensor_scalar(
                            out=dg[q][:, k, :], in0=g.ident_f, scalar1=gates[:, si, k:k + 1], scalar2=None, op0=ALU.mult),
                            [("gates", si), "cst"], [("e_dg%d" % q, k)])
                    for c in range(NCH):
                        for k in range(4):
                            S.op("tensor", lambda e, q=q, c=c, k=k: e.matmul(
                                fp[q][:, c, :], lhsT=yk_t[q][:, k, c * 128:(c + 1) * 128], rhs=dg[q][:, k, :], start=(k == 0), stop=(k == 3)),
                                [("e_yk%d" % q, k), ("e_dg%d" % q, k)], ["e_fp%d" % q])
                    for c in range(NCH):
                        S.op("vector", lambda e, q=q, c=c, x=x, s=s, stream=stream: e.scalar_tensor_tensor(
                            out=x[:, c, s * 128:(s + 1) * 128], in0=fp[q][:, c, :], scalar=g.mods[l][:, 5, c, stream:stream + 1],
                            in1=x[:, c, s * 128:(s + 1) * 128], op0=ALU.mult, op1=ALU.add), ["e_fp%d" % q, xn, ("mods", l)], [xn])
                    si += 1
                S.dma("scalar", XTv[:, :, j * 512:(j + 1) * 512], x[:], reads=[xn], writes=[("XT", j)])
            S.barrier()


TWO_PI = 6.283185307179586
NBK = 544


def mkap(base, step, cnt):
    return bass.AP(tensor=base.tensor, offset=base.offset, ap=[list(base.ap[0]), [step, cnt]])


def s5_layer(g, l):
    nc, S = g.nc, g.S
    jj = l // 3
    last = (l == 3)
    XTv = g.XT.rearrange("(c p) t -> p c t", p=128)
    HTv = g.HT.rearrange("(c p) t -> p c t", p=128)

    def V(fn, r, w, eng="vector"):
        S.op(eng, fn, r, w)

    def tt(out, a, b, op, r, w, eng="vector"):
        S.op(eng, lambda e: e.tensor_tensor(out=out, in0=a, in1=b, op=op), r, w)

    def ts(out, a, s1, s2, op0, op1, r, w):
        if s2 is None:
            S.op("vector", lambda e: e.tensor_scalar(out=out, in0=a, scalar1=s1, scalar2=None, op0=op0), r, w)
        else:
            S.op("vector", lambda e: e.tensor_scalar(out=out, in0=a, scalar1=s1, scalar2=s2, op0=op0, op1=op1), r, w)

    def act(out, in_, func, r, w, **kw):
        S.op("scalar", lambda e: e.activation(out=out, in_=in_, func=func, **kw), r, w)

    def sinred(y, out, ki, kf, fr, quarter, rd, tagw, names=("sr_ki", "sr_kf", "sr_fr")):
        nki, nkf, nfr = names
        if quarter:
            ts(fr, y, 0.25, None, ALU.add, None, rd, [nfr])
            src, rsrc = fr, [nfr]
        else:
            src, rsrc = y, rd
        V(lambda e: e.tensor_copy(out=ki, in_=src), rsrc, [nki])
        V(lambda e: e.tensor_copy(out=kf, in_=ki), [nki], [nkf])
        tt(fr, src, kf, ALU.subtract, rsrc + [nkf], [nfr])
        ts(kf, fr, 0.5, None, ALU.is_gt, None, [nfr], [nkf])
        tt(fr, fr, kf, ALU.subtract, [nfr, nkf], [nfr])
        act(out, fr, AF.Sin, [nfr], tagw, scale=TWO_PI)

    def discretize(tag, pv, Fn, Pre, Pim, co, fr8, svals, st2):
        sb2 = lambda n, s_, d=F32: g.sb("z" + tag + n, s_, d, st2)
        dt = sb2("dt", [128, Fn]); dta = sb2("dta", [128, Fn]); ang = sb2("ang", [128, Fn])
        eS = sb2("eS", [128, Fn, 9]); yS = sb2("yS", [128, Fn, 9])
        ki = sb2("ki", [128, Fn, 9], I32); kf = sb2("kf", [128, Fn, 9]); fr = sb2("fr", [128, Fn, 9])
        t1 = sb2("t1", [128, Fn]); t2 = sb2("t2", [128, Fn]); den = sb2("den", [128, Fn])
        pn = "s5prm" + tag
        act(dt[:], pv[:, :, 2], AF.Exp, [pn], ["dz_dt"])
        tt(dta[:], dt[:], pv[:, :, 0], ALU.mult, ["dz_dt", pn], ["dz_dta"])
        tt(ang[:], dt[:], pv[:, :, 1], ALU.mult, ["dz_dt", pn], ["dz_ang"])
        tt(eS[:], bc(dta[:], [128, Fn, 9], 2), bc(svals[:], [128, Fn, 9], 1), ALU.mult, ["dz_dta", "svals"], ["dz_eS"])
        act(eS[:], eS[:], AF.Exp, ["dz_eS"], ["dz_eS"])
        tt(yS[:], bc(ang[:], [128, Fn, 9], 2), bc(svals[:], [128, Fn, 9], 1), ALU.mult, ["dz_ang", "svals"], ["dz_yS"])
        ts(yS[:], yS[:], 1.0 / TWO_PI, None, ALU.mult, None, ["dz_yS"], ["dz_yS"])
        sinred(yS[:], Pim, ki[:], kf[:], fr[:], False, ["dz_yS"], [tag + "P"])
        V(lambda e: e.tensor_copy(out=fr8, in_=fr[:, :, 8]), ["sr_fr"], [tag + "fr8"])
        sinred(yS[:], Pre, ki[:], kf[:], fr[:], True, ["dz_yS"], [tag + "P"])
        tt(Pre, Pre, eS[:], ALU.mult, ["dz_eS", tag + "P"], [tag + "P"])
        tt(Pim, Pim, eS[:], ALU.mult, ["dz_eS", tag + "P"], [tag + "P"])
        Pre1, Pim1 = Pre[:, :, 1], Pim[:, :, 1]
        are, aim = pv[:, :, 0], pv[:, :, 1]
        tt(den[:], are, are, ALU.mult, [pn], ["dz_den"])
        tt(t1[:], aim, aim, ALU.mult, [pn], ["dz_t1"])
        tt(den[:], den[:], t1[:], ALU.add, ["dz_den", "dz_t1"], ["dz_den"])
        V(lambda e: e.reciprocal(out=den[:], in_=den[:]), ["dz_den"], ["dz_den"])
        ts(t1[:], Pre1, -1.0, None, ALU.add, None, [tag + "P"], ["dz_t1"])
        tt(t2[:], t1[:], are, ALU.mult, ["dz_t1", pn], ["dz_t2"])
        tt(dt[:], Pim1, aim, ALU.mult, [tag + "P", pn], ["dz_dt"])
        tt(t2[:], t2[:], dt[:], ALU.add, ["dz_t2", "dz_dt"], ["dz_t2"])
        tt(co[:, 0, :], t2[:], den[:], ALU.mult, ["dz_t2", "dz_den"], [tag + "co"])
        tt(t2[:], Pim1, are, ALU.mult, [tag + "P", pn], ["dz_t2"])
        tt(dt[:], t1[:], aim, ALU.mult, ["dz_t1", pn], ["dz_dt"])
        tt(t2[:], t2[:], dt[:], ALU.subtract, ["dz_t2", "dz_dt"], ["dz_t2"])
        tt(co[:, 1, :], t2[:], den[:], ALU.mult, ["dz_t2", "dz_den"], [tag + "co"])

    with ExitStack() as st:
        sb = lambda n, s_, d=F32: g.sb(n, s_, d, st)
        xt = [sb("n_xt%d" % i, [128, NCH, 512]) for i in range(2)]
        sq = sb("n_sq", [128, NCH, 512], BF16)
        tmp = sb("n_tmp", [128, NCH, 512])
        hbf = [sb("n_hbf%d" % i, [128, NCH, 512], BF16) for i in range(2)]
        rstd = sb("n_rstd", [128, 512])
        ssp = g.ps("n_ssp", [128, 512], F32, st)
        for j in range(NT):
            x, xn = xt[j % 2], "n_xt%d" % (j % 2)
            S.dma("sync", x[:], XTv[:, :, j * 512:(j + 1) * 512], reads=[("XT", j)], writes=[xn])
            normmod(g, l, 1, x, xn, sq, ssp, rstd, tmp, None, hbf[j % 2], stream_of_tile(j), tag="N", hbfn="hbfN%d" % (j % 2))
            S.dma("sync", HTv[:, :, j * 512:(j + 1) * 512], hbf[j % 2][:], reads=["hbfN%d" % (j % 2)], writes=[("HT", j)])
        S.barrier()

    with ExitStack() as st:
        sb = lambda n, s_, d=F32: g.sb(n, s_, d, st)
        svals = sb("s_svals", [128, 9])
        irow = sb("s_irow", [128, NBK])
        S.dma("sync", svals[:], g.s5c[:, 0:9], writes=["svals"])
        S.dma("sync", irow[:], g.s5c[:, 16:16 + NBK], writes=["irow"])
        PB_re = sb("s_PBre", [128, 64, 9]); PB_im = sb("s_PBim", [128, 64, 9]); coB = sb("s_coB", [128, 2, 64]); fr8B = sb("s_fr8B", [128, 64])
        with ExitStack() as st2:
            prmB = g.sb("s_prmB", [128, 64, 3], F32, st2)
            S.dma("sync", prmB[:], g.s5B[jj].rearrange("p d q k -> p (d q) k"), writes=["s5prmB"])
            discretize("B", prmB, 64, PB_re[:], PB_im[:], coB, fr8B[:], svals, st2)
            S.barrier()
        PBr = PB_re[:].rearrange("p (d q) k -> p d q k", d=2); PBi = PB_im[:].rearrange("p (d q) k -> p d q k", d=2)
        coBv = coB[:].rearrange("p r (d q) -> p r d q", d=2)
        fr8Bv = fr8B[:].rearrange("p (d q) -> p d q", d=2)
        rho = sb("s_rho", [128, 2, 32])
        t_r = sb("s_tr", [128, 2, 32]); t_i = sb("s_ti", [128, 2, 32])
        tt(t_r[:], PBr[:, :, :, 8], PBr[:, :, :, 8], ALU.mult, ["BP"], ["s_tr"])
        tt(t_i[:], PBi[:, :, :, 8], PBi[:, :, :, 8], ALU.mult, ["BP"], ["s_ti"])
        tt(t_r[:], t_r[:], t_i[:], ALU.add, ["s_tr", "s_ti"], ["s_tr"])
        act(rho[:], t_r[:], AF.Sqrt, ["s_tr"], ["rho"])
        dsk = sb("s_dsk", [128, NCH])
        S.dma("sync", dsk[:], g.s5d[jj, :, :], writes=["dsk"])

        Vw = sb("s_Vw", [128, 2, 8, 2, 512], BF16)
        Y1 = sb("s_Y1", [128, 2, 8, 2, 4, 128], BF16)
        Kw = sb("s_Kw", [128, 15, 128], BF16)
        XS = [sb("s_XS%d" % d, [128, 2, 4, NBK + 1], BF16) for d in range(2)]
        for d in range(2):
            V(lambda e, d=d: e.memset(XS[d][:], 0.0), [], ["XS%d" % d])
        kps = g.ps("s_kps", [128, 128], F32, st)
        vps_c = [g.ps("s_vpc%d" % i, [128, 32], F32, st) for i in range(2)]
        vps_l = [g.ps("s_vpl%d" % i, [128, 512], F32, st) for i in range(2)]

        for ct in range(NCH):
            with ExitStack() as spa:
                PA_re = g.sb("p_PAre", [128, 128, 9], F32, spa); PA_im = g.sb("p_PAim", [128, 128, 9], F32, spa)
                coA = g.sb("p_coA", [128, 2, 128], F32, spa); fr8A = g.sb("p_fr8A", [128, 128], F32, spa)
                with ExitStack() as st2:
                    prmA4 = g.sb("p_prmA", [128, 2, 64, 3], F32, st2)
                    S.dma("sync", prmA4[:], g.s5A[jj][:, :, ct, :, :], writes=["s5prmA"])
                    prmA = prmA4[:].rearrange("p d q k -> p (d q) k")
                    discretize("A", prmA, 128, PA_re[:], PA_im[:], coA, fr8A[:], svals, st2)
                    S.barrier()
                PAr = PA_re[:].rearrange("p (d q) k -> p d q k", d=2); PAi = PA_im[:].rearrange("p (d q) k -> p d q k", d=2)
                coAv = coA[:].rearrange("p r (d q) -> p r d q", d=2)
                with ExitStack() as sp:
                    sbp = lambda n, s_, d=F32: g.sb(n, s_, d, sp)
                    BzB = sbp("p_BzB", [128, 2, 2, 4, 128]); CzB = sbp("p_CzB", [128, 2, 2, 4, 128]); bbz = sbp("p_bbz", [128, 2, 2, 4, 128])
                    BzA = sbp("p_BzA", [128, 2, 2, 8, 64]); bbA = sbp("p_bbA", [128, 2, 2, 8, 64])
                    u1 = sbp("p_u1", [128, 2, 4, 128]); u2 = sbp("p_u2", [128, 2, 4, 128])
                    cp = sbp("p_cp", [128, 2, 4, 128])
                    w1 = sbp("p_w1", [128, 8, 64]); w2 = sbp("p_w2", [128, 8, 64])
                    dgd = sbp("p_dgd", [128, 128])
                    S.dma("sync", BzB[:], g.s5BzB[jj][:, :, :, 4 * ct:4 * ct + 4, :], writes=["BzB"])
                    S.dma("sync", CzB[:], g.s5CzB[jj][:, :, :, 4 * ct:4 * ct + 4, :], writes=["CzB"])
                    S.dma("sync", BzA[:], g.s5BzA[jj][:, :, :, ct, :, :], writes=["BzA"])
                    co_re = bc(coBv[:, 0, :, 4 * ct:4 * ct + 4], [128, 2, 4, 128], 3)
                    co_im = bc(coBv[:, 1, :, 4 * ct:4 * ct + 4], [128, 2, 4, 128], 3)
                    tt(u1[:], BzB[:, :, 0], co_re, ALU.mult, ["BzB", "Bco"], ["u1"])
                    tt(u2[:], BzB[:, :, 1], co_im, ALU.mult, ["BzB", "Bco"], ["u2"])
                    tt(bbz[:, :, 0], u1[:], u2[:], ALU.subtract, ["u1", "u2"], ["bbz"])
                    tt(u1[:], BzB[:, :, 1], co_re, ALU.mult, ["BzB", "Bco"], ["u1"])
                    tt(u2[:], BzB[:, :, 0], co_im, ALU.mult, ["BzB", "Bco"], ["u2"])
                    tt(bbz[:, :, 1], u1[:], u2[:], ALU.add, ["u1", "u2"], ["bbz"])
                    for d in range(2):
                        cr = bc(coAv[:, 0, d, :], [128, 8, 64], 1)
                        ci = bc(coAv[:, 1, d, :], [128, 8, 64], 1)
                        tt(w1[:], BzA[:, d, 0], cr, ALU.mult, ["BzA", "Aco"], ["w1"])
                        tt(w2[:], BzA[:, d, 1], ci, ALU.mult, ["BzA", "Aco"], ["w2"])
                        tt(bbA[:, d, 0], w1[:], w2[:], ALU.subtract, ["w1", "w2"], ["bbA"])
                        tt(w1[:], BzA[:, d, 1], cr, ALU.mult, ["BzA", "Aco"], ["w1"])
                        tt(w2[:], BzA[:, d, 0], ci, ALU.mult, ["BzA", "Aco"], ["w2"])
                        tt(bbA[:, d, 1], w1[:], w2[:], ALU.add, ["w1", "w2"], ["bbA"])
                    for d in range(2):
                        for s in range(8):
                            pw = 7 - s if d == 0 else s
                            pr = bc(PAr[:, d, :, pw], [128, 8, 64], 1)
                            pi = bc(PAi[:, d, :, pw], [128, 8, 64], 1)
                            o_re = Vw[:, d, s, 0, :].rearrange("p (a b) -> p a b", a=8)
                            o_im = Vw[:, d, s, 1, :].rearrange("p (a b) -> p a b", a=8)
                            tt(w1[:], bbA[:, d, 0], pr, ALU.mult, ["bbA", "AP"], ["w1"])
                            tt(w2[:], bbA[:, d, 1], pi, ALU.mult, ["bbA", "AP"], ["w2"])
                            tt(o_re, w1[:], w2[:], ALU.subtract, ["w1", "w2"], ["Vw"])
                            tt(w1[:], bbA[:, d, 1], pr, ALU.mult, ["bbA", "AP"], ["w1"])
                            tt(w2[:], bbA[:, d, 0], pi, ALU.mult, ["bbA", "AP"], ["w2"])
                            tt(o_im, w1[:], w2[:], ALU.add, ["w1", "w2"], ["Vw"])
                    ts(dgd[:], g.ident_f, dsk[:, ct:ct + 1], None, ALU.mult, None, ["cst", "dsk"], ["dgd"])
                    for k in range(9):
                        for d in range(2):
                            pr = bc(PBr[:, d, 4 * ct:4 * ct + 4, k], [128, 4, 128], 2)
                            pi = bc(PBi[:, d, 4 * ct:4 * ct + 4, k], [128, 4, 128], 2)
                            tt(u1[:, 0], CzB[:, d, 0], pr, ALU.mult, ["CzB", "BP"], ["u1"])
                            tt(u2[:, 0], CzB[:, d, 1], pi, ALU.mult, ["CzB", "BP"], ["u2"])
                            tt(cp[:, 0], u1[:, 0], u2[:, 0], ALU.subtract, ["u1", "u2"], ["cp"])
                            tt(u1[:, 0], CzB[:, d, 0], pi, ALU.mult, ["CzB", "BP"], ["u1"])
                            tt(u2[:, 0], CzB[:, d, 1], pr, ALU.mult, ["CzB", "BP"], ["u2"])
                            V(lambda e: e.scalar_tensor_tensor(out=cp[:, 1], in0=u1[:, 0], scalar=-1.0, in1=u2[:, 0], op0=ALU.mult, op1=ALU.subtract),
                              ["u1", "u2"], ["cp"])
                            if k >= 1:
                                S.op("gpsimd", lambda e, d=d, k=k: e.tensor_copy(out=Y1[:, d, k - 1], in_=cp[:]), ["cp"], ["Y1"])
                            if k <= 7:
                                n = 0
                                for q in range(4):
                                    for ri in range(2):
                                        st_ = (n == 0) and (k > 0 or d == 0)
                                        sp_ = (n == 7) and (k > 0)
                                        S.op("tensor", lambda e, d=d, ri=ri, q=q, st_=st_, sp_=sp_: e.matmul(
                                            kps[:], lhsT=bbz[:, d, ri, q, :], rhs=cp[:, ri, q, :], start=st_, stop=sp_), ["bbz", "cp"], ["kps"])
                                        n += 1
                                if k == 0 and d == 1:
                                    S.op("tensor", lambda e: e.matmul(kps[:], lhsT=g.ident_f, rhs=dgd[:], start=False, stop=True), ["cst", "dgd"], ["kps"])
                                if k > 0 or d == 1:
                                    slot = 0 if k == 0 else (k if d == 0 else 7 + k)
                                    act(Kw[:, slot, :], kps[:], AF.Copy, ["kps"], ["Kw"])
                    S.barrier()

            with ExitStack() as sd:
                sbd = lambda n, s_, d=F32: g.sb(n, s_, d, sd)
                Us = sbd("d_U", [128, 4352], BF16)
                Ys = sbd("d_Y", [128, 4352], BF16)
                Vb = sbd("d_V", [128, 2, 2, NBK]); Wb = sbd("d_W", [128, 2, 2, NBK]); Tb = sbd("d_T", [128, 2, NBK])
                cosT = sbd("d_cosT", [128, 2, NBK]); sinT = sbd("d_sinT", [128, 2, NBK]); tki = sbd("d_tki", [128, 2, NBK], I32)
                gq = sbd("d_gq", [128, 512]); gt2 = sbd("d_gt2", [128, 512]); gsg = sbd("d_gsg", [128, 512])
                Un, Yn = "d_U", "d_Y"
                for sq_ in range(2):
                    cc = slice(sq_ * 256, sq_ * 256 + 256)
                    lc = slice(512 + sq_ * 4096, 512 + (sq_ + 1) * 4096)
                    S.dma("sync", Us[:, 0:256], g.HT[ct * 128:(ct + 1) * 128, cc], reads=[], writes=[Un])
                    S.dma("sync", Us[:, 256:4352], g.HT[ct * 128:(ct + 1) * 128, lc], reads=[], writes=[Un])

                    def ucols(start, step, cnt):
                        return mkap(Us[:, start:start + 1], step, cnt)

                    for d in range(2):
                        for qh in range(2):
                            q0 = 4 * ct + 2 * qh
                            tt(Tb[:], bc(fr8Bv[:, d, q0:q0 + 2], [128, 2, NBK], 2), bc(irow[:], [128, 2, NBK], 1), ALU.mult, ["Bfr8", "irow"], ["Tb"])
                            sinred(Tb[:], sinT[:], tki[:], Wb[:, 0], Wb[:, 1], False, ["Tb"], ["sinT"], names=("tki", "Wb", "Wb"))
                            sinred(Tb[:], cosT[:], tki[:], Wb[:, 0], Wb[:, 1], True, ["Tb"], ["cosT"], names=("tki", "Wb", "Wb"))
                            n = 0
                            for ql in range(2):
                                q = 2 * qh + ql
                                for ri in range(2):
                                    pc, pl = vps_c[n % 2], vps_l[n % 2]
                                    pcn, pln = "s_vpc%d" % (n % 2), "s_vpl%d" % (n % 2)
                                    n += 1
                                    for s in range(8):
                                        if d == 0:
                                            rc, rl = ucols(s, 8, 32), ucols(256 + s, 8, 512)
                                        else:
                                            rc, rl = ucols(31 * 8 + s, -8, 32), ucols(256 + 511 * 8 + s, -8, 512)
                                        S.op("tensor", lambda e, d=d, s=s, ri=ri, q=q, pc=pc, rc=rc: e.matmul(
                                            pc[:], lhsT=Vw[:, d, s, ri, q * 128:(q + 1) * 128], rhs=rc, start=(s == 0), stop=(s == 7)), ["Vw", Un], [pcn])
                                        S.op("tensor", lambda e, d=d, s=s, ri=ri, q=q, pl=pl, rl=rl: e.matmul(
                                            pl[:], lhsT=Vw[:, d, s, ri, q * 128:(q + 1) * 128], rhs=rl, start=(s == 0), stop=(s == 7)), ["Vw", Un], [pln])
                                    act(Vb[:, ri, ql, 0:32], pc[:], AF.Copy, [pcn], ["Vb"])
                                    act(Vb[:, ri, ql, 32:NBK], pl[:], AF.Copy, [pln], ["Vb"])
                            tt(Wb[:, 0], Vb[:, 0], cosT[:], ALU.mult, ["Vb", "cosT"], ["Wb"])
                            tt(Tb[:], Vb[:, 1], sinT[:], ALU.mult, ["Vb", "sinT"], ["Tb"])
                            tt(Wb[:, 0], Wb[:, 0], Tb[:], ALU.add, ["Wb", "Tb"], ["Wb"])
                            tt(Wb[:, 1], Vb[:, 1], cosT[:], ALU.mult, ["Vb", "cosT"], ["Wb"])
                            tt(Tb[:], Vb[:, 0], sinT[:], ALU.mult, ["Vb", "sinT"], ["Tb"])
                            tt(Wb[:, 1], Wb[:, 1], Tb[:], ALU.subtract, ["Wb", "Tb"], ["Wb"])
                            for ql in range(2):
                                for ri in range(2):
                                    V(lambda e, d=d, ql=ql, ri=ri, q0=q0: e.tensor_tensor_scan(
                                        out=Vb[:, ri, ql, :], data0=rho[:, d, q0 + ql:q0 + ql + 1].to_broadcast([128, NBK]),
                                        data1=Wb[:, ri, ql, :], initial=0.0, op0=ALU.mult, op1=ALU.add), ["Wb", "rho", "Vb"], ["Vb"])
                            xs, xn = XS[d], "XS%d" % d
                            qs = slice(2 * qh, 2 * qh + 2)
                            tt(Wb[:, 0], Vb[:, 0], cosT[:], ALU.mult, ["Vb", "cosT"], ["Wb"])
                            tt(Tb[:], Vb[:, 1], sinT[:], ALU.mult, ["Vb", "sinT"], ["Tb"])
                            tt(xs[:, 0, qs, 1:NBK + 1], Wb[:, 0], Tb[:], ALU.subtract, ["Wb", "Tb"], [xn])
                            tt(Wb[:, 1], Vb[:, 1], cosT[:], ALU.mult, ["Vb", "cosT"], ["Wb"])
                            tt(Tb[:], Vb[:, 0], sinT[:], ALU.mult, ["Vb", "sinT"], ["Tb"])
                            tt(xs[:, 1, qs, 1:NBK + 1], Wb[:, 1], Tb[:], ALU.add, ["Wb", "Tb"], [xn])

                    def xcols(d, ri, q, start, step, cnt):
                        return mkap(XS[d][:, ri, q, start:start + 1], step, cnt)

                    def ycols(start, step, cnt):
                        return mkap(Ys[:, start:start + 1], step, cnt)

                    for s in range(8):
                        pc, pl = vps_c[s % 2], vps_l[s % 2]
                        pcn, pln = "s_vpc%d" % (s % 2), "s_vpl%d" % (s % 2)
                        mm = []
                        for q in range(4):
                            for ri in range(2):
                                mm.append((Y1[:, 0, s, ri, q, :], xcols(0, ri, q, 0, 1, 32), xcols(0, ri, q, 32, 1, 512), ["Y1", "XS0"]))
                                mm.append((Y1[:, 1, 7 - s, ri, q, :], xcols(1, ri, q, 31, -1, 32), xcols(1, ri, q, 543, -1, 512), ["Y1", "XS1"]))
                        for s2 in range(8):
                            slot = 0 if s2 == s else ((s - s2) if s2 < s else 7 + (s2 - s))
                            mm.append((Kw[:, slot, :], ucols(s2, 8, 32), ucols(256 + s2, 8, 512), ["Kw", Un]))
                        for i, (lw, rc, rl, rd) in enumerate(mm):
                            S.op("tensor", lambda e, lw=lw, rc=rc, pc=pc, i=i, nm=len(mm): e.matmul(pc[:], lhsT=lw, rhs=rc, start=(i == 0), stop=(i == nm - 1)), rd, [pcn])
                            S.op("tensor", lambda e, lw=lw, rl=rl, pl=pl, i=i, nm=len(mm): e.matmul(pl[:], lhsT=lw, rhs=rl, start=(i == 0), stop=(i == nm - 1)), rd, [pln])
                        for (pp, ppn, N, oc) in ((pc, pcn, 32, ycols(s, 8, 32)), (pl, pln, 512, ycols(256 + s, 8, 512))):
                            act(gq[:, :N], pp[:], AF.Square, [ppn], ["gq"])
                            ts(gq[:, :N], gq[:, :N], 0.044715, 1.0, ALU.mult, ALU.add, ["gq"], ["gq"])
                            tt(gt2[:, :N], gq[:, :N], pp[:], ALU.mult, ["gq", ppn], ["gt2"])
                            act(gsg[:, :N], gt2[:, :N], AF.Sigmoid, ["gt2"], ["gsg"], scale=1.5957691216057308)
                            tt(oc, gsg[:, :N], pp[:], ALU.mult, ["gsg", ppn], [Yn])
                    S.dma("sync", g.GT[ct * 128:(ct + 1) * 128, cc], Ys[:, 0:256], reads=[Yn], writes=[("GT", ct, sq_, 0)])
                    S.dma("sync", g.GT[ct * 128:(ct + 1) * 128, lc], Ys[:, 256:4352], reads=[Yn], writes=[("GT", ct, sq_, 1)])
                S.barrier()

    with ExitStack() as st:
        sb = lambda n, s_, d=F32: g.sb(n, s_, d, st)
        GTv = g.GT.rearrange("(c p) t -> p c t", p=128)
        gw = sb("g_w", [128, NCH, 2 * D], BF16)
        gb = sb("g_b", [128, 16])
        S.op("gpsimd", lambda e: e.dma_start(out=gw[:], in_=g.s5gw[jj].rearrange("(kc p) n -> p kc n", p=128)), [], ["g_w"], dma=True)
        S.dma("sync", gb[:], g.s5gb[jj, :, :], writes=["g_b"])
        xt = [sb("g_xt%d" % i, [128, NCH, 512]) for i in range(2)]
        gi = [sb("g_gi%d" % i, [128, NCH, 512], BF16) for i in range(2)]
        sgm = [sb("g_sg%d" % i, [128, 512]) for i in range(2)]
        yl = [sb("g_yl%d" % i, [128, 512]) for i in range(2)]
        pa = [g.ps("g_pa%d" % i, [128, 512], F32, st) for i in range(2)]
        pb = [g.ps("g_pb%d" % i, [128, 512], F32, st) for i in range(2)]
        tiles = list(range(1 if last else 0, NT))
        for jn, j in enumerate(tiles):
            x, xn = xt[jn % 2], "g_xt%d" % (jn % 2)
            gg, ggn = gi[jn % 2], "g_gi%d" % (jn % 2)
            stream = stream_of_tile(j)
            S.dma("sync", x[:], XTv[:, :, j * 512:(j + 1) * 512], reads=[("XT", j)], writes=[xn])
            S.dma("sync", gg[:], GTv[:, :, j * 512:(j + 1) * 512], reads=[], writes=[ggn])
            for oc in range(NCH):
                q = oc % 2
                for kc in range(NCH):
                    S.op("tensor", lambda e, oc=oc, kc=kc, q=q, gg=gg: e.matmul(pa[q][:], lhsT=gw[:, kc, oc * 128:(oc + 1) * 128], rhs=gg[:, kc, :],
                                                                               start=(kc == 0), stop=(kc == NCH - 1)), ["g_w", ggn], ["g_pa%d" % q])
                for kc in range(NCH):
                    S.op("tensor", lambda e, oc=oc, kc=kc, q=q, gg=gg: e.matmul(pb[q][:], lhsT=gw[:, kc, D + oc * 128:D + (oc + 1) * 128], rhs=gg[:, kc, :],
                                                                               start=(kc == 0), stop=(kc == NCH - 1)), ["g_w", ggn], ["g_pb%d" % q])
                act(sgm[q][:], pb[q][:], AF.Sigmoid, ["g_pb%d" % q, "g_b"], ["g_sg%d" % q], bias=gb[:, 8 + oc:9 + oc], scale=1.0)
                V(lambda e, oc=oc, q=q: e.scalar_tensor_tensor(out=yl[q][:], in0=pa[q][:], scalar=gb[:, oc:oc + 1], in1=sgm[q][:], op0=ALU.add, op1=ALU.mult),
                  ["g_pa%d" % q, "g_sg%d" % q, "g_b"], ["g_yl%d" % q])
                V(lambda e, oc=oc, q=q, x=x, stream=stream: e.scalar_tensor_tensor(
                    out=x[:, oc, :], in0=yl[q][:], scalar=g.mods[l][:, 2, oc, stream:stream + 1], in1=x[:, oc, :], op0=ALU.mult, op1=ALU.add),
                  ["g_yl%d" % q, xn, ("mods", l)], [xn])
            S.dma("sync", XTv[:, :, j * 512:(j + 1) * 512], x[:], reads=[xn], writes=[("XT", j)])
        S.barrier()


def ssd_layer(g, l):
    nc, S = g.nc, g.S
    last = (l == 3)
    XTv = g.XT.rearrange("(c p) t -> p c t", p=128)
    DI = 2 * D

    def V(fn, r, w, eng="vector"):
        S.op(eng, fn, r, w)

    def tt(out, a, b, op, r, w, eng="vector"):
        S.op(eng, lambda e: e.tensor_tensor(out=out, in0=a, in1=b, op=op), r, w)

    def act(out, in_, func, r, w, **kw):
        S.op("scalar", lambda e: e.activation(out=out, in_=in_, func=func, **kw), r, w)

    with ExitStack() as st:
        sb = lambda n, s_, d=F32: g.sb(n, s_, d, st)
        w = sb("m_w", [128, NCH, 6208], BF16)
        Wv = g.ssd_inw.rearrange("(kc p) n -> p kc n", p=128)
        for i in range(4):
            c0, c1 = i * 1552, (i + 1) * 1552
            S.op("gpsimd", lambda e, c0=c0, c1=c1: e.dma_start(out=w[:, :, c0:c1], in_=Wv[:, :, c0:c1]), [], [("m_w", i)], dma=True)
        wtok = [("m_w", i) for i in range(4)]
        xt = [sb("m_xt%d" % i, [128, NCH, 512]) for i in range(2)]
        sq = sb("m_sq", [128, NCH, 512], BF16)
        tmp = sb("m_tmp", [128, NCH, 512])
        hbf = sb("m_hbf", [128, NCH, 512], BF16)
        rstd = sb("m_rstd", [128, 512])
        ob = [sb("m_ob%d" % i, [128, 8, 512], BF16) for i in range(2)]
        dtb = sb("m_dtb", [64, 512])
        ssp = g.ps("m_ssp", [128, 512], F32, st)
        pp = [g.ps("m_pp%d" % i, [128, 512], F32, st) for i in range(3)]
        n = 0
        nb = 0
        for j in range(NT):
            x, xn = xt[j % 2], "m_xt%d" % (j % 2)
            cols = slice(j * 512, (j + 1) * 512)
            S.dma("sync", x[:], XTv[:, :, cols], reads=[("XT", j)], writes=[xn])
            normmod(g, l, 1, x, xn, sq, ssp, rstd, tmp, None, hbf, stream_of_tile(j), tag="M")
            for grp in range(6):
                o_, on = ob[nb % 2], "m_ob%d" % (nb % 2)
                nb += 1
                for ci in range(8):
                    oc = grp * 8 + ci
                    p_, pn = pp[n % 3], "m_pp%d" % (n % 3)
                    n += 1
                    for kc in range(NCH):
                        S.op("tensor", lambda e, kc=kc, oc=oc, p_=p_: e.matmul(p_[:], lhsT=w[:, kc, oc * 128:(oc + 1) * 128], rhs=hbf[:, kc, :],
                                                                           start=(kc == 0), stop=(kc == NCH - 1)), wtok + ["hbfM"], [pn])
                    act(o_[:, ci, :], p_[:], AF.Silu if grp < 2 else AF.Copy, [pn], [(on, ci)])
                rd = [(on, ci) for ci in range(8)]
                if grp < 2:
                    dstv = g.SZ.rearrange("(c p) t -> p c t", p=128)[:, grp * 8:(grp + 1) * 8, cols]
                else:
                    dstv = g.XBCp.rearrange("(c p) t -> p c t", p=128)[:, (grp - 2) * 8:(grp - 1) * 8, cols]
                S.dma("sync", dstv, o_[:], reads=rd, writes=[("inproj", j, grp)])
            p_, pn = pp[n % 3], "m_pp%d" % (n % 3)
            n += 1
            for kc in range(NCH):
                S.op("tensor", lambda e, kc=kc, p_=p_: e.matmul(p_[0:64, :], lhsT=w[:, kc, 6144:6208], rhs=hbf[:, kc, :], start=(kc == 0),
                                                             stop=(kc == NCH - 1)), wtok + ["hbfM"], [pn])
            act(dtb[:], p_[0:64, :], AF.Copy, [pn], ["dtb"])
            S.dma("sync", g.DTr[:, cols], dtb[:], reads=["dtb"], writes=[("dtr", j)])
        S.barrier()

    PBW = TCORE + 16
    seg = [(0, 256), (256, 256), (512, 4096), (4608, 4096)]
    segp = [2, 2 + 260, 2 + 520, 2 + 520 + 4100]
    with ExitStack() as st:
        sb = lambda n, s_, d=F32: g.sb(n, s_, d, st)
        pb_ = [sb("c_pb%d" % i, [128, PBW], BF16) for i in range(2)]
        acc = sb("c_acc", [128, PBW])
        cob = [sb("c_co%d" % i, [128, PBW], BF16) for i in range(2)]
        cw = sb("c_w", [128, 32, 6])
        tr_sb = [sb("c_tr%d" % i, [128, 4, 128], BF16) for i in range(2)]
        trp = [g.ps("c_trp%d" % i, [128, 4, 128], BF16, st) for i in range(2)]
        S.dma("sync", cw[:], g.ssd_cw[:, :, :], writes=["c_w"])
        for i in range(2):
            V(lambda e, i=i: e.memset(pb_[i][:], 0.0), [], ["c_pb%d" % i])
        L = PBW - 4
        ntr = 0
        for c in range(32):
            p_, pn = pb_[c % 2], "c_pb%d" % (c % 2)
            o_, on = cob[c % 2], "c_co%d" % (c % 2)
            for si, (gc, ln) in enumerate(seg):
                S.dma("sync", p_[:, segp[si]:segp[si] + ln], g.XBCp[c * 128:(c + 1) * 128, gc:gc + ln], writes=[pn])
            V(lambda e, c=c, p_=p_: e.tensor_scalar(out=acc[:, 0:L], in0=p_[:, 0:L], scalar1=cw[:, c, 0:1], scalar2=None, op0=ALU.mult), [pn, "c_w"], ["c_acc"])
            for k in range(1, 5):
                V(lambda e, c=c, k=k, p_=p_: e.scalar_tensor_tensor(out=acc[:, 0:L], in0=p_[:, k:k + L], scalar=cw[:, c, k:k + 1], in1=acc[:, 0:L],
                                                                   op0=ALU.mult, op1=ALU.add), [pn, "c_w", "c_acc"], ["c_acc"])
            act(o_[:, 2:2 + L], acc[:, 0:L], AF.Silu, ["c_acc", "c_w"], [on], bias=cw[:, c, 5:6], scale=1.0)
            for si, (gc, ln) in enumerate(seg):
                src = o_[:, segp[si]:segp[si] + ln]
                if c < 16:
                    S.dma("sync", g.XFo[c * 128:(c + 1) * 128, gc:gc + ln], src, reads=[on], writes=[("xfo", c, si)])
                else:
                    S.dma("sync", g.BCo[(c - 16) * 128:(c - 15) * 128, gc:gc + ln], src, reads=[on], writes=[("bco", c, si)])
            if c < 16:
                for si, (gc, ln) in enumerate(seg):
                    for t4 in range(ln // 512 if ln >= 512 else 1):
                        nsub = 4 if ln >= 512 else ln // 128
                        tp, tpn = trp[ntr % 2], "c_trp%d" % (ntr % 2)
                        ts_, tsn = tr_sb[ntr % 2], "c_tr%d" % (ntr % 2)
                        ntr += 1
                        for u in range(nsub):
                            a0 = segp[si] + t4 * 512 + u * 128
                            S.op("tensor", lambda e, tp=tp, u=u, a0=a0, o_=o_: e.transpose(tp[:, u, :], o_[:, a0:a0 + 128], g.ident_bf[:]),
                                 [on, "ident_bf"], [tpn])
                        act(ts_[:, 0:nsub, :], tp[:, 0:nsub, :], AF.Copy, [tpn], [tsn])
                        r0 = gc + t4 * 512
                        S.dma("sync", g.XTOK[r0:r0 + nsub * 128, c * 128:(c + 1) * 128].rearrange("(u p) v -> p u v", p=128), ts_[:, 0:nsub, :],
                              reads=[tsn], writes=[("xtok", c, si, t4)])
        S.barrier()

    with ExitStack() as st:
        sb = lambda n, s_, d=F32: g.sb(n, s_, d, st)
        hp = sb("k_hp", [64, 4])
        mk = sb("k_mk", [128, 2, 4, 512], BF16)
        S.dma("sync", hp[:], g.ssd_hp[:, :], writes=["k_hp"])
        S.op("gpsimd", lambda e: e.dma_start(out=mk[:], in_=g.ssd_mask[:, :, :, :]), [], ["k_mk"], dma=True)
        aneg = sb("k_aneg", [64, 1])
        act(aneg[:], hp[:, 1:2], AF.Exp, ["k_hp"], ["k_aneg"])
        V(lambda e: e.tensor_scalar(out=aneg[:], in0=aneg[:], scalar1=-1.0, scalar2=None, op0=ALU.mult), ["k_aneg"], ["k_aneg"])
        dt = sb("k_dt", [64, 4352]); psi = sb("k_psi", [64, 4352]); dta = sb("k_dta", [64, 4352])
        psiT = sb("k_psiT", [128, 34, 64]); dtT = sb("k_dtT", [128, 34, 64])
        sel = sb("k_sel", [64, 128])
        bcs = sb("k_bcs", [128, 8, 512])
        Bg = sb("k_B", [128, 4352], BF16); Cg = sb("k_C", [128, 4352], BF16)
        Xg = sb("k_X", [128, 34, 256], BF16)
        xdt = sb("k_xdt", [128, 34, 2, 256], BF16)
        Gs = [sb("k_G%d" % i, [128, 512], BF16) for i in range(2)]
        Gm = [sb("k_Gm%d" % i, [128, 512], BF16) for i in range(2)]
        Et = [sb("k_E%d" % i, [128, 512], BF16) for i in range(6)]
        Wt = [sb("k_W%d" % i, [128, 512], BF16) for i in range(6)]
        args_ = [sb("k_arg%d" % i, [128, 512]) for i in range(3)]
        yo = [sb("k_yo%d" % i, [64, 512], BF16) for i in range(2)]
        tps = g.ps("k_tps", [128, 4, 64], F32, st)
        bps = g.ps("k_bps", [128, 512], F32, st)
        gps = [g.ps("k_gps%d" % i, [128, 512], F32, st) for i in range(2)]
        yps = [g.ps("k_yps%d" % i, [64, 512], F32, st) for i in range(4)]
        ei = 0
        gi_ = 0
        yi = 0
        for sq_ in range(2):
            cc = slice(sq_ * 256, sq_ * 256 + 256)
            lc = slice(512 + sq_ * 4096, 512 + (sq_ + 1) * 4096)
            S.dma("sync", dt[:, 0:256], g.DTr[:, cc], writes=["k_dt"])
            S.dma("sync", dt[:, 256:4352], g.DTr[:, lc], writes=["k_dt"])
            act(dt[:], dt[:], AF.Exp, ["k_dt", "k_hp"], ["k_dt"], bias=hp[:, 0:1], scale=1.0)
            V(lambda e: e.tensor_scalar(out=dt[:], in0=dt[:], scalar1=1.0, scalar2=None, op0=ALU.add), ["k_dt"], ["k_dt"])
            act(dt[:], dt[:], AF.Ln, ["k_dt"], ["k_dt"])
            V(lambda e: e.tensor_scalar(out=dta[:], in0=dt[:], scalar1=aneg[:, 0:1], scalar2=None, op0=ALU.mult), ["k_dt", "k_aneg"], ["k_dta"])
            V(lambda e: e.tensor_tensor_scan(out=psi[0:32, :], data0=g.cst_t[0:32, 2, 0:1].to_broadcast([32, 4352]), data1=dta[0:32, :], initial=0.0, op0=ALU.mult, op1=ALU.add),
              ["k_dta", "cst"], ["k_psi"])
            V(lambda e: e.tensor_tensor_scan(out=mkap(psi[32:64, 255:256], -1, 256), data0=g.cst_t[32:64, 2, 0:1].to_broadcast([32, 256]),
                                             data1=mkap(dta[32:64, 255:256], -1, 256), initial=0.0, op0=ALU.mult, op1=ALU.add),
              ["k_dta", "cst"], ["k_psi"])
            V(lambda e: e.tensor_tensor_scan(out=mkap(psi[32:64, 4351:4352], -1, 4096), data0=g.cst_t[32:64, 2, 0:1].to_broadcast([32, 4096]),
                                             data1=mkap(dta[32:64, 4351:4352], -1, 4096), initial=0.0, op0=ALU.mult, op1=ALU.add),
              ["k_dta", "cst"], ["k_psi"])
            V(lambda e: e.tensor_scalar(out=psi[32:64, 256:4352], in0=psi[32:64, 256:4352], scalar1=psi[32:64, 0:1], scalar2=None, op0=ALU.add),
              ["k_psi"], ["k_psi"])
            for which, (src, dst, dn, scl) in enumerate(((psi, psiT, "k_psiT", -1.0), (dt, dtT, "k_dtT", 1.0))):
                srcn = "k_psi" if which == 0 else "k_dt"
                for k4 in range(9):
                    nk = 4 if k4 < 8 else 2
                    for u in range(nk):
                        kt = k4 * 4 + u
                        S.op("tensor", lambda e, u=u, kt=kt, src=src: e.transpose(tps[:, u, :], src[:, kt * 128:(kt + 1) * 128], g.cst_t[0:64, 0, 0:64]),
                             [srcn, "cst"], ["k_tps"])
                    act(dst[:, k4 * 4:k4 * 4 + nk, :], tps[:, 0:nk, :], AF.Copy, ["k_tps"], [dn], scale=scl)
            for grp in range(8):
                S.dma("sync", Bg[:, 0:256], g.BCo[grp * 128:(grp + 1) * 128, cc], writes=["k_B"])
                S.dma("sync", Bg[:, 256:4352], g.BCo[grp * 128:(grp + 1) * 128, lc], writes=["k_B"])
                S.dma("sync", Cg[:, 0:256], g.BCo[D + grp * 128:D + (grp + 1) * 128, cc], writes=["k_C"])
                S.dma("sync", Cg[:, 256:4352], g.BCo[D + grp * 128:D + (grp + 1) * 128, lc], writes=["k_C"])
                S.dma("sync", Xg[:, 0:2, :], g.XTOK[cc, grp * 256:(grp + 1) * 256].rearrange("(kt p) v -> p kt v", p=128), writes=["k_X"])
                S.dma("sync", Xg[:, 2:34, :], g.XTOK[lc, grp * 256:(grp + 1) * 256].rearrange("(kt p) v -> p kt v", p=128), writes=["k_X"])
                for d in range(2):
                    V(lambda e, d=d, grp=grp: e.tensor_tensor(
                        out=xdt[:, :, d, :].rearrange("p k (h v) -> p k h v", h=4), in0=Xg[:].rearrange("p k (h v) -> p k h v", h=4),
                        in1=bc(dtT[:, :, d * 32 + 4 * grp:d * 32 + 4 * grp + 4], [128, 34, 4, 64], 3), op=ALU.mult), ["k_X", "k_dtT"], ["k_xdt"])
                for qt in range(9):
                    if qt == 0:
                        q0, N, ktq = 0, 256, 0
                    else:
                        q0, N, ktq = 256 + (qt - 1) * 512, 512, 2 + 4 * (qt - 1)
                    nq = N // 128
                    for d in range(2):
                        for h in range(4):
                            dh = d * 32 + 4 * grp + h
                            V(lambda e, dh=dh: e.tensor_copy(out=sel[:], in_=g.cst_t[0:64, 0, dh:dh + 1].to_broadcast([64, 128])), ["cst"], ["k_sel"])
                            S.op("tensor", lambda e, q0=q0, N=N: e.matmul(bps[:, :N], lhsT=sel[:], rhs=psi[:, q0:q0 + N], start=True, stop=True),
                                 ["k_sel", "k_psi"], ["k_bps"])
                            act(bcs[:, d * 4 + h, :N], bps[:, :N], AF.Copy, ["k_bps"], [("k_bcs", d * 4 + h)])
                    work = []
                    for kt in range(34):
                        inq = ktq <= kt < ktq + nq
                        o = kt - ktq
                        if qt == 0:
                            if kt < 2:
                                work.append((kt, True, True, o))
                        else:
                            if kt < 2:
                                work.append((kt, True, True, None))
                            elif inq:
                                work.append((kt, True, True, o))
                            elif kt < ktq:
                                work.append((kt, True, False, None))
                            else:
                                work.append((kt, False, True, None))
                    nf = sum(1 for w_ in work if w_[1])
                    nbk = sum(1 for w_ in work if w_[2])
                    tot = nf + nbk
                    cnt = [0, 0, 0, 0]
                    ginfo = {}

                    def emitG(wi):
                        nonlocal gi_
                        kt = work[wi][0]
                        gp_, gpn = gps[gi_ % 2], "k_gps%d" % (gi_ % 2)
                        G_, Gn = Gs[gi_ % 2], "k_G%d" % (gi_ % 2)
                        gi_ += 1
                        S.op("tensor", lambda e, gp_=gp_, kt=kt, q0=q0, N=N: e.matmul(gp_[:, :N], lhsT=Bg[:, kt * 128:(kt + 1) * 128], rhs=Cg[:, q0:q0 + N],
                                                                                   start=True, stop=True), ["k_B", "k_C"], [gpn])
                        V(lambda e, G_=G_, gp_=gp_, N=N: e.tensor_copy(out=G_[:, :N], in_=gp_[:, :N]), [gpn], [Gn])
                        ginfo[wi] = (G_, Gn)

                    def emitRest(wi):
                        nonlocal ei
                        (kt, fw, bw, o) = work[wi]
                        G_, Gn = ginfo.pop(wi)
                        for d in range(2):
                            if not (fw if d == 0 else bw):
                                continue
                            if o is not None:
                                S.op("gpsimd", lambda e, d=d, o=o, G_=G_, N=N: e.tensor_tensor(out=Gm[d][:, :N], in0=G_[:, :N], in1=mk[:, d, o, :N], op=ALU.mult),
                                     [Gn, "k_mk"], ["k_Gm%d" % d])
                                Gsrc, Gsn = Gm[d], "k_Gm%d" % d
                            else:
                                Gsrc, Gsn = G_, Gn
                            for h in range(4):
                                dh = d * 32 + 4 * grp + h
                                E_, En = Et[ei % 6], "k_E%d" % (ei % 6)
                                W_, Wn = Wt[ei % 6], "k_W%d" % (ei % 6)
                                arg, argn = args_[ei % 3], "k_arg%d" % (ei % 3)
                                ei += 1
                                if o is not None:
                                    V(lambda e, d=d, h=h, kt=kt, dh=dh, N=N, arg=arg: e.tensor_scalar(out=arg[:, :N], in0=bcs[:, d * 4 + h, :N], scalar1=psiT[:, kt, dh:dh + 1],
                                                                                    scalar2=0.0, op0=ALU.add, op1=ALU.min),
                                      [("k_bcs", d * 4 + h), "k_psiT"], [argn])
                                    act(E_[:, :N], arg[:, :N], AF.Exp, [argn], [En])
                                else:
                                    act(E_[:, :N], bcs[:, d * 4 + h, :N], AF.Exp, [("k_bcs", d * 4 + h), "k_psiT"], [En], bias=psiT[:, kt, dh:dh + 1], scale=1.0)
                                weng = "gpsimd" if (ei % 6 == 0) else "vector"
                                S.op(weng, lambda e, W_=W_, E_=E_, Gsrc=Gsrc, N=N: e.tensor_tensor(out=W_[:, :N], in0=Gsrc[:, :N], in1=E_[:, :N], op=ALU.mult),
                                     [Gsn, En], [Wn])
                                S.op("tensor", lambda e, h=h, kt=kt, d=d, W_=W_, N=N, st_=(cnt[h] == 0), sp_=(cnt[h] == tot - 1): e.matmul(
                                    yps[h][:, :N], lhsT=xdt[:, kt, d, h * 64:(h + 1) * 64], rhs=W_[:, :N], start=st_, stop=sp_), ["k_xdt", Wn], ["k_yps%d" % h])
                                cnt[h] += 1

                    for wi in range(len(work) + 1):
                        if wi < len(work):
                            emitG(wi)
                        if wi >= 1:
                            emitRest(wi - 1)
                    for h in range(4):
                        y_, yn = yo[yi % 2], "k_yo%d" % (yi % 2)
                        yi += 1
                        act(y_[:, :N], yps[h][:, :N], AF.Copy, ["k_yps%d" % h], [yn])
                        r0 = (4 * grp + h) * 64
                        c0 = (sq_ * 256) if qt == 0 else (512 + sq_ * 4096 + (qt - 1) * 512)
                        S.dma("sync", g.YT[r0:r0 + 64, c0:c0 + N], y_[:, :N], reads=[yn], writes=[("yt", sq_, grp, qt, h)])
        S.barrier()

    with ExitStack() as st:
        sb = lambda n, s_, d=F32: g.sb(n, s_, d, st)
        fv = sb("f_v", [128, 16, 2])
        S.dma("sync", fv[:], g.ssd_fv[:, :, :], writes=["f_v"])
        yb = [sb("f_y%d" % i, [128, 16, 512], BF16) for i in range(2)]
        xb = [sb("f_x%d" % i, [128, 16, 512], BF16) for i in range(2)]
        zb = [sb("f_z%d" % i, [128, 16, 512], BF16) for i in range(2)]
        gy = sb("f_gy", [128, 16, 512])
        gsq = sb("f_gsq", [128, 16, 512], BF16)
        rr = sb("f_rr", [128, 512])
        go = [sb("f_go%d" % i, [128, 16, 512], BF16) for i in range(2)]
        nps = [g.ps("f_nps%d" % i, [128, 512], F32, st) for i in range(2)]
        YTv = g.YT.rearrange("(c p) t -> p c t", p=128)
        XFv = g.XFo.rearrange("(c p) t -> p c t", p=128)
        SZv = g.SZ.rearrange("(c p) t -> p c t", p=128)
        G2v = g.GT2.rearrange("(c p) t -> p c t", p=128)
        tiles = list(range(1 if last else 0, NT))
        for jn, j in enumerate(tiles):
            q = jn % 2
            cols = slice(j * 512, (j + 1) * 512)
            S.dma("sync", yb[q][:], YTv[:, :, cols], writes=["f_y%d" % q])
            S.dma("sync", xb[q][:], XFv[:, :, cols], writes=["f_x%d" % q])
            S.dma("sync", zb[q][:], SZv[:, :, cols], writes=["f_z%d" % q])
            for c in range(16):
                V(lambda e, c=c, q=q: e.scalar_tensor_tensor(out=gy[:, c, :], in0=xb[q][:, c, :], scalar=fv[:, c, 0:1], in1=yb[q][:, c, :],
                                                             op0=ALU.mult, op1=ALU.add), ["f_x%d" % q, "f_y%d" % q, "f_v"], [("f_gy", c)])
                S.op("gpsimd", lambda e, c=c, q=q: e.tensor_tensor(out=gy[:, c, :], in0=gy[:, c, :], in1=zb[q][:, c, :], op=ALU.mult),
                     [("f_gy", c), "f_z%d" % q], [("f_gy", c)])
                act(gsq[:, c, :], gy[:, c, :], AF.Square, [("f_gy", c)], [("f_gsq", c)])
            for grp in range(8):
                np_, npn = nps[grp % 2], "f_nps%d" % (grp % 2)
                for u in range(2):
                    S.op("tensor", lambda e, grp=grp, u=u, np_=np_: e.matmul(np_[:], lhsT=g.ones_bf[:], rhs=gsq[:, 2 * grp + u, :], start=(u == 0), stop=(u == 1)),
                         [("f_gsq", 2 * grp + u), "ones_bf"], [npn])
                V(lambda e, np_=np_: e.tensor_scalar(out=rr[:], in0=np_[:], scalar1=1.0 / 256, scalar2=EPS, op0=ALU.mult, op1=ALU.add), [npn], ["f_rr"])
                act(rr[:], rr[:], AF.Sqrt, ["f_rr"], ["f_rr"])
                V(lambda e: e.reciprocal(out=rr[:], in_=rr[:]), ["f_rr"], ["f_rr"])
                for u in range(2):
                    c = 2 * grp + u
                    V(lambda e, c=c, q=q: e.scalar_tensor_tensor(out=go[q][:, c, :], in0=gy[:, c, :], scalar=fv[:, c, 1:2], in1=rr[:],
                                                                 op0=ALU.mult, op1=ALU.mult), [("f_gy", c), "f_rr", "f_v"], [("f_go%d" % q, c)])
            S.dma("sync", G2v[:, :, cols], go[q][:], reads=[("f_go%d" % q, c) for c in range(16)], writes=[("gt2", j)])
        S.barrier()
    out_proj_residual(g, l, g.ssd_ow, DI, last, "so")


def da_layer(g, l):
    import math
    nc, S = g.nc, g.S
    last = (l == 3)
    lam_init = 0.8 - 0.6 * math.exp(-0.3 * l)
    XTv = g.XT.rearrange("(c p) t -> p c t", p=128)
    GTv = g.GT.rearrange("(c p) t -> p c t", p=128)

    def V(fn, r, w, eng="vector"):
        S.op(eng, fn, r, w)

    def tt(out, a, b, op, r, w, eng="vector"):
        S.op(eng, lambda e: e.tensor_tensor(out=out, in0=a, in1=b, op=op), r, w)

    def act(out, in_, func, r, w, **kw):
        S.op("scalar", lambda e: e.activation(out=out, in_=in_, func=func, **kw), r, w)

    with ExitStack() as sl:
        dac = g.sb("a_dac", [128, 3, 128], F32, sl)
        Rm = g.sb("a_Rm", [128, 128], BF16, sl)
        bones = g.sb("a_bones", [128, 128], BF16, sl)
        gv = g.sb("a_gv", [128, 4], F32, sl)
        lamt = g.sb("a_lamt", [64, 4], F32, sl)
        lpr = g.sb("a_lpr", [64, 2], F32, sl)
        lbc = g.sb("a_lbc", [128, 2], F32, sl)
        nlam = g.sb("a_nlam", [128, 1], F32, sl)
        S.dma("sync", dac[:], g.dac[:, :, :], writes=["dac"])
        S.dma("sync", gv[:], g.dagv[:, :], writes=["gv"])
        S.dma("sync", lamt[:], g.dalam[:, :], writes=["lamt"])
        V(lambda e: e.tensor_copy(out=Rm[:], in_=dac[:, 0, :]), ["dac"], ["Rm"])
        V(lambda e: e.tensor_copy(out=bones[:], in_=dac[:, 1, :]), ["dac"], ["bones"])
        V(lambda e: e.tensor_scalar(out=gv[:, 0:1], in0=gv[:, 0:1], scalar1=0.125, scalar2=None, op0=ALU.mult), ["gv"], ["gv"])
        V(lambda e: e.tensor_scalar(out=gv[:, 2:3], in0=gv[:, 2:3], scalar1=1.0 - lam_init, scalar2=None, op0=ALU.mult), ["gv"], ["gv"])
        tt(lpr[:, 0:1], lamt[:, 0:1], lamt[:, 1:2], ALU.mult, ["lamt"], ["lpr"])
        tt(lpr[:, 1:2], lamt[:, 2:3], lamt[:, 3:4], ALU.mult, ["lamt"], ["lpr"])
        with ExitStack() as s0:
            lps = g.ps("a_lps", [128, 2], F32, s0)
            S.op("tensor", lambda e: e.matmul(lps[:], lhsT=g.cst_t[0:64, 2, :], rhs=lpr[:], start=True, stop=True), ["lpr", "cst"], ["lps"])
            act(lbc[:], lps[:], AF.Exp, ["lps"], ["lbc"])
            V(lambda e: e.scalar_tensor_tensor(out=nlam[:], in0=lbc[:, 1:2], scalar=-lam_init, in1=lbc[:, 0:1], op0=ALU.add, op1=ALU.subtract),
              ["lbc"], ["nlam"])
            S.barrier()

        with ExitStack() as st:
            sb = lambda n, s_, d=F32: g.sb(n, s_, d, st)
            w = sb("a_w", [128, NCH, 3 * D], BF16)
            S.op("gpsimd", lambda e: e.dma_start(out=w[:, :, 0:1536], in_=g.daqkv.rearrange("(kc p) n -> p kc n", p=128)[:, :, 0:1536]), [], ["a_w0"], dma=True)
            S.op("gpsimd", lambda e: e.dma_start(out=w[:, :, 1536:3072], in_=g.daqkv.rearrange("(kc p) n -> p kc n", p=128)[:, :, 1536:3072]), [], ["a_w1"], dma=True)
            xt = [sb("a_xt%d" % i, [128, NCH, 512]) for i in range(2)]
            sq = sb("a_sq", [128, NCH, 512], BF16)
            tmp = sb("a_tmp", [128, NCH, 512])
            hbf = sb("a_hbf", [128, NCH, 512], BF16)
            rstd = sb("a_rstd", [128, 512])
            rc = sb("a_rc", [128, 512]); rs = sb("a_rs", [128, 512])
            qsq = sb("a_qsq", [128, 512], BF16)
            qr = sb("a_qr", [128, 512]); qn = sb("a_qn", [128, 512]); qnb = sb("a_qnb", [128, 512], BF16)
            t1 = sb("a_t1", [128, 512]); t2 = sb("a_t2", [128, 512])
            qo = [sb("a_qo%d" % i, [128, 512], BF16) for i in range(2)]
            vt = [sb("a_vt%d" % i, [128, D], BF16) for i in range(2)]
            ssp = g.ps("a_ssp", [128, 512], F32, st)
            pq = [g.ps("a_pq%d" % i, [128, 512], F32, st) for i in range(2)]
            pss = g.ps("a_pss", [128, 512], F32, st)
            prq = g.ps("a_prq", [128, 512], F32, st)
            pv = [g.ps("a_pv%d" % i, [128, 512], F32, st) for i in range(2)]
            n = 0
            nv = 0
            for j in range(NT):
                x, xn = xt[j % 2], "a_xt%d" % (j % 2)
                S.dma("sync", x[:], XTv[:, :, j * 512:(j + 1) * 512], reads=[("XT", j)], writes=[xn])
                normmod(g, l, 1, x, xn, sq, ssp, rstd, tmp, None, hbf, stream_of_tile(j), tag="DA")
                if j >= 1:
                    pos0 = ((j - 1) % 8) * 512
                    S.dma("sync", rc[:], g.ropeC[:, pos0:pos0 + 512], writes=["rc"])
                    S.dma("sync", rs[:], g.ropeS[:, pos0:pos0 + 512], writes=["rs"])
                for which in range(2):
                    dst = g.QT if which == 0 else g.KT
                    for hd in range(8):
                        p_, pn = pq[n % 2], "a_pq%d" % (n % 2)
                        o_, on = qo[n % 2], "a_qo%d" % (n % 2)
                        n += 1
                        c0 = which * D + hd * 128
                        for kc in range(NCH):
                            S.op("tensor", lambda e, kc=kc, c0=c0, p_=p_: e.matmul(p_[:], lhsT=w[:, kc, c0:c0 + 128], rhs=hbf[:, kc, :], start=(kc == 0),
                                                                               stop=(kc == NCH - 1)), ["a_w0", "a_w1", "hbfDA"], [pn])
                        act(qsq[:], p_[:], AF.Square, [pn], ["qsq"])
                        S.op("tensor", lambda e: e.matmul(pss[:], lhsT=bones[:], rhs=qsq[:], start=True, stop=True), ["qsq", "bones"], ["pss"])
                        V(lambda e: e.tensor_scalar(out=qr[:], in0=pss[:], scalar1=1.0 / 64, scalar2=EPS, op0=ALU.mult, op1=ALU.add), ["pss"], ["qr"])
                        act(qr[:], qr[:], AF.Sqrt, ["qr"], ["qr"])
                        V(lambda e: e.reciprocal(out=qr[:], in_=qr[:]), ["qr"], ["qr"])
                        if j == 0:
                            V(lambda e, which=which, p_=p_, o_=o_: e.scalar_tensor_tensor(out=o_[:], in0=p_[:], scalar=gv[:, which:which + 1], in1=qr[:],
                                                                                      op0=ALU.mult, op1=ALU.mult), [pn, "qr", "gv"], [on])
                        else:
                            V(lambda e, which=which, p_=p_: e.scalar_tensor_tensor(out=qn[:], in0=p_[:], scalar=gv[:, which:which + 1], in1=qr[:],
                                                                               op0=ALU.mult, op1=ALU.mult), [pn, "qr", "gv"], ["qn"])
                            S.op("gpsimd", lambda e: e.tensor_copy(out=qnb[:], in_=qn[:]), ["qn"], ["qnb"])
                            S.op("tensor", lambda e: e.matmul(prq[:], lhsT=Rm[:], rhs=qnb[:], start=True, stop=True), ["qnb", "Rm"], ["prq"])
                            S.op("gpsimd", lambda e: e.tensor_tensor(out=t1[:], in0=qn[:], in1=rc[:], op=ALU.mult), ["qn", "rc"], ["t1"])
                            tt(t2[:], prq[:], rs[:], ALU.mult, ["prq", "rs"], ["t2"])
                            S.op("gpsimd", lambda e, o_=o_: e.tensor_tensor(out=o_[:], in0=t1[:], in1=t2[:], op=ALU.add), ["t1", "t2"], [on])
                        S.dma("sync", dst[hd * 128:(hd + 1) * 128, j * 512:(j + 1) * 512], o_[:], reads=[on], writes=[("qk", which, hd, j)])
                for s_ in range(4):
                    v_, vn = vt[nv % 2], "a_vt%d" % (nv % 2)
                    nv += 1
                    for half in range(2):
                        for kc in range(NCH):
                            S.op("tensor", lambda e, kc=kc, half=half, s_=s_: e.matmul(
                                pv[half][:], lhsT=hbf[:, kc, s_ * 128:(s_ + 1) * 128], rhs=w[:, kc, 2 * D + half * 512:2 * D + (half + 1) * 512],
                                start=(kc == 0), stop=(kc == NCH - 1)), ["a_w0", "a_w1", "hbfDA"], ["a_pv%d" % half])
                        act(v_[:, half * 512:(half + 1) * 512], pv[half][:], AF.Copy, ["a_pv%d" % half], [(vn, half)])
                    r0 = j * 512 + s_ * 128
                    S.dma("sync", g.VTOK[r0:r0 + 128, :], v_[:], reads=[(vn, 0), (vn, 1)], writes=[("vtok", j, s_)])
            S.barrier()

        with ExitStack() as st:
            sb = lambda n, s_, d=F32: g.sb(n, s_, d, st)
            Kh = [sb("b_K%d" % i, [128, 4352], BF16) for i in range(2)]
            Qh = [sb("b_Q%d" % i, [128, 4352], BF16) for i in range(2)]
            Vh = [sb("b_V%d" % i, [128, 34, 128], BF16) for i in range(2)]
            pt = [sb("b_pt%d" % i, [128, 512], BF16) for i in range(3)]
            rsum = sb("b_rsum", [128, 512])
            oc_ = [sb("b_oc%d" % i, [128, 512]) for i in range(2)]
            osq = sb("b_osq", [128, 512], BF16)
            orr = sb("b_orr", [128, 512])
            ao = [sb("b_ao%d" % i, [128, 512], BF16) for i in range(2)]
            sps = [g.ps("b_sps%d" % i, [128, 512], F32, st) for i in range(3)]
            ops_ = [g.ps("b_ops%d" % i, [128, 512], F32, st) for i in range(2)]
            sums = [g.ps("b_sum%d" % i, [128, 512], F32, st) for i in range(2)]
            oss = g.ps("b_oss", [128, 512], F32, st)
            hn = 0
            ei = 0
            an = 0
            for sq_ in range(2):
                cc = slice(sq_ * 256, sq_ * 256 + 256)
                lc = slice(512 + sq_ * 4096, 512 + (sq_ + 1) * 4096)
                for hd in range(8):
                    K_, Q_, V_ = Kh[hn % 2], Qh[hn % 2], Vh[hn % 2]
                    Kn, Qn, Vn = "b_K%d" % (hn % 2), "b_Q%d" % (hn % 2), "b_V%d" % (hn % 2)
                    hn += 1
                    hr = slice(hd * 128, (hd + 1) * 128)
                    S.dma("sync", K_[:, 0:256], g.KT[hr, cc], writes=[Kn])
                    S.dma("sync", K_[:, 256:4352], g.KT[hr, lc], writes=[Kn])
                    S.dma("sync", Q_[:, 0:256], g.QT[hr, cc], writes=[Qn])
                    S.dma("sync", Q_[:, 256:4352], g.QT[hr, lc], writes=[Qn])
                    S.dma("sync", V_[:, 0:2, :], g.VTOK[cc, hr].rearrange("(kt p) v -> p kt v", p=128), writes=[Vn])
                    S.dma("sync", V_[:, 2:34, :], g.VTOK[lc, hr].rearrange("(kt p) v -> p kt v", p=128), writes=[Vn])
                    for qt in range(9):
                        if qt == 0:
                            q0, N, nkt = 0, 256, 2
                        else:
                            q0, N, nkt = 256 + (qt - 1) * 512, 512, 34
                        for comp in range(2):
                            cr = slice(comp * 64, (comp + 1) * 64)
                            o_ps, o_n = ops_[comp], "b_ops%d" % comp
                            s_ps, s_n = sums[comp], "b_sum%d" % comp
                            pend = {}
                            for it in range(nkt + 2):
                                if it < nkt:
                                    kt = it
                                    sp_, spn = sps[ei % 3], "b_sps%d" % (ei % 3)
                                    p_, ptn = pt[ei % 3], "b_pt%d" % (ei % 3)
                                    ei += 1
                                    S.op("tensor", lambda e, sp_=sp_, K_=K_, Q_=Q_, cr=cr, kt=kt, q0=q0, N=N: e.matmul(
                                        sp_[:, :N], lhsT=K_[cr, kt * 128:(kt + 1) * 128], rhs=Q_[cr, q0:q0 + N], start=True, stop=True), [Kn, Qn], [spn])
                                    act(p_[:, :N], sp_[:, :N], AF.Exp, [spn], [ptn])
                                    pend[kt] = (p_, ptn)
                                if it >= 2:
                                    kt = it - 2
                                    p_, ptn = pend.pop(kt)
                                    S.op("tensor", lambda e, o_ps=o_ps, V_=V_, kt=kt, p_=p_, N=N, nkt=nkt: e.matmul(
                                        o_ps[:, :N], lhsT=V_[:, kt, :], rhs=p_[:, :N], start=(kt == 0), stop=(kt == nkt - 1)), [Vn, ptn], [o_n])
                                    S.op("tensor", lambda e, s_ps=s_ps, p_=p_, N=N, kt=kt, nkt=nkt: e.matmul(
                                        s_ps[:, :N], lhsT=g.ones_bf[:], rhs=p_[:, :N], start=(kt == 0), stop=(kt == nkt - 1)), ["ones_bf", ptn], [s_n])
                            V(lambda e, s_ps=s_ps, N=N: e.reciprocal(out=rsum[:, :N], in_=s_ps[:, :N]), [s_n], ["rsum"])
                            tt(oc_[comp][:, :N], o_ps[:, :N], rsum[:, :N], ALU.mult, [o_n, "rsum"], ["b_oc%d" % comp])
                        V(lambda e, N=N: e.scalar_tensor_tensor(out=oc_[0][:, :N], in0=oc_[1][:, :N], scalar=nlam[:, 0:1], in1=oc_[0][:, :N],
                                                                op0=ALU.mult, op1=ALU.add), ["b_oc0", "b_oc1", "nlam"], ["b_oc0"])
                        act(osq[:, :N], oc_[0][:, :N], AF.Square, ["b_oc0"], ["osq"])
                        S.op("tensor", lambda e, N=N: e.matmul(oss[:, :N], lhsT=g.ones_bf[:], rhs=osq[:, :N], start=True, stop=True), ["osq", "ones_bf"], ["oss"])
                        V(lambda e, N=N: e.tensor_scalar(out=orr[:, :N], in0=oss[:, :N], scalar1=1.0 / 128, scalar2=EPS, op0=ALU.mult, op1=ALU.add),
                          ["oss"], ["orr"])
                        act(orr[:, :N], orr[:, :N], AF.Sqrt, ["orr"], ["orr"])
                        V(lambda e, N=N: e.reciprocal(out=orr[:, :N], in_=orr[:, :N]), ["orr"], ["orr"])
                        a_, a_n = ao[an % 2], "b_ao%d" % (an % 2)
                        an += 1
                        V(lambda e, N=N, a_=a_: e.scalar_tensor_tensor(out=a_[:, :N], in0=oc_[0][:, :N], scalar=gv[:, 2:3], in1=orr[:, :N],
                                                                      op0=ALU.mult, op1=ALU.mult), ["b_oc0", "orr", "gv"], [a_n])
                        if qt == 0:
                            S.dma("sync", g.GT[hr, cc], a_[:, :256], reads=[a_n], writes=[("AT", sq_, hd, qt)])
                        else:
                            c0 = 512 + sq_ * 4096 + (qt - 1) * 512
                            S.dma("sync", g.GT[hr, c0:c0 + 512], a_[:, :512], reads=[a_n], writes=[("AT", sq_, hd, qt)])
            S.barrier()

        out_proj_residual(g, l, g.daow, D, last, "o")


def out_proj_residual(g, l, wdram, kdim, last, tag):
    nc, S = g.nc, g.S
    XTv = g.XT.rearrange("(c p) t -> p c t", p=128)
    nk = kdim // 128
    src = g.GT if kdim == D else g.GT2
    SRCv = src.rearrange("(c p) t -> p c t", p=128)
    with ExitStack() as st:
        sb = lambda n, s_, d=F32: g.sb(n, s_, d, st)
        w = sb(tag + "_w", [128, nk, D], BF16)
        S.op("gpsimd", lambda e: e.dma_start(out=w[:], in_=wdram.rearrange("(kc p) n -> p kc n", p=128)), [], [tag + "_w"], dma=True)
        xt = [sb(tag + "_xt%d" % i, [128, NCH, 512]) for i in range(2)]
        ai = [sb(tag + "_ai%d" % i, [128, nk, 512], BF16) for i in range(2)]
        po = [g.ps(tag + "_po%d" % i, [128, 512], F32, st) for i in range(2)]
        tiles = list(range(1 if last else 0, NT))
        for jn, j in enumerate(tiles):
            x, xn = xt[jn % 2], tag + "_xt%d" % (jn % 2)
            a, an = ai[jn % 2], tag + "_ai%d" % (jn % 2)
            stream = stream_of_tile(j)
            S.dma("sync", x[:], XTv[:, :, j * 512:(j + 1) * 512], reads=[("XT", j)], writes=[xn])
            S.dma("sync", a[:], SRCv[:, :, j * 512:(j + 1) * 512], reads=[], writes=[an])
            for oc in range(NCH):
                q = oc % 2
                for kc in range(nk):
                    S.op("tensor", lambda e, oc=oc, kc=kc, q=q, a=a: e.matmul(po[q][:], lhsT=w[:, kc, oc * 128:(oc + 1) * 128], rhs=a[:, kc, :],
                                                                             start=(kc == 0), stop=(kc == nk - 1)), [tag + "_w", an], [tag + "_po%d" % q])
                S.op("vector", lambda e, oc=oc, q=q, x=x, stream=stream: e.scalar_tensor_tensor(
                    out=x[:, oc, :], in0=po[q][:], scalar=g.mods[l][:, 2, oc, stream:stream + 1], in1=x[:, oc, :], op0=ALU.mult, op1=ALU.add),
                    [tag + "_po%d" % q, xn, ("mods", l)], [xn])
            S.dma("sync", XTv[:, :, j * 512:(j + 1) * 512], x[:], reads=[xn], writes=[("XT", j)])
        S.barrier()


def host_consts():
    cst = np.zeros((128, 6, 128), np.float32)
    cst[:, 0, :] = np.eye(128)
    cst[:, 1, :] = np.triu(np.ones((128, 128)), 1)
    cst[:, 2, :] = 1.0
    cst[:, 3, :] = np.arange(128)[None, :]
    cst[:, 4, :] = np.arange(128)[:, None]
    cblk = np.zeros((128, 128), np.float32)
    cblk[:, :] = (np.arange(128) * BLK)[None, :]
    cblk[:, 100:118] = (np.arange(18) * BLK)[None, :]
    return cst, cblk


def fm(v):
    v = np.asarray(v, np.float32)
    lead = v.shape[:-1]
    n = v.shape[-1] // 128
    return np.ascontiguousarray(np.swapaxes(v.reshape(lead + (n, 128)), -1, -2))


def make_s5_inputs(inp):
    out = {}
    are, aim, ldt = inp["s5_a_re"], inp["s5_a_im"], inp["s5_log_dt"]
    ldtb = np.broadcast_to(ldt[..., None], are.shape)
    prm = np.stack([are, aim, ldtb], -1).astype(np.float32)
    pB = prm.reshape(2, 2, 32, 2, 64, 3).transpose(0, 3, 4, 1, 2, 5).reshape(2, 128, 2, 32, 3)
    out["s5B"] = np.ascontiguousarray(pB)
    pA = prm.reshape(2, 2, 8, 8, 64, 3)
    pA = np.broadcast_to(pA[:, :, :, :, None], (2, 2, 8, 8, 16, 64, 3))
    out["s5A"] = np.ascontiguousarray(pA.transpose(0, 3, 4, 1, 2, 5, 6).reshape(2, 128, 2, 8, 64, 3))
    b = np.stack([inp["s5_b_re"], inp["s5_b_im"]], 2).astype(np.float32)
    c = np.stack([inp["s5_c_re"], inp["s5_c_im"]], 2).astype(np.float32)
    BzB = np.zeros((2, 2, 64, 2, 2, 32, 128), np.float32)
    CzB = np.zeros_like(BzB)
    for q in range(32):
        for gp in range(2):
            c0 = 32 * (q % 4) + 16 * gp
            BzB[:, gp, :, :, :, q, c0:c0 + 16] = b[:, :, :, 2 * q + gp].transpose(0, 3, 1, 2, 4)
            CzB[:, gp, :, :, :, q, c0:c0 + 16] = c[:, :, :, 2 * q + gp].transpose(0, 4, 1, 2, 3)
    out["s5BzB"] = BzB.reshape(2, 128, 2, 2, 32, 128)
    out["s5CzB"] = CzB.reshape(2, 128, 2, 2, 32, 128)
    BzA = np.zeros((2, 8, 16, 2, 2, 8, 8, 64), np.float32)
    for ct in range(8):
        for g8 in range(8):
            BzA[:, g8, :, :, :, ct, g8, :] = b[:, :, :, 8 * ct + g8].transpose(0, 4, 1, 2, 3)
    out["s5BzA"] = BzA.reshape(2, 128, 2, 2, 8, 8, 64)
    out["s5d"] = fm(inp["s5_d"])
    out["s5gw"] = np.ascontiguousarray(inp["s5_glu_w"], dtype=np.float32)
    out["s5gb"] = fm(inp["s5_glu_b"])
    s5c = np.zeros((128, 16 + NBK), np.float32)
    s5c[:, 0:9] = np.arange(9)[None, :]
    s5c[:, 16:] = np.arange(NBK)[None, :]
    out["s5c"] = s5c
    return out


def make_ssd_inputs(inp):
    out = {}
    out["ssd_inw"] = np.ascontiguousarray(inp["ssd_in_w"][0], dtype=np.float32)
    cw = np.zeros((128, 32, 6), np.float32)
    w = inp["ssd_conv_w"][0]
    b = inp["ssd_conv_b"][0]
    cw[:, :, 0:5] = w.T.reshape(32, 128, 5).transpose(1, 0, 2)
    cw[:, :, 5] = b.reshape(32, 128).T
    out["ssd_cw"] = cw
    hp = np.zeros((64, 4), np.float32)
    hp[:, 0] = inp["ssd_dt_bias"][0].reshape(64)
    hp[:, 1] = inp["ssd_a_log"][0].reshape(64)
    out["ssd_hp"] = hp
    m = np.zeros((128, 2, 4, 512), np.float32)
    s_ = np.arange(128)[:, None]
    t = np.arange(512)[None, :]
    for o in range(4):
        m[:, 0, o, :] = (t >= 128 * o + s_)
        m[:, 1, o, :] = (t <= 128 * o + s_)
    out["ssd_mask"] = m
    fv = np.zeros((128, 16, 2), np.float32)
    dsk = np.repeat(inp["ssd_d"][0], 64)
    fv[:, :, 0] = dsk.reshape(16, 128).T
    fv[:, :, 1] = inp["ssd_norm_g"][0].reshape(16, 128).T
    out["ssd_fv"] = fv
    out["ssd_ow"] = np.ascontiguousarray(inp["ssd_out_w"][0], dtype=np.float32)
    return out


def make_da_inputs(inp):
    out = {}
    out["daqkv"] = np.ascontiguousarray(inp["da_qkv_w"][0], dtype=np.float32)
    out["daow"] = np.ascontiguousarray(inp["da_out_w"][0], dtype=np.float32)
    gv = np.zeros((128, 4), np.float32)
    gv[:, 0] = np.tile(inp["da_q_g"][0], 2)
    gv[:, 1] = np.tile(inp["da_k_g"][0], 2)
    gv[:, 2] = inp["da_sub_g"][0]
    out["dagv"] = gv
    out["dalam"] = np.ascontiguousarray(inp["da_lam"][0].T, dtype=np.float32)
    dac = np.zeros((128, 3, 128), np.float32)
    for m in range(128):
        if (m % 32) < 16:
            dac[m + 16, 0, m] = -1.0
        else:
            dac[m - 16, 0, m] = 1.0
    dac[:, 1, :] = (np.arange(128)[:, None] // 64 == np.arange(128)[None, :] // 64)
    out["dac"] = dac
    t = np.arange(4096)
    row, col = (t // 64).astype(np.float64), (t % 64).astype(np.float64)
    inv = (np.float32(10000.0) ** (-np.arange(16, dtype=np.float32) / np.float32(16))).astype(np.float64)
    ang = np.zeros((128, 4096))
    for p in range(128):
        dd = p % 64
        f = dd % 16
        ang[p] = (row if dd < 32 else col) * inv[f]
    out["ropeC"] = np.cos(ang).astype(np.float32)
    out["ropeS"] = np.sin(ang).astype(np.float32)
    return out


def make_in_maps(inp, layers, core_ids=range(8), stub_moe=False):
    L = list(layers)
    cst, cblk = host_consts()
    shared = {
        "mod_w": np.ascontiguousarray(inp["mod_w"][L]),
        "mod_bT": fm(inp["mod_b"][L]),
        "n1T": fm(inp["norm1_g"][L]),
        "n2T": fm(inp["norm2_g"][L]),
        "rwT": np.ascontiguousarray(inp["moe_router_w"][L].reshape(len(L), NCH, 128, NEXP).transpose(0, 2, 1, 3)),
        "rb_bc": np.ascontiguousarray(np.broadcast_to(inp["moe_router_b"][L][:, None, :], (len(L), 128, NEXP))),
        "gu_w": np.ascontiguousarray(inp["moe_gu_w"][L].reshape(len(L), NEXP * D, 2 * D)),
        "dn_w": np.ascontiguousarray(inp["moe_dn_w"][L].reshape(len(L), NEXP * D, D)),
        "gu_bT": np.ascontiguousarray(inp["moe_gu_b"][L].reshape(len(L), NEXP, 16, 128).transpose(0, 1, 3, 2).reshape(len(L), NEXP * 128, 16)),
        "dn_b": np.ascontiguousarray(inp["moe_dn_b"][L]),
        "cst": cst, "cblk": cblk,
    }
    if any(l % 3 == 0 for l in L):
        shared.update(make_s5_inputs(inp))
    if any(l % 3 == 2 for l in L):
        shared.update(make_da_inputs(inp))
    if any(l % 3 == 1 for l in L):
        shared.update(make_ssd_inputs(inp))
    if stub_moe:
        shared["gu_w"] = shared["gu_w"][:, :8].copy()
        shared["dn_w"] = shared["dn_w"][:, :8].copy()
    maps = []
    for c in core_ids:
        b0, b1 = 2 * c, 2 * c + 1
        x = inp["x"]
        ctx = inp["ctx"]
        xT = np.concatenate([ctx[b0].T, ctx[b1].T, x[b0].T, x[b1].T], axis=1)
        cvec = np.stack([inp["c_ctx"], inp["c"][b0], inp["c"][b1]], axis=-1)
        cT = np.ascontiguousarray(cvec.reshape(NCH, 128, 3).transpose(1, 0, 2))
        m = dict(shared)
        m["xT0"] = np.ascontiguousarray(xT, dtype=np.float32)
        m["cT"] = cT.astype(np.float32)
        maps.append(m)
    return maps


def kernel(**inputs):
    inp = {k: np.asarray(v) for k, v in inputs.items()}
    layers = [0, 1, 2, 3]
    nc = build_program(layers, 4)
    maps = make_in_maps(inp, layers)
    res = run_bass_kernel_spmd(nc, maps, core_ids=list(range(8)))
    out = np.zeros((16, 4096, D), np.float32)
    for c in range(8):
        o = res.results[c]["outT"]
        out[2 * c] = o[:, :4096].T
        out[2 * c + 1] = o[:, 4096:].T
    return out
```

```python
import numpy as np
import ml_dtypes
import concourse.bass as bass
import concourse.mybir as mybir
from concourse.bass_utils import run_bass_kernel_spmd
from contextlib import ExitStack

F32 = mybir.dt.float32
BF16 = mybir.dt.bfloat16
I32 = mybir.dt.int32
AF = mybir.ActivationFunctionType
ALU = mybir.AluOpType
AX = mybir.AxisListType

ENGS = ["sync", "scalar", "vector", "gpsimd", "tensor"]
NSLOT = {"sync": 8, "scalar": 4, "vector": 2, "gpsimd": 8, "tensor": 2}
SAME_ENGINE_SYNC = {"sync": True, "scalar": True, "vector": True, "gpsimd": True, "tensor": False}
EPOCH = 24000

D = 1024
NCH = 8
TCORE = 8704
NT = 17
EPS = 1e-6
NEXP = 32
BLK = 512


class Op:
    __slots__ = ("eng", "fn", "dma", "deps", "sem", "val", "prev")

    def __init__(self, eng, fn, dma):
        self.eng = eng
        self.fn = fn
        self.dma = dma
        self.deps = ()
        self.sem = None
        self.val = 0
        self.prev = None


class Sched:
    def __init__(self, nc):
        self.nc = nc
        self.eng_ops = {e: [] for e in ENGS}
        self.last_w = {}
        self.readers = {}
        self.nsem = 0
        self.csem = {e: [self._newsem(), 0] for e in ENGS}
        self.dslot = {e: [[self._newsem(), 0, None] for _ in range(NSLOT[e])] for e in ENGS}
        self.dma_rr = {e: 0 for e in ENGS}
        self.last_op = {e: None for e in ENGS}

    def _newsem(self):
        self.nsem += 1
        return self.nsem - 1

    def op(self, eng, fn, reads=(), writes=(), dma=False):
        o = Op(eng, fn, dma)
        deps = {}
        for t in reads:
            w = self.last_w.get(t)
            if w is not None:
                deps[id(w)] = w
        for t in writes:
            w = self.last_w.get(t)
            if w is not None:
                deps[id(w)] = w
            for r in self.readers.get(t, ()):
                deps[id(r)] = r
        o.deps = tuple(deps.values())
        for t in writes:
            self.last_w[t] = o
            self.readers[t] = []
        wset = set(writes)
        for t in reads:
            if t not in wset:
                self.readers.setdefault(t, []).append(o)
        if dma:
            s = self.dma_rr[eng]
            self.dma_rr[eng] = (s + 1) % NSLOT[eng]
            slot = self.dslot[eng][s]
            o.prev = slot[2]
            if slot[1] + 16 > EPOCH:
                slot[0] = self._newsem()
                slot[1] = 0
            slot[1] += 16
            o.sem, o.val = slot[0], slot[1]
            slot[2] = o
        else:
            c = self.csem[eng]
            if c[1] + 1 > EPOCH:
                c[0] = self._newsem()
                c[1] = 0
            c[1] += 1
            o.sem, o.val = c[0], c[1]
            self.last_op[eng] = o
        self.eng_ops[eng].append(o)
        return o

    def dma(self, eng, out, in_, reads=(), writes=(), **kw):
        return self.op(eng, lambda e: e.dma_start(out=out, in_=in_, **kw), reads, writes, dma=True)

    def barrier(self):
        deps = []
        for e in ENGS:
            if self.last_op[e] is not None:
                deps.append(self.last_op[e])
            for slot in self.dslot[e]:
                if slot[2] is not None:
                    deps.append(slot[2])
        for e in ENGS:
            b = Op(e, None, False)
            b.deps = tuple(deps)
            self.eng_ops[e].append(b)

    def emit(self):
        nc = self.nc
        with ExitStack() as es:
            sems = [es.enter_context(nc.semaphore("s%d" % i)) for i in range(self.nsem)]
            block = es.enter_context(nc.Block())

            def run(engname, e):
                waited = {}

                def wait(d):
                    if waited.get(d.sem, 0) >= d.val:
                        return
                    waited[d.sem] = d.val
                    e.wait_ge(sems[d.sem], d.val)

                for o in self.eng_ops[engname]:
                    if o.prev is not None:
                        wait(o.prev)
                    for d in o.deps:
                        if d.eng == engname and not d.dma and not SAME_ENGINE_SYNC[engname] and o.fn is not None:
                            continue
                        wait(d)
                    if o.fn is None:
                        continue
                    ins = o.fn(e)
                    ins.then_inc(sems[o.sem], 16 if o.dma else 1)

            @block.sync
            def _(e):
                run("sync", e)

            @block.scalar
            def _(e):
                run("scalar", e)

            @block.vector
            def _(e):
                run("vector", e)

            @block.gpsimd
            def _(e):
                run("gpsimd", e)

            @block.tensor
            def _(e):
                run("tensor", e)


def bc(ap, shape, axis):
    return ap.unsqueeze(axis).to_broadcast(shape)


class Ctx:
    pass


def stream_of_tile(j):
    return 0 if j == 0 else (1 if j <= 8 else 2)


def build_program(layers, n_layers_weights, phases_per_layer=None, dbg=None):
    nc = bass.Bass("TRN2", target_bir_lowering=False)
    S = Sched(nc)
    g = Ctx()
    g.nc, g.S = nc, S
    g.lidx = {l: i for i, l in enumerate(layers)}
    NLW = n_layers_weights

    def din(name, shape, dt=F32):
        return nc.dram_tensor(name, shape, dt, kind="ExternalInput").ap()

    def dscr(name, shape, dt=F32):
        return nc.dram_tensor(name, shape, dt, kind="Internal").ap()

    g.xT0 = din("xT0", [D, TCORE])
    g.cT = din("cT", [128, NCH, 3])
    g.mod_w = din("mod_w", [NLW, D, 6 * D])
    g.mod_bT = din("mod_bT", [NLW, 128, 48])
    g.n1T = din("n1T", [NLW, 128, NCH])
    g.n2T = din("n2T", [NLW, 128, NCH])
    g.rwT = din("rwT", [NLW, 128, NCH, NEXP])
    g.rb_bc = din("rb_bc", [NLW, 128, NEXP])
    if phases_per_layer is not None and "moe" not in phases_per_layer:
        g.gu_w = din("gu_w", [NLW, 8, 2 * D])
        g.dn_w = din("dn_w", [NLW, 8, D])
    else:
        g.gu_w = din("gu_w", [NLW, NEXP * D, 2 * D])
        g.dn_w = din("dn_w", [NLW, NEXP * D, D])
    g.gu_bT = din("gu_bT", [NLW, NEXP * 128, 16])
    g.dn_b = din("dn_b", [NLW, NEXP, D])
    g.cst = din("cst", [128, 6, 128])
    g.cblk = din("cblk", [128, 128])
    g.outT = nc.dram_tensor("outT", [D, 8192], F32, kind="ExternalOutput").ap()

    ns5 = len([l for l in layers if l % 3 == 0])
    if ns5:
        g.s5B = din("s5B", [2, 128, 2, 32, 3])
        g.s5A = din("s5A", [2, 128, 2, 8, 64, 3])
        g.s5BzB = din("s5BzB", [2, 128, 2, 2, 32, 128])
        g.s5CzB = din("s5CzB", [2, 128, 2, 2, 32, 128])
        g.s5BzA = din("s5BzA", [2, 128, 2, 2, 8, 8, 64])
        g.s5d = din("s5d", [2, 128, NCH])
        g.s5gw = din("s5gw", [2, D, 2 * D])
        g.s5gb = din("s5gb", [2, 128, 16])
        g.s5c = din("s5c", [128, 16 + NBK])
    if any(l % 3 == 2 for l in layers):
        g.daqkv = din("daqkv", [D, 3 * D])
        g.daow = din("daow", [D, D])
        g.dagv = din("dagv", [128, 4])
        g.dalam = din("dalam", [64, 4])
        g.dac = din("dac", [128, 3, 128])
        g.ropeC = din("ropeC", [128, 4096])
        g.ropeS = din("ropeS", [128, 4096])
        g.QT = dscr("QT", [D, TCORE], BF16)
        g.KT = dscr("KT", [D, TCORE], BF16)
        g.VTOK = dscr("VTOK", [TCORE, D], BF16)
    if any(l % 3 == 1 for l in layers):
        g.ssd_inw = din("ssd_inw", [D, 6208])
        g.ssd_cw = din("ssd_cw", [128, 32, 6])
        g.ssd_hp = din("ssd_hp", [64, 4])
        g.ssd_mask = din("ssd_mask", [128, 2, 4, 512])
        g.ssd_fv = din("ssd_fv", [128, 16, 2])
        g.ssd_ow = din("ssd_ow", [2 * D, D])
        g.SZ = dscr("SZ", [2 * D, TCORE], BF16)
        g.XBCp = dscr("XBCp", [4 * D, TCORE], BF16)
        g.DTr = dscr("DTr", [64, TCORE], F32)
        g.XFo = dscr("XFo", [2 * D, TCORE], BF16)
        g.BCo = dscr("BCo", [2 * D, TCORE], BF16)
        g.XTOK = dscr("XTOK", [TCORE, 2 * D], BF16)
        g.YT = dscr("YT", [2 * D, TCORE], BF16)
    g.GT2 = dscr("GT2", [2 * D, TCORE], BF16)
    g.HT = dscr("HT", [D, TCORE], BF16)
    g.GT = dscr("GT", [D, TCORE], BF16)
    g.XT = dscr("XT", [D, TCORE])
    g.Htok = dscr("Htok", [TCORE, D], BF16)
    NSLOTS = (TCORE * 4 // BLK + NEXP) * BLK
    g.NB = NSLOTS // BLK
    g.Hs = dscr("Hs", [NSLOTS, D], BF16)
    g.Ys = dscr("Ys", [NSLOTS, D], F32)

    with ExitStack() as top:
        uid = [0]

        def sb(name, shape, dt=F32, stack=top):
            uid[0] += 1
            return stack.enter_context(nc.sbuf_tensor("%s_%d" % (name, uid[0]), shape, dt))

        def ps(name, shape, dt=F32, stack=top):
            uid[0] += 1
            return stack.enter_context(nc.psum_tensor("%s_%d" % (name, uid[0]), shape, dt))

        g.sb, g.ps = sb, ps
        g.cst_t = sb("cst_t", [128, 6, 128])
        g.cblk_t = sb("cblk_t", [128, 128])
        g.ident_bf = sb("ident_bf", [128, 128], BF16)
        g.ones_bf = sb("ones_bf", [128, 128], BF16)
        S.dma("sync", g.cst_t[:], g.cst[:, :, :], writes=["cst"])
        S.dma("sync", g.cblk_t[:], g.cblk[:, :], writes=["cblk"])
        S.op("vector", lambda e: e.tensor_copy(out=g.ident_bf[:], in_=g.cst_t[:, 0, :]), ["cst"], ["ident_bf"])
        S.op("vector", lambda e: e.tensor_copy(out=g.ones_bf[:], in_=g.cst_t[:, 2, :]), ["cst"], ["ones_bf"])
        g.ident_f = g.cst_t[:, 0, :]
        g.tri_f = g.cst_t[:, 1, :]
        g.ones_f = g.cst_t[:, 2, :]
        g.iota_row = g.cst_t[:, 3, :]
        g.iota_p = g.cst_t[:, 4, 0:1]
        g.mods = {}
        g.es1 = {}
        g.es2 = {}
        for l in layers:
            g.mods[l] = sb("mods%d" % l, [128, 6, NCH, 3])
            g.es1[l] = sb("es1_%d" % l, [128, NCH, 3])
            g.es2[l] = sb("es2_%d" % l, [128, NCH, 3])

        prologue(g, layers)
        with ExitStack() as st:
            buf = [sb("cpx%d" % i, [128, NCH, 512], F32, st) for i in range(2)]
            for j in range(NT):
                b = buf[j % 2]
                cols = slice(j * 512, (j + 1) * 512)
                S.dma("sync", b[:], g.xT0.rearrange("(c p) t -> p c t", p=128)[:, :, cols], writes=["cpx%d" % (j % 2)])
                S.dma("scalar", g.XT.rearrange("(c p) t -> p c t", p=128)[:, :, cols], b[:], reads=["cpx%d" % (j % 2)],
                      writes=[("XT", j)])
            S.barrier()

        for l in layers:
            ph = phases_per_layer or ("mix", "moe")
            if "mix" in ph:
                kind = l % 3
                if kind == 0:
                    s5_layer(g, l)
                elif kind == 1:
                    ssd_layer(g, l)
                else:
                    da_layer(g, l)
            if "moe" in ph:
                moe_layer(g, l, last=(l == 3))

        with ExitStack() as st:
            buf = [sb("cpo%d" % i, [128, NCH, 512], F32, st) for i in range(2)]
            outs = []
            for j in range(1, NT):
                b = buf[j % 2]
                cols = slice(j * 512, (j + 1) * 512)
                S.dma("sync", b[:], g.XT.rearrange("(c p) t -> p c t", p=128)[:, :, cols], reads=[("XT", j)],
                      writes=["cpo%d" % (j % 2)])
                S.dma("scalar", g.outT.rearrange("(c p) t -> p c t", p=128)[:, :, (j - 1) * 512:j * 512], b[:],
                      reads=["cpo%d" % (j % 2)], writes=[("out", j)])
            S.barrier()
        S.emit()
    return nc


def prologue(g, layers):
    nc, S = g.nc, g.S
    with ExitStack() as st:
        sb = lambda n, s, d=F32: g.sb(n, s, d, st)
        ct = sb("ct", [128, NCH, 3])
        sc = sb("sc", [128, NCH, 3])
        mw = [sb("mw%d" % i, [128, NCH, 1024]) for i in range(2)]
        mb = sb("mb", [128, 48])
        gn = sb("gn", [128, 2, NCH])
        pm = g.ps("pm", [128, 8, 4], F32, st)
        S.dma("sync", ct[:], g.cT[:, :, :], writes=["ct"])
        S.op("scalar", lambda e: e.activation(out=sc[:], in_=ct[:], func=AF.Silu), ["ct"], ["sc"])
        k = 0
        for li, l in enumerate(layers):
            S.dma("sync", mb[:], g.mod_bT[li, :, :], writes=["mb"])
            S.dma("sync", gn[:, 0, :], g.n1T[li, :, :], writes=["gn0"])
            S.dma("sync", gn[:, 1, :], g.n2T[li, :, :], writes=["gn1"])
            for part in range(6):
                w = mw[k % 2]
                wn = "mw%d" % (k % 2)
                k += 1
                S.dma("sync", w[:], g.mod_w[li].rearrange("(kc p) n -> p kc n", p=128)[:, :, part * 1024:(part + 1) * 1024],
                      writes=[wn])
                for oc in range(8):
                    for kc in range(NCH):
                        S.op("tensor", lambda e, w=w, oc=oc, kc=kc: e.matmul(
                            pm[:, oc, 0:3], lhsT=w[:, kc, oc * 128:(oc + 1) * 128], rhs=sc[:, kc, :],
                            start=(kc == 0), stop=(kc == NCH - 1)), [wn, "sc"], ["pm"])
                S.op("vector", lambda e, l=l, part=part: e.tensor_tensor(
                    out=g.mods[l][:, part, :, :], in0=pm[:, :, 0:3],
                    in1=bc(mb[:, part * 8:(part + 1) * 8], [128, 8, 3], 2), op=ALU.add), ["pm", "mb"], [("mods", l)])
            S.op("vector", lambda e, l=l: e.scalar_tensor_tensor(
                out=g.es1[l][:], in0=g.mods[l][:, 1, :, :], scalar=1.0, in1=bc(gn[:, 0, :], [128, NCH, 3], 2),
                op0=ALU.add, op1=ALU.mult), [("mods", l), "gn0"], [("es1", l)])
            S.op("vector", lambda e, l=l: e.scalar_tensor_tensor(
                out=g.es2[l][:], in0=g.mods[l][:, 4, :, :], scalar=1.0, in1=bc(gn[:, 1, :], [128, NCH, 3], 2),
                op0=ALU.add, op1=ALU.mult), [("mods", l), "gn1"], [("es2", l)])
        S.barrier()


def normmod(g, l, which, xt, xtn, sq, ssp, rstd, tmp, h32, hbf, stream, N=512, tag="", hbfn=None):
    S = g.S
    hbfn = hbfn or ("hbf" + tag)
    es = g.es1[l] if which == 1 else g.es2[l]
    esn = ("es1", l) if which == 1 else ("es2", l)
    shp = 0 if which == 1 else 3
    S.op("scalar", lambda e: e.activation(out=sq[:, :, :N], in_=xt[:, :, :N], func=AF.Square), [xtn], ["sq" + tag])
    for c in range(NCH):
        S.op("tensor", lambda e, c=c: e.matmul(ssp[:, :N], lhsT=g.ones_bf[:], rhs=sq[:, c, :N], start=(c == 0),
                                              stop=(c == NCH - 1)), ["sq" + tag, "ones_bf"], ["ssp" + tag])
    S.op("vector", lambda e: e.tensor_scalar(out=rstd[:, :N], in0=ssp[:, :N], scalar1=1.0 / D, scalar2=EPS, op0=ALU.mult,
                                             op1=ALU.add), ["ssp" + tag], ["rstd" + tag])
    S.op("scalar", lambda e: e.activation(out=rstd[:, :N], in_=rstd[:, :N], func=AF.Sqrt), ["rstd" + tag], ["rstd" + tag])
    S.op("vector", lambda e: e.reciprocal(out=rstd[:, :N], in_=rstd[:, :N]), ["rstd" + tag], ["rstd" + tag])
    for c in range(NCH):
        S.op("vector", lambda e, c=c: e.scalar_tensor_tensor(
            out=tmp[:, c, :N], in0=xt[:, c, :N], scalar=es[:, c, stream:stream + 1], in1=rstd[:, :N], op0=ALU.mult,
            op1=ALU.mult), [xtn, "rstd" + tag, esn], ["nm_tmp" + tag])
        if h32 is not None:
            S.op("scalar", lambda e, c=c: e.activation(out=h32[:, c, :N], in_=tmp[:, c, :N], func=AF.Identity,
                                                       bias=g.mods[l][:, shp, c, stream:stream + 1], scale=1.0),
                 ["nm_tmp" + tag, ("mods", l)], ["h32" + tag])
            if hbf is not None:
                S.op("gpsimd", lambda e, c=c: e.tensor_copy(out=hbf[:, c, :N], in_=h32[:, c, :N]), ["h32" + tag], [hbfn])
        else:
            S.op("scalar", lambda e, c=c: e.activation(out=hbf[:, c, :N], in_=tmp[:, c, :N], func=AF.Identity,
                                                       bias=g.mods[l][:, shp, c, stream:stream + 1], scale=1.0),
                 ["nm_tmp" + tag, ("mods", l)], [hbfn])


def moe_layer(g, l, last):
    nc, S = g.nc, g.S
    li = g.lidx[l]
    j0 = 1 if last else 0
    tiles = list(range(j0, NT))
    nsub = len(tiles) * 4
    NB = (nsub * 128 * 4) // BLK + NEXP
    XTv = g.XT.rearrange("(c p) t -> p c t", p=128)
    with ExitStack() as st:
        sb = lambda n, s, d=F32: g.sb(n, s, d, st)
        lg_all = sb("lg_all", [128, nsub, NEXP])
        rk_all = sb("rk_all", [128, nsub, NEXP])
        t8_all = sb("t8_all", [128, nsub, 8])
        gates = sb("gates", [128, nsub, 4])
        dest_f = sb("dest_f", [128, nsub, 4])
        dest_i = sb("dest_i", [128, nsub, 4], I32)
        macc = sb("macc", [128, NEXP])
        rw = sb("rw", [128, NCH, NEXP])
        rbb = sb("rbb", [128, NEXP])
        idxw = sb("idxw", [128, 128], I32)
        idxb = sb("idxb", [128, 128], I32)
        idxe = sb("idxe", [128, 128], I32)
        S.dma("sync", rw[:], g.rwT[li, :, :, :], writes=["rw"])
        S.dma("sync", rbb[:], g.rb_bc[li, :, :], writes=["rbb"])
        S.op("vector", lambda e: e.memset(macc[:], 0.0), [], ["macc"])

        with ExitStack() as sa:
            sba = lambda n, s, d=F32: g.sb(n, s, d, sa)
            xt = [sba("a_xt%d" % i, [128, NCH, 512]) for i in range(2)]
            sq = sba("a_sq", [128, NCH, 512], BF16)
            tmp = sba("a_tmp", [128, NCH, 512])
            h32 = sba("a_h32", [128, NCH, 512])
            hbf = sba("a_hbf", [128, NCH, 512], BF16)
            rstd = sba("a_rstd", [128, 512])
            hrow = [sba("a_hrow%d" % i, [128, D], BF16) for i in range(2)]
            lgt = sba("a_lgt", [128, NEXP])
            msk = sba("a_msk", [128, NEXP])
            nv0 = sba("a_nv0", [128, 1])
            ex = sba("a_ex", [128, 4])
            sme = sba("a_sme", [128, 1])
            ssp = g.ps("a_ssp", [128, 512], F32, sa)
            lgp = g.ps("a_lgp", [128, NEXP], F32, sa)
            rkp = g.ps("a_rkp", [128, NEXP], F32, sa)
            trp = [g.ps("a_trp%d" % i, [128, D], BF16, sa) for i in range(2)]
            si = 0
            for jn, j in enumerate(tiles):
                x = xt[jn % 2]
                xn = "a_xt%d" % (jn % 2)
                S.dma("sync", x[:], XTv[:, :, j * 512:(j + 1) * 512], reads=[("XT", j)], writes=[xn])
                normmod(g, l, 2, x, xn, sq, ssp, rstd, tmp, h32, hbf, stream_of_tile(j), tag="A")
                for s in range(4):
                    cs = slice(s * 128, (s + 1) * 128)
                    for kc in range(NCH):
                        S.op("tensor", lambda e, kc=kc, cs=cs: e.matmul(lgp[:], lhsT=h32[:, kc, cs], rhs=rw[:, kc, :],
                                                                      start=(kc == 0), stop=(kc == NCH - 1)),
                             ["h32A", "rw"], ["lgp"])
                    S.op("vector", lambda e, si=si: e.tensor_tensor(out=lg_all[:, si, :], in0=lgp[:], in1=rbb[:], op=ALU.add),
                         ["lgp", "rbb"], [("lg", si)])
                    S.op("vector", lambda e, si=si: e.max(out=t8_all[:, si, :], in_=lg_all[:, si, :]), [("lg", si)], [("t8", si)])
                    S.op("vector", lambda e, si=si: e.tensor_scalar(out=nv0[:], in0=t8_all[:, si, 0:1], scalar1=-1.0, scalar2=None,
                                                                    op0=ALU.mult), [("t8", si)], ["nv0"])
                    S.op("scalar", lambda e, si=si: e.activation(out=ex[:], in_=t8_all[:, si, 0:4], func=AF.Exp, bias=nv0[:, 0:1],
                                                                 scale=1.0, accum_out=sme[:, 0:1]), [("t8", si), "nv0"], ["ex", "sme"])
                    S.op("vector", lambda e: e.reciprocal(out=sme[:], in_=sme[:]), ["sme"], ["sme"])
                    S.op("vector", lambda e, si=si: e.tensor_scalar(out=gates[:, si, :], in0=ex[:], scalar1=sme[:, 0:1], scalar2=None,
                                                                    op0=ALU.mult), ["ex", "sme"], [("gates", si)])
                    S.op("vector", lambda e, si=si: e.tensor_scalar(out=msk[:], in0=lg_all[:, si, :], scalar1=t8_all[:, si, 3:4],
                                                                    scalar2=None, op0=ALU.is_ge), [("lg", si), ("t8", si)], ["msk"])
                    S.op("tensor", lambda e: e.matmul(rkp[:], lhsT=g.tri_f, rhs=msk[:], start=True, stop=False), ["msk", "cst"], ["rkp"])
                    S.op("tensor", lambda e: e.matmul(rkp[:], lhsT=g.ones_f, rhs=macc[:], start=False, stop=True), ["macc", "cst"], ["rkp"])
                    S.op("vector", lambda e, si=si: e.tensor_copy(out=rk_all[:, si, :], in_=rkp[:]), ["rkp"], [("rk", si)])
                    S.op("vector", lambda e: e.tensor_tensor(out=macc[:], in0=macc[:], in1=msk[:], op=ALU.add), ["macc", "msk"], ["macc"])
                    tp = trp[si % 2]
                    tpn = "a_trp%d" % (si % 2)
                    hr = hrow[si % 2]
                    hrn = "a_hrow%d" % (si % 2)
                    for c in range(NCH):
                        S.op("tensor", lambda e, c=c, cs=cs, tp=tp: e.transpose(tp[:, c * 128:(c + 1) * 128], hbf[:, c, cs], g.ident_bf[:]),
                             ["hbfA", "ident_bf"], [tpn])
                    S.op("scalar", lambda e, tp=tp, hr=hr: e.activation(out=hr[:], in_=tp[:], func=AF.Copy), [tpn], [hrn])
                    tok0 = j * 512 + s * 128
                    S.dma("sync", g.Htok[tok0:tok0 + 128, :], hr[:], reads=[hrn], writes=[("Htok", si)])
                    si += 1
            cnt = sba("a_cnt", [128, NEXP])
            cmp3 = sba("a_cmp3", [128, NEXP, 18])
            pad = sba("a_pad", [128, NEXP])
            pend = sba("a_pend", [128, NEXP])
            pstart = sba("a_pstart", [128, NEXP])
            onesr = sba("a_onesr", [128, NEXP])
            be = sba("a_be", [128, 128])
            bef = sba("a_bef", [128, 128])
            S.op("tensor", lambda e: e.matmul(rkp[:], lhsT=g.ones_f, rhs=macc[:], start=True, stop=True), ["macc", "cst"], ["rkp"])
            S.op("vector", lambda e: e.tensor_copy(out=cnt[:], in_=rkp[:]), ["rkp"], ["cnt"])
            S.op("vector", lambda e: e.tensor_tensor(out=cmp3[:], in0=bc(cnt[:], [128, NEXP, 18], 2),
                                                     in1=bc(g.cblk_t[:, 100:118], [128, NEXP, 18], 1), op=ALU.is_gt),
                 ["cnt", "cblk"], ["cmp3"])
            S.op("vector", lambda e: e.tensor_reduce(out=pad[:], in_=cmp3[:], axis=AX.X, op=ALU.add), ["cmp3"], ["pad"])
            S.op("vector", lambda e: e.tensor_scalar(out=pad[:], in0=pad[:], scalar1=float(BLK), scalar2=None, op0=ALU.mult), ["pad"], ["pad"])
            S.op("vector", lambda e: e.memset(onesr[:], 1.0), [], ["onesr"])
            S.op("vector", lambda e: e.tensor_tensor_scan(out=pend[:], data0=onesr[:], data1=pad[:], initial=0.0, op0=ALU.mult,
                                                          op1=ALU.add), ["onesr", "pad"], ["pend"])
            S.op("vector", lambda e: e.tensor_tensor(out=pstart[:], in0=pend[:], in1=pad[:], op=ALU.subtract), ["pend", "pad"], ["pstart"])
            S.op("vector", lambda e: e.memset(be[:], 0.0), [], ["be"])
            for ex_ in range(NEXP):
                S.op("vector", lambda e, ex_=ex_: e.scalar_tensor_tensor(out=be[:], in0=g.cblk_t[:], scalar=pend[:, ex_:ex_ + 1], in1=be[:],
                                                                        op0=ALU.is_ge, op1=ALU.add), ["cblk", "pend", "be"], ["be"])
            S.op("vector", lambda e: e.tensor_scalar(out=be[:], in0=be[:], scalar1=float(NEXP - 1), scalar2=None, op0=ALU.min), ["be"], ["be"])
            S.op("vector", lambda e: e.tensor_scalar(out=bef[:], in0=be[:], scalar1=float(D), scalar2=g.iota_p, op0=ALU.mult, op1=ALU.add),
                 ["be", "cst"], ["bef"])
            S.op("vector", lambda e: e.tensor_copy(out=idxw[:], in_=bef[:]), ["bef"], ["idxw"])
            S.op("vector", lambda e: e.tensor_scalar(out=bef[:], in0=be[:], scalar1=128.0, scalar2=g.iota_p, op0=ALU.mult, op1=ALU.add),
                 ["be", "cst"], ["bef"])
            S.op("vector", lambda e: e.tensor_copy(out=idxb[:], in_=bef[:]), ["bef"], ["idxb"])
            S.op("vector", lambda e: e.tensor_copy(out=idxe[:], in_=be[:]), ["be"], ["idxe"])
            dall = sba("a_dall", [128, nsub, NEXP])
            oh = sba("a_oh", [128, nsub, NEXP])
            allsi_lg = [("lg", i) for i in range(nsub)]
            allsi_rk = [("rk", i) for i in range(nsub)]
            allsi_t8 = [("t8", i) for i in range(nsub)]
            S.op("vector", lambda e: e.tensor_tensor(out=dall[:], in0=rk_all[:], in1=bc(pstart[:], [128, nsub, NEXP], 1), op=ALU.add),
                 allsi_rk + ["pstart"], ["dall"])
            for k in range(4):
                S.op("vector", lambda e, k=k: e.tensor_tensor(out=oh[:], in0=lg_all[:], in1=bc(t8_all[:, :, k], [128, nsub, NEXP], 2),
                                                              op=ALU.is_equal), allsi_lg + allsi_t8, ["oh"])
                S.op("vector", lambda e: e.tensor_tensor(out=oh[:], in0=oh[:], in1=dall[:], op=ALU.mult), ["oh", "dall"], ["oh"])
                S.op("vector", lambda e, k=k: e.tensor_reduce(out=dest_f[:, :, k], in_=oh[:], axis=AX.X, op=ALU.add), ["oh"], ["dest_f"])
            S.op("vector", lambda e: e.tensor_copy(out=dest_i[:], in_=dest_f[:]), ["dest_f"], ["dest_i"])
            for si in range(nsub):
                hr = hrow[si % 2]
                hrn = "a_hrow%d" % (si % 2)
                jn, s = divmod(si, 4)
                tok0 = tiles[jn] * 512 + s * 128
                S.dma("sync", hr[:], g.Htok[tok0:tok0 + 128, :], reads=[("Htok", si)], writes=[hrn])
                for k in range(4):
                    S.op("gpsimd", lambda e, hr=hr, si=si, k=k: e.indirect_dma_start(
                        out=g.Hs[:, :], out_offset=bass.IndirectOffsetOnAxis(ap=dest_i[:, si, k:k + 1], axis=0), in_=hr[:],
                        in_offset=None), [hrn, "dest_i"], [("Hs", si, k)], dma=True)
            S.barrier()

        with ExitStack() as sd:
            sbd = lambda n, s, d=F32: g.sb(n, s, d, sd)
            guw = [sbd("d_guw%d" % i, [128, NCH, 2 * D], BF16) for i in range(2)]
            dnw = [sbd("d_dnw%d" % i, [128, NCH, D], BF16) for i in range(2)]
            gub = [sbd("d_gub%d" % i, [128, 16]) for i in range(2)]
            dnb = [sbd("d_dnb%d" % i, [128, D]) for i in range(2)]
            hs = [sbd("d_hs%d" % i, [128, 4, D], BF16) for i in range(2)]
            hsT = sbd("d_hsT", [128, NCH, BLK], BF16)
            gt = [sbd("d_gt%d" % i, [128, BLK]) for i in range(2)]
            sg = [sbd("d_sg%d" % i, [128, BLK]) for i in range(2)]
            ut = [sbd("d_ut%d" % i, [128, BLK]) for i in range(2)]
            act = sbd("d_act", [128, NCH, BLK], BF16)
            yt = [sbd("d_yt%d" % i, [128, D]) for i in range(2)]
            trp = [g.ps("d_trp%d" % i, [128, BLK], BF16, sd) for i in range(2)]
            pg = [g.ps("d_pg%d" % i, [128, BLK], F32, sd) for i in range(2)]
            pu = [g.ps("d_pu%d" % i, [128, BLK], F32, sd) for i in range(2)]
            py = [g.ps("d_py%d" % i, [128, BLK], F32, sd) for i in range(2)]
            guv = g.gu_w.rearrange("l r c -> (l r) c")
            dnv = g.dn_w.rearrange("l r c -> (l r) c")
            gbv = g.gu_bT.rearrange("l r c -> (l r) c")
            dbv = g.dn_b.rearrange("l r c -> (l r) c")
            yk = [0]
            hsT2 = [hsT, sbd("d_hsT1", [128, NCH, BLK], BF16)]

            def names(b):
                p = b % 2
                return p, "d_guw%d" % p, "d_dnw%d" % p, "d_gub%d" % p, "d_dnb%d" % p, "d_hs%d" % p

            def emitLoad(b):
                p, wn, dn_, gbn, dbn, hsn = names(b)
                for kc in range(NCH):
                    S.op("gpsimd", lambda e, b=b, kc=kc, p=p: e.indirect_dma_start(
                        out=guw[p][:, kc, :], out_offset=None, in_=guv[:, :],
                        in_offset=bass.IndirectOffsetOnAxis(ap=idxw[:, b:b + 1], axis=0), element_offset=(li * NEXP * D + kc * 128) * 2 * D),
                        ["idxw"], [(wn, kc)], dma=True)
                for kc in range(NCH):
                    S.op("gpsimd", lambda e, b=b, kc=kc, p=p: e.indirect_dma_start(
                        out=dnw[p][:, kc, :], out_offset=None, in_=dnv[:, :],
                        in_offset=bass.IndirectOffsetOnAxis(ap=idxw[:, b:b + 1], axis=0), element_offset=(li * NEXP * D + kc * 128) * D),
                        ["idxw"], [(dn_, kc)], dma=True)
                S.op("gpsimd", lambda e, b=b, p=p: e.indirect_dma_start(
                    out=gub[p][:], out_offset=None, in_=gbv[:, :],
                    in_offset=bass.IndirectOffsetOnAxis(ap=idxb[:, b:b + 1], axis=0), element_offset=li * NEXP * 128 * 16), ["idxb"], [gbn], dma=True)
                S.op("gpsimd", lambda e, b=b, p=p: e.indirect_dma_start(
                    out=dnb[p][:], out_offset=None, in_=dbv[:, :],
                    in_offset=bass.IndirectOffsetOnAxis(ap=idxe[:, b:b + 1], axis=0), element_offset=li * NEXP * D), ["idxe"], [dbn], dma=True)
                S.dma("sync", hs[p][:], g.Hs[b * BLK:(b + 1) * BLK, :].rearrange("(s p) f -> p s f", p=128), reads=[], writes=[hsn])

            def emitT(b):
                p, wn, dn_, gbn, dbn, hsn = names(b)
                hT = hsT2[p]
                for kc in range(NCH):
                    tp = trp[kc % 2]
                    tpn = "d_trp%d" % (kc % 2)
                    for s_ in range(4):
                        S.op("tensor", lambda e, tp=tp, s_=s_, kc=kc, p=p: e.transpose(
                            tp[:, s_ * 128:(s_ + 1) * 128], hs[p][:, s_, kc * 128:(kc + 1) * 128], g.ident_bf[:]),
                            [hsn, "ident_bf"], [tpn])
                    S.op("scalar", lambda e, tp=tp, kc=kc, hT=hT: e.activation(out=hT[:, kc, :], in_=tp[:], func=AF.Copy), [tpn], [("hsT", p, kc)])

            def emitGU(b):
                p, wn, dn_, gbn, dbn, hsn = names(b)
                hT = hsT2[p]
                for fc in range(NCH):
                    q = fc % 2
                    for kc in range(NCH):
                        S.op("tensor", lambda e, fc=fc, kc=kc, p=p, q=q, hT=hT: e.matmul(
                            pg[q][:], lhsT=guw[p][:, kc, fc * 128:(fc + 1) * 128], rhs=hT[:, kc, :], start=(kc == 0),
                            stop=(kc == NCH - 1)), [(wn, kc), ("hsT", p, kc)], ["d_pg%d" % q])
                    for kc in range(NCH):
                        S.op("tensor", lambda e, fc=fc, kc=kc, p=p, q=q, hT=hT: e.matmul(
                            pu[q][:], lhsT=guw[p][:, kc, D + fc * 128:D + (fc + 1) * 128], rhs=hT[:, kc, :], start=(kc == 0),
                            stop=(kc == NCH - 1)), [(wn, kc), ("hsT", p, kc)], ["d_pu%d" % q])
                    gtn, sgn, utn = "d_gt%d" % q, "d_sg%d" % q, "d_ut%d" % q
                    S.op("vector", lambda e, fc=fc, p=p, q=q: e.tensor_scalar(
                        out=gt[q][:], in0=pg[q][:], scalar1=gub[p][:, fc:fc + 1], scalar2=7.0, op0=ALU.add, op1=ALU.min),
                        ["d_pg%d" % q, gbn], [gtn])
                    S.op("scalar", lambda e, q=q: e.activation(out=sg[q][:], in_=gt[q][:], func=AF.Sigmoid, scale=1.702), [gtn], [sgn])
                    S.op("vector", lambda e, fc=fc, p=p, q=q: e.tensor_scalar(
                        out=ut[q][:], in0=pu[q][:], scalar1=gub[p][:, 8 + fc:9 + fc], scalar2=7.0, op0=ALU.add, op1=ALU.min),
                        ["d_pu%d" % q, gbn], [utn])
                    S.op("vector", lambda e, q=q: e.tensor_scalar(out=ut[q][:], in0=ut[q][:], scalar1=-7.0, scalar2=1.0, op0=ALU.max,
                                                                  op1=ALU.add), [utn], [utn])
                    S.op("vector", lambda e, q=q: e.tensor_tensor(out=gt[q][:], in0=gt[q][:], in1=sg[q][:], op=ALU.mult), [gtn, sgn], [gtn])
                    S.op("vector", lambda e, fc=fc, q=q: e.tensor_tensor(out=act[:, fc, :], in0=gt[q][:], in1=ut[q][:], op=ALU.mult),
                         [gtn, utn], [("act", fc)])

            def emitDN(b):
                p, wn, dn_, gbn, dbn, hsn = names(b)
                for s_ in range(4):
                    y = yt[yk[0] % 2]
                    yn = "d_yt%d" % (yk[0] % 2)
                    yk[0] += 1
                    for half in range(2):
                        for fc in range(NCH):
                            S.op("tensor", lambda e, s_=s_, half=half, fc=fc, p=p: e.matmul(
                                py[half][:], lhsT=act[:, fc, s_ * 128:(s_ + 1) * 128], rhs=dnw[p][:, fc, half * 512:(half + 1) * 512],
                                start=(fc == 0), stop=(fc == NCH - 1)), [("act", fc), (dn_, fc)], ["d_py%d" % half])
                        S.op("vector", lambda e, half=half, y=y, p=p: e.tensor_tensor(
                            out=y[:, half * 512:(half + 1) * 512], in0=py[half][:], in1=dnb[p][:, half * 512:(half + 1) * 512], op=ALU.add),
                            ["d_py%d" % half, dbn], [(yn, half)])
                    r0 = b * BLK + s_ * 128
                    S.dma("sync", g.Ys[r0:r0 + 128, :], y[:], reads=[(yn, 0), (yn, 1)], writes=[("Ys", b, s_)])

            emitLoad(0)
            emitT(0)
            for b in range(NB):
                if b + 1 < NB:
                    emitLoad(b + 1)
                emitGU(b)
                if b + 1 < NB:
                    emitT(b + 1)
                emitDN(b)
            S.barrier()

        with ExitStack() as se:
            sbe = lambda n, s, d=F32: g.sb(n, s, d, se)
            xt = [sbe("e_xt%d" % i, [128, NCH, 512]) for i in range(2)]
            yk_t = [sbe("e_yk%d" % i, [128, 4, D]) for i in range(2)]
            dg = [sbe("e_dg%d" % i, [128, 4, 128]) for i in range(2)]
            fp = [g.ps("e_fp%d" % i, [128, NCH, 128], F32, se) for i in range(2)]
            si = 0
            for jn, j in enumerate(tiles):
                x = xt[jn % 2]
                xn = "e_xt%d" % (jn % 2)
                stream = stream_of_tile(j)
                S.dma("sync", x[:], XTv[:, :, j * 512:(j + 1) * 512], reads=[("XT", j)], writes=[xn])
                for s in range(4):
                    q = si % 2
                    for k in range(4):
                        S.op("gpsimd", lambda e, q=q, si=si, k=k: e.indirect_dma_start(
                            out=yk_t[q][:, k, :], out_offset=None, in_=g.Ys[:, :],
                            in_offset=bass.IndirectOffsetOnAxis(ap=dest_i[:, si, k:k + 1], axis=0)), ["dest_i"],
                            [("e_yk%d" % q, k)], dma=True)
                        S.op("vector", lambda e, q=q, si=si, k=k: e.tensor_scalar(
                            out=dg[q][:, k, :], in0=g.ident_f, scalar1=gates[:, si, k:k + 1], scalar2=None, op0=ALU.mult),
                            [("gates", si), "cst"], [("e_dg%d" % q, k)])
                    for c in range(NCH):
                        for k in range(4):
                            S.op("tensor", lambda e, q=q, c=c, k=k: e.matmul(
                                fp[q][:, c, :], lhsT=yk_t[q][:, k, c * 128:(c + 1) * 128], rhs=dg[q][:, k, :], start=(k == 0), stop=(k == 3)),
                                [("e_yk%d" % q, k), ("e_dg%d" % q, k)], ["e_fp%d" % q])
                    for c in range(NCH):
                        S.op("vector", lambda e, q=q, c=c, x=x, s=s, stream=stream: e.scalar_tensor_tensor(
                            out=x[:, c, s * 128:(s + 1) * 128], in0=fp[q][:, c, :], scalar=g.mods[l][:, 5, c, stream:stream + 1],
                            in1=x[:, c, s * 128:(s + 1) * 128], op0=ALU.mult, op1=ALU.add), ["e_fp%d" % q, xn, ("mods", l)], [xn])
                    si += 1
                S.dma("scalar", XTv[:, :, j * 512:(j + 1) * 512], x[:], reads=[xn], writes=[("XT", j)])
            S.barrier()


TWO_PI = 6.283185307179586
NBK = 544


def mkap(base, step, cnt):
    return bass.AP(tensor=base.tensor, offset=base.offset, ap=[list(base.ap[0]), [step, cnt]])


def s5_layer(g, l):
    nc, S = g.nc, g.S
    jj = l // 3
    last = (l == 3)
    XTv = g.XT.rearrange("(c p) t -> p c t", p=128)
    HTv = g.HT.rearrange("(c p) t -> p c t", p=128)

    def V(fn, r, w, eng="vector"):
        S.op(eng, fn, r, w)

    def tt(out, a, b, op, r, w, eng="vector"):
        S.op(eng, lambda e: e.tensor_tensor(out=out, in0=a, in1=b, op=op), r, w)

    def ts(out, a, s1, s2, op0, op1, r, w):
        if s2 is None:
            S.op("vector", lambda e: e.tensor_scalar(out=out, in0=a, scalar1=s1, scalar2=None, op0=op0), r, w)
        else:
            S.op("vector", lambda e: e.tensor_scalar(out=out, in0=a, scalar1=s1, scalar2=s2, op0=op0, op1=op1), r, w)

    def act(out, in_, func, r, w, **kw):
        S.op("scalar", lambda e: e.activation(out=out, in_=in_, func=func, **kw), r, w)

    def sinred(y, out, ki, kf, fr, quarter, rd, tagw, names=("sr_ki", "sr_kf", "sr_fr")):
        nki, nkf, nfr = names
        if quarter:
            ts(fr, y, 0.25, None, ALU.add, None, rd, [nfr])
            src, rsrc = fr, [nfr]
        else:
            src, rsrc = y, rd
        V(lambda e: e.tensor_copy(out=ki, in_=src), rsrc, [nki])
        V(lambda e: e.tensor_copy(out=kf, in_=ki), [nki], [nkf])
        tt(fr, src, kf, ALU.subtract, rsrc + [nkf], [nfr])
        ts(kf, fr, 0.5, None, ALU.is_gt, None, [nfr], [nkf])
        tt(fr, fr, kf, ALU.subtract, [nfr, nkf], [nfr])
        act(out, fr, AF.Sin, [nfr], tagw, scale=TWO_PI)

    def discretize(tag, pv, Fn, Pre, Pim, co, fr8, svals, st2):
        sb2 = lambda n, s_, d=F32: g.sb("z" + tag + n, s_, d, st2)
        dt = sb2("dt", [128, Fn]); dta = sb2("dta", [128, Fn]); ang = sb2("ang", [128, Fn])
        eS = sb2("eS", [128, Fn, 9]); yS = sb2("yS", [128, Fn, 9])
        ki = sb2("ki", [128, Fn, 9], I32); kf = sb2("kf", [128, Fn, 9]); fr = sb2("fr", [128, Fn, 9])
        t1 = sb2("t1", [128, Fn]); t2 = sb2("t2", [128, Fn]); den = sb2("den", [128, Fn])
        pn = "s5prm" + tag
        act(dt[:], pv[:, :, 2], AF.Exp, [pn], ["dz_dt"])
        tt(dta[:], dt[:], pv[:, :, 0], ALU.mult, ["dz_dt", pn], ["dz_dta"])
        tt(ang[:], dt[:], pv[:, :, 1], ALU.mult, ["dz_dt", pn], ["dz_ang"])
        tt(eS[:], bc(dta[:], [128, Fn, 9], 2), bc(svals[:], [128, Fn, 9], 1), ALU.mult, ["dz_dta", "svals"], ["dz_eS"])
        act(eS[:], eS[:], AF.Exp, ["dz_eS"], ["dz_eS"])
        tt(yS[:], bc(ang[:], [128, Fn, 9], 2), bc(svals[:], [128, Fn, 9], 1), ALU.mult, ["dz_ang", "svals"], ["dz_yS"])
        ts(yS[:], yS[:], 1.0 / TWO_PI, None, ALU.mult, None, ["dz_yS"], ["dz_yS"])
        sinred(yS[:], Pim, ki[:], kf[:], fr[:], False, ["dz_yS"], [tag + "P"])
        V(lambda e: e.tensor_copy(out=fr8, in_=fr[:, :, 8]), ["sr_fr"], [tag + "fr8"])
        sinred(yS[:], Pre, ki[:], kf[:], fr[:], True, ["dz_yS"], [tag + "P"])
        tt(Pre, Pre, eS[:], ALU.mult, ["dz_eS", tag + "P"], [tag + "P"])
        tt(Pim, Pim, eS[:], ALU.mult, ["dz_eS", tag + "P"], [tag + "P"])
        Pre1, Pim1 = Pre[:, :, 1], Pim[:, :, 1]
        are, aim = pv[:, :, 0], pv[:, :, 1]
        tt(den[:], are, are, ALU.mult, [pn], ["dz_den"])
        tt(t1[:], aim, aim, ALU.mult, [pn], ["dz_t1"])
        tt(den[:], den[:], t1[:], ALU.add, ["dz_den", "dz_t1"], ["dz_den"])
        V(lambda e: e.reciprocal(out=den[:], in_=den[:]), ["dz_den"], ["dz_den"])
        ts(t1[:], Pre1, -1.0, None, ALU.add, None, [tag + "P"], ["dz_t1"])
        tt(t2[:], t1[:], are, ALU.mult, ["dz_t1", pn], ["dz_t2"])
        tt(dt[:], Pim1, aim, ALU.mult, [tag + "P", pn], ["dz_dt"])
        tt(t2[:], t2[:], dt[:], ALU.add, ["dz_t2", "dz_dt"], ["dz_t2"])
        tt(co[:, 0, :], t2[:], den[:], ALU.mult, ["dz_t2", "dz_den"], [tag + "co"])
        tt(t2[:], Pim1, are, ALU.mult, [tag + "P", pn], ["dz_t2"])
        tt(dt[:], t1[:], aim, ALU.mult, ["dz_t1", pn], ["dz_dt"])
        tt(t2[:], t2[:], dt[:], ALU.subtract, ["dz_t2", "dz_dt"], ["dz_t2"])
        tt(co[:, 1, :], t2[:], den[:], ALU.mult, ["dz_t2", "dz_den"], [tag + "co"])

    with ExitStack() as st:
        sb = lambda n, s_, d=F32: g.sb(n, s_, d, st)
        xt = [sb("n_xt%d" % i, [128, NCH, 512]) for i in range(2)]
        sq = sb("n_sq", [128, NCH, 512], BF16)
        tmp = sb("n_tmp", [128, NCH, 512])
        hbf = [sb("n_hbf%d" % i, [128, NCH, 512], BF16) for i in range(2)]
        rstd = sb("n_rstd", [128, 512])
        ssp = g.ps("n_ssp", [128, 512], F32, st)
        for j in range(NT):
            x, xn = xt[j % 2], "n_xt%d" % (j % 2)
            S.dma("sync", x[:], XTv[:, :, j * 512:(j + 1) * 512], reads=[("XT", j)], writes=[xn])
            normmod(g, l, 1, x, xn, sq, ssp, rstd, tmp, None, hbf[j % 2], stream_of_tile(j), tag="N", hbfn="hbfN%d" % (j % 2))
            S.dma("sync", HTv[:, :, j * 512:(j + 1) * 512], hbf[j % 2][:], reads=["hbfN%d" % (j % 2)], writes=[("HT", j)])
        S.barrier()

    with ExitStack() as st:
        sb = lambda n, s_, d=F32: g.sb(n, s_, d, st)
        svals = sb("s_svals", [128, 9])
        irow = sb("s_irow", [128, NBK])
        S.dma("sync", svals[:], g.s5c[:, 0:9], writes=["svals"])
        S.dma("sync", irow[:], g.s5c[:, 16:16 + NBK], writes=["irow"])
        PB_re = sb("s_PBre", [128, 64, 9]); PB_im = sb("s_PBim", [128, 64, 9]); coB = sb("s_coB", [128, 2, 64]); fr8B = sb("s_fr8B", [128, 64])
        with ExitStack() as st2:
            prmB = g.sb("s_prmB", [128, 64, 3], F32, st2)
            S.dma("sync", prmB[:], g.s5B[jj].rearrange("p d q k -> p (d q) k"), writes=["s5prmB"])
            discretize("B", prmB, 64, PB_re[:], PB_im[:], coB, fr8B[:], svals, st2)
            S.barrier()
        PBr = PB_re[:].rearrange("p (d q) k -> p d q k", d=2); PBi = PB_im[:].rearrange("p (d q) k -> p d q k", d=2)
        coBv = coB[:].rearrange("p r (d q) -> p r d q", d=2)
        fr8Bv = fr8B[:].rearrange("p (d q) -> p d q", d=2)
        rho = sb("s_rho", [128, 2, 32])
        t_r = sb("s_tr", [128, 2, 32]); t_i = sb("s_ti", [128, 2, 32])
        tt(t_r[:], PBr[:, :, :, 8], PBr[:, :, :, 8], ALU.mult, ["BP"], ["s_tr"])
        tt(t_i[:], PBi[:, :, :, 8], PBi[:, :, :, 8], ALU.mult, ["BP"], ["s_ti"])
        tt(t_r[:], t_r[:], t_i[:], ALU.add, ["s_tr", "s_ti"], ["s_tr"])
        act(rho[:], t_r[:], AF.Sqrt, ["s_tr"], ["rho"])
        dsk = sb("s_dsk", [128, NCH])
        S.dma("sync", dsk[:], g.s5d[jj, :, :], writes=["dsk"])

        Vw = sb("s_Vw", [128, 2, 8, 2, 512], BF16)
        Y1 = sb("s_Y1", [128, 2, 8, 2, 4, 128], BF16)
        Kw = sb("s_Kw", [128, 15, 128], BF16)
        XS = [sb("s_XS%d" % d, [128, 2, 4, NBK + 1], BF16) for d in range(2)]
        for d in range(2):
            V(lambda e, d=d: e.memset(XS[d][:], 0.0), [], ["XS%d" % d])
        kps = g.ps("s_kps", [128, 128], F32, st)
        vps_c = [g.ps("s_vpc%d" % i, [128, 32], F32, st) for i in range(2)]
        vps_l = [g.ps("s_vpl%d" % i, [128, 512], F32, st) for i in range(2)]

        for ct in range(NCH):
            with ExitStack() as spa:
                PA_re = g.sb("p_PAre", [128, 128, 9], F32, spa); PA_im = g.sb("p_PAim", [128, 128, 9], F32, spa)
                coA = g.sb("p_coA", [128, 2, 128], F32, spa); fr8A = g.sb("p_fr8A", [128, 128], F32, spa)
                with ExitStack() as st2:
                    prmA4 = g.sb("p_prmA", [128, 2, 64, 3], F32, st2)
                    S.dma("sync", prmA4[:], g.s5A[jj][:, :, ct, :, :], writes=["s5prmA"])
                    prmA = prmA4[:].rearrange("p d q k -> p (d q) k")
                    discretize("A", prmA, 128, PA_re[:], PA_im[:], coA, fr8A[:], svals, st2)
                    S.barrier()
                PAr = PA_re[:].rearrange("p (d q) k -> p d q k", d=2); PAi = PA_im[:].rearrange("p (d q) k -> p d q k", d=2)
                coAv = coA[:].rearrange("p r (d q) -> p r d q", d=2)
                with ExitStack() as sp:
                    sbp = lambda n, s_, d=F32: g.sb(n, s_, d, sp)
                    BzB = sbp("p_BzB", [128, 2, 2, 4, 128]); CzB = sbp("p_CzB", [128, 2, 2, 4, 128]); bbz = sbp("p_bbz", [128, 2, 2, 4, 128])
                    BzA = sbp("p_BzA", [128, 2, 2, 8, 64]); bbA = sbp("p_bbA", [128, 2, 2, 8, 64])
                    u1 = sbp("p_u1", [128, 2, 4, 128]); u2 = sbp("p_u2", [128, 2, 4, 128])
                    cp = sbp("p_cp", [128, 2, 4, 128])
                    w1 = sbp("p_w1", [128, 8, 64]); w2 = sbp("p_w2", [128, 8, 64])
                    dgd = sbp("p_dgd", [128, 128])
                    S.dma("sync", BzB[:], g.s5BzB[jj][:, :, :, 4 * ct:4 * ct + 4, :], writes=["BzB"])
                    S.dma("sync", CzB[:], g.s5CzB[jj][:, :, :, 4 * ct:4 * ct + 4, :], writes=["CzB"])
                    S.dma("sync", BzA[:], g.s5BzA[jj][:, :, :, ct, :, :], writes=["BzA"])
                    co_re = bc(coBv[:, 0, :, 4 * ct:4 * ct + 4], [128, 2, 4, 128], 3)
                    co_im = bc(coBv[:, 1, :, 4 * ct:4 * ct + 4], [128, 2, 4, 128], 3)
                    tt(u1[:], BzB[:, :, 0], co_re, ALU.mult, ["BzB", "Bco"], ["u1"])
                    tt(u2[:], BzB[:, :, 1], co_im, ALU.mult, ["BzB", "Bco"], ["u2"])
                    tt(bbz[:, :, 0], u1[:], u2[:], ALU.subtract, ["u1", "u2"], ["bbz"])
                    tt(u1[:], BzB[:, :, 1], co_re, ALU.mult, ["BzB", "Bco"], ["u1"])
                    tt(u2[:], BzB[:, :, 0], co_im, ALU.mult, ["BzB", "Bco"], ["u2"])
                    tt(bbz[:, :, 1], u1[:], u2[:], ALU.add, ["u1", "u2"], ["bbz"])
                    for d in range(2):
                        cr = bc(coAv[:, 0, d, :], [128, 8, 64], 1)
                        ci = bc(coAv[:, 1, d, :], [128, 8, 64], 1)
                        tt(w1[:], BzA[:, d, 0], cr, ALU.mult, ["BzA", "Aco"], ["w1"])
                        tt(w2[:], BzA[:, d, 1], ci, ALU.mult, ["BzA", "Aco"], ["w2"])
                        tt(bbA[:, d, 0], w1[:], w2[:], ALU.subtract, ["w1", "w2"], ["bbA"])
                        tt(w1[:], BzA[:, d, 1], cr, ALU.mult, ["BzA", "Aco"], ["w1"])
                        tt(w2[:], BzA[:, d, 0], ci, ALU.mult, ["BzA", "Aco"], ["w2"])
                        tt(bbA[:, d, 1], w1[:], w2[:], ALU.add, ["w1", "w2"], ["bbA"])
                    for d in range(2):
                        for s in range(8):
                            pw = 7 - s if d == 0 else s
                            pr = bc(PAr[:, d, :, pw], [128, 8, 64], 1)
                            pi = bc(PAi[:, d, :, pw], [128, 8, 64], 1)
                            o_re = Vw[:, d, s, 0, :].rearrange("p (a b) -> p a b", a=8)
                            o_im = Vw[:, d, s, 1, :].rearrange("p (a b) -> p a b", a=8)
                            tt(w1[:], bbA[:, d, 0], pr, ALU.mult, ["bbA", "AP"], ["w1"])
                            tt(w2[:], bbA[:, d, 1], pi, ALU.mult, ["bbA", "AP"], ["w2"])
                            tt(o_re, w1[:], w2[:], ALU.subtract, ["w1", "w2"], ["Vw"])
                            tt(w1[:], bbA[:, d, 1], pr, ALU.mult, ["bbA", "AP"], ["w1"])
                            tt(w2[:], bbA[:, d, 0], pi, ALU.mult, ["bbA", "AP"], ["w2"])
                            tt(o_im, w1[:], w2[:], ALU.add, ["w1", "w2"], ["Vw"])
                    ts(dgd[:], g.ident_f, dsk[:, ct:ct + 1], None, ALU.mult, None, ["cst", "dsk"], ["dgd"])
                    for k in range(9):
                        for d in range(2):
                            pr = bc(PBr[:, d, 4 * ct:4 * ct + 4, k], [128, 4, 128], 2)
                            pi = bc(PBi[:, d, 4 * ct:4 * ct + 4, k], [128, 4, 128], 2)
                            tt(u1[:, 0], CzB[:, d, 0], pr, ALU.mult, ["CzB", "BP"], ["u1"])
                            tt(u2[:, 0], CzB[:, d, 1], pi, ALU.mult, ["CzB", "BP"], ["u2"])
                            tt(cp[:, 0], u1[:, 0], u2[:, 0], ALU.subtract, ["u1", "u2"], ["cp"])
                            tt(u1[:, 0], CzB[:, d, 0], pi, ALU.mult, ["CzB", "BP"], ["u1"])
                            tt(u2[:, 0], CzB[:, d, 1], pr, ALU.mult, ["CzB", "BP"], ["u2"])
                            V(lambda e: e.scalar_tensor_tensor(out=cp[:, 1], in0=u1[:, 0], scalar=-1.0, in1=u2[:, 0], op0=ALU.mult, op1=ALU.subtract),
                              ["u1", "u2"], ["cp"])
                            if k >= 1:
                                S.op("gpsimd", lambda e, d=d, k=k: e.tensor_copy(out=Y1[:, d, k - 1], in_=cp[:]), ["cp"], ["Y1"])
                            if k <= 7:
                                n = 0
                                for q in range(4):
                                    for ri in range(2):
                                        st_ = (n == 0) and (k > 0 or d == 0)
                                        sp_ = (n == 7) and (k > 0)
                                        S.op("tensor", lambda e, d=d, ri=ri, q=q, st_=st_, sp_=sp_: e.matmul(
                                            kps[:], lhsT=bbz[:, d, ri, q, :], rhs=cp[:, ri, q, :], start=st_, stop=sp_), ["bbz", "cp"], ["kps"])
                                        n += 1
                                if k == 0 and d == 1:
                                    S.op("tensor", lambda e: e.matmul(kps[:], lhsT=g.ident_f, rhs=dgd[:], start=False, stop=True), ["cst", "dgd"], ["kps"])
                                if k > 0 or d == 1:
                                    slot = 0 if k == 0 else (k if d == 0 else 7 + k)
                                    act(Kw[:, slot, :], kps[:], AF.Copy, ["kps"], ["Kw"])
                    S.barrier()

            with ExitStack() as sd:
                sbd = lambda n, s_, d=F32: g.sb(n, s_, d, sd)
                Us = sbd("d_U", [128, 4352], BF16)
                Ys = sbd("d_Y", [128, 4352], BF16)
                Vb = sbd("d_V", [128, 2, 2, NBK]); Wb = sbd("d_W", [128, 2, 2, NBK]); Tb = sbd("d_T", [128, 2, NBK])
                cosT = sbd("d_cosT", [128, 2, NBK]); sinT = sbd("d_sinT", [128, 2, NBK]); tki = sbd("d_tki", [128, 2, NBK], I32)
                gq = sbd("d_gq", [128, 512]); gt2 = sbd("d_gt2", [128, 512]); gsg = sbd("d_gsg", [128, 512])
                Un, Yn = "d_U", "d_Y"
                for sq_ in range(2):
                    cc = slice(sq_ * 256, sq_ * 256 + 256)
                    lc = slice(512 + sq_ * 4096, 512 + (sq_ + 1) * 4096)
                    S.dma("sync", Us[:, 0:256], g.HT[ct * 128:(ct + 1) * 128, cc], reads=[], writes=[Un])
                    S.dma("sync", Us[:, 256:4352], g.HT[ct * 128:(ct + 1) * 128, lc], reads=[], writes=[Un])

                    def ucols(start, step, cnt):
                        return mkap(Us[:, start:start + 1], step, cnt)

                    for d in range(2):
                        for qh in range(2):
                            q0 = 4 * ct + 2 * qh
                            tt(Tb[:], bc(fr8Bv[:, d, q0:q0 + 2], [128, 2, NBK], 2), bc(irow[:], [128, 2, NBK], 1), ALU.mult, ["Bfr8", "irow"], ["Tb"])
                            sinred(Tb[:], sinT[:], tki[:], Wb[:, 0], Wb[:, 1], False, ["Tb"], ["sinT"], names=("tki", "Wb", "Wb"))
                            sinred(Tb[:], cosT[:], tki[:], Wb[:, 0], Wb[:, 1], True, ["Tb"], ["cosT"], names=("tki", "Wb", "Wb"))
                            n = 0
                            for ql in range(2):
                                q = 2 * qh + ql
                                for ri in range(2):
                                    pc, pl = vps_c[n % 2], vps_l[n % 2]
                                    pcn, pln = "s_vpc%d" % (n % 2), "s_vpl%d" % (n % 2)
                                    n += 1
                                    for s in range(8):
                                        if d == 0:
                                            rc, rl = ucols(s, 8, 32), ucols(256 + s, 8, 512)
                                        else:
                                            rc, rl = ucols(31 * 8 + s, -8, 32), ucols(256 + 511 * 8 + s, -8, 512)
                                        S.op("tensor", lambda e, d=d, s=s, ri=ri, q=q, pc=pc, rc=rc: e.matmul(
                                            pc[:], lhsT=Vw[:, d, s, ri, q * 128:(q + 1) * 128], rhs=rc, start=(s == 0), stop=(s == 7)), ["Vw", Un], [pcn])
                                        S.op("tensor", lambda e, d=d, s=s, ri=ri, q=q, pl=pl, rl=rl: e.matmul(
                                            pl[:], lhsT=Vw[:, d, s, ri, q * 128:(q + 1) * 128], rhs=rl, start=(s == 0), stop=(s == 7)), ["Vw", Un], [pln])
                                    act(Vb[:, ri, ql, 0:32], pc[:], AF.Copy, [pcn], ["Vb"])
                                    act(Vb[:, ri, ql, 32:NBK], pl[:], AF.Copy, [pln], ["Vb"])
                            tt(Wb[:, 0], Vb[:, 0], cosT[:], ALU.mult, ["Vb", "cosT"], ["Wb"])
                            tt(Tb[:], Vb[:, 1], sinT[:], ALU.mult, ["Vb", "sinT"], ["Tb"])
                            tt(Wb[:, 0], Wb[:, 0], Tb[:], ALU.add, ["Wb", "Tb"], ["Wb"])
                            tt(Wb[:, 1], Vb[:, 1], cosT[:], ALU.mult, ["Vb", "cosT"], ["Wb"])
                            tt(Tb[:], Vb[:, 0], sinT[:], ALU.mult, ["Vb", "sinT"], ["Tb"])
                            tt(Wb[:, 1], Wb[:, 1], Tb[:], ALU.subtract, ["Wb", "Tb"], ["Wb"])
                            for ql in range(2):
                                for ri in range(2):
                                    V(lambda e, d=d, ql=ql, ri=ri, q0=q0: e.tensor_tensor_scan(
                                        out=Vb[:, ri, ql, :], data0=rho[:, d, q0 + ql:q0 + ql + 1].to_broadcast([128, NBK]),
                                        data1=Wb[:, ri, ql, :], initial=0.0, op0=ALU.mult, op1=ALU.add), ["Wb", "rho", "Vb"], ["Vb"])
                            xs, xn = XS[d], "XS%d" % d
                            qs = slice(2 * qh, 2 * qh + 2)
                            tt(Wb[:, 0], Vb[:, 0], cosT[:], ALU.mult, ["Vb", "cosT"], ["Wb"])
                            tt(Tb[:], Vb[:, 1], sinT[:], ALU.mult, ["Vb", "sinT"], ["Tb"])
                            tt(xs[:, 0, qs, 1:NBK + 1], Wb[:, 0], Tb[:], ALU.subtract, ["Wb", "Tb"], [xn])
                            tt(Wb[:, 1], Vb[:, 1], cosT[:], ALU.mult, ["Vb", "cosT"], ["Wb"])
                            tt(Tb[:], Vb[:, 0], sinT[:], ALU.mult, ["Vb", "sinT"], ["Tb"])
                            tt(xs[:, 1, qs, 1:NBK + 1], Wb[:, 1], Tb[:], ALU.add, ["Wb", "Tb"], [xn])

                    def xcols(d, ri, q, start, step, cnt):
                        return mkap(XS[d][:, ri, q, start:start + 1], step, cnt)

                    def ycols(start, step, cnt):
                        return mkap(Ys[:, start:start + 1], step, cnt)

                    for s in range(8):
                        pc, pl = vps_c[s % 2], vps_l[s % 2]
                        pcn, pln = "s_vpc%d" % (s % 2), "s_vpl%d" % (s % 2)
                        mm = []
                        for q in range(4):
                            for ri in range(2):
                                mm.append((Y1[:, 0, s, ri, q, :], xcols(0, ri, q, 0, 1, 32), xcols(0, ri, q, 32, 1, 512), ["Y1", "XS0"]))
                                mm.append((Y1[:, 1, 7 - s, ri, q, :], xcols(1, ri, q, 31, -1, 32), xcols(1, ri, q, 543, -1, 512), ["Y1", "XS1"]))
                        for s2 in range(8):
                            slot = 0 if s2 == s else ((s - s2) if s2 < s else 7 + (s2 - s))
                            mm.append((Kw[:, slot, :], ucols(s2, 8, 32), ucols(256 + s2, 8, 512), ["Kw", Un]))
                        for i, (lw, rc, rl, rd) in enumerate(mm):
                            S.op("tensor", lambda e, lw=lw, rc=rc, pc=pc, i=i, nm=len(mm): e.matmul(pc[:], lhsT=lw, rhs=rc, start=(i == 0), stop=(i == nm - 1)), rd, [pcn])
                            S.op("tensor", lambda e, lw=lw, rl=rl, pl=pl, i=i, nm=len(mm): e.matmul(pl[:], lhsT=lw, rhs=rl, start=(i == 0), stop=(i == nm - 1)), rd, [pln])
                        for (pp, ppn, N, oc) in ((pc, pcn, 32, ycols(s, 8, 32)), (pl, pln, 512, ycols(256 + s, 8, 512))):
                            act(gq[:, :N], pp[:], AF.Square, [ppn], ["gq"])
                            ts(gq[:, :N], gq[:, :N], 0.044715, 1.0, ALU.mult, ALU.add, ["gq"], ["gq"])
                            tt(gt2[:, :N], gq[:, :N], pp[:], ALU.mult, ["gq", ppn], ["gt2"])
                            act(gsg[:, :N], gt2[:, :N], AF.Sigmoid, ["gt2"], ["gsg"], scale=1.5957691216057308)
                            tt(oc, gsg[:, :N], pp[:], ALU.mult, ["gsg", ppn], [Yn])
                    S.dma("sync", g.GT[ct * 128:(ct + 1) * 128, cc], Ys[:, 0:256], reads=[Yn], writes=[("GT", ct, sq_, 0)])
                    S.dma("sync", g.GT[ct * 128:(ct + 1) * 128, lc], Ys[:, 256:4352], reads=[Yn], writes=[("GT", ct, sq_, 1)])
                S.barrier()

    with ExitStack() as st:
        sb = lambda n, s_, d=F32: g.sb(n, s_, d, st)
        GTv = g.GT.rearrange("(c p) t -> p c t", p=128)
        gw = sb("g_w", [128, NCH, 2 * D], BF16)
        gb = sb("g_b", [128, 16])
        S.op("gpsimd", lambda e: e.dma_start(out=gw[:], in_=g.s5gw[jj].rearrange("(kc p) n -> p kc n", p=128)), [], ["g_w"], dma=True)
        S.dma("sync", gb[:], g.s5gb[jj, :, :], writes=["g_b"])
        xt = [sb("g_xt%d" % i, [128, NCH, 512]) for i in range(2)]
        gi = [sb("g_gi%d" % i, [128, NCH, 512], BF16) for i in range(2)]
        sgm = [sb("g_sg%d" % i, [128, 512]) for i in range(2)]
        yl = [sb("g_yl%d" % i, [128, 512]) for i in range(2)]
        pa = [g.ps("g_pa%d" % i, [128, 512], F32, st) for i in range(2)]
        pb = [g.ps("g_pb%d" % i, [128, 512], F32, st) for i in range(2)]
        tiles = list(range(1 if last else 0, NT))
        for jn, j in enumerate(tiles):
            x, xn = xt[jn % 2], "g_xt%d" % (jn % 2)
            gg, ggn = gi[jn % 2], "g_gi%d" % (jn % 2)
            stream = stream_of_tile(j)
            S.dma("sync", x[:], XTv[:, :, j * 512:(j + 1) * 512], reads=[("XT", j)], writes=[xn])
            S.dma("sync", gg[:], GTv[:, :, j * 512:(j + 1) * 512], reads=[], writes=[ggn])
            for oc in range(NCH):
                q = oc % 2
                for kc in range(NCH):
                    S.op("tensor", lambda e, oc=oc, kc=kc, q=q, gg=gg: e.matmul(pa[q][:], lhsT=gw[:, kc, oc * 128:(oc + 1) * 128], rhs=gg[:, kc, :],
                                                                               start=(kc == 0), stop=(kc == NCH - 1)), ["g_w", ggn], ["g_pa%d" % q])
                for kc in range(NCH):
                    S.op("tensor", lambda e, oc=oc, kc=kc, q=q, gg=gg: e.matmul(pb[q][:], lhsT=gw[:, kc, D + oc * 128:D + (oc + 1) * 128], rhs=gg[:, kc, :],
                                                                               start=(kc == 0), stop=(kc == NCH - 1)), ["g_w", ggn], ["g_pb%d" % q])
                act(sgm[q][:], pb[q][:], AF.Sigmoid, ["g_pb%d" % q, "g_b"], ["g_sg%d" % q], bias=gb[:, 8 + oc:9 + oc], scale=1.0)
                V(lambda e, oc=oc, q=q: e.scalar_tensor_tensor(out=yl[q][:], in0=pa[q][:], scalar=gb[:, oc:oc + 1], in1=sgm[q][:], op0=ALU.add, op1=ALU.mult),
                  ["g_pa%d" % q, "g_sg%d" % q, "g_b"], ["g_yl%d" % q])
                V(lambda e, oc=oc, q=q, x=x, stream=stream: e.scalar_tensor_tensor(
                    out=x[:, oc, :], in0=yl[q][:], scalar=g.mods[l][:, 2, oc, stream:stream + 1], in1=x[:, oc, :], op0=ALU.mult, op1=ALU.add),
                  ["g_yl%d" % q, xn, ("mods", l)], [xn])
            S.dma("sync", XTv[:, :, j * 512:(j + 1) * 512], x[:], reads=[xn], writes=[("XT", j)])
        S.barrier()


def ssd_layer(g, l):
    nc, S = g.nc, g.S
    last = (l == 3)
    XTv = g.XT.rearrange("(c p) t -> p c t", p=128)
    DI = 2 * D

    def V(fn, r, w, eng="vector"):
        S.op(eng, fn, r, w)

    def tt(out, a, b, op, r, w, eng="vector"):
        S.op(eng, lambda e: e.tensor_tensor(out=out, in0=a, in1=b, op=op), r, w)

    def act(out, in_, func, r, w, **kw):
        S.op("scalar", lambda e: e.activation(out=out, in_=in_, func=func, **kw), r, w)

    with ExitStack() as st:
        sb = lambda n, s_, d=F32: g.sb(n, s_, d, st)
        w = sb("m_w", [128, NCH, 6208], BF16)
        Wv = g.ssd_inw.rearrange("(kc p) n -> p kc n", p=128)
        for i in range(4):
            c0, c1 = i * 1552, (i + 1) * 1552
            S.op("gpsimd", lambda e, c0=c0, c1=c1: e.dma_start(out=w[:, :, c0:c1], in_=Wv[:, :, c0:c1]), [], [("m_w", i)], dma=True)
        wtok = [("m_w", i) for i in range(4)]
        xt = [sb("m_xt%d" % i, [128, NCH, 512]) for i in range(2)]
        sq = sb("m_sq", [128, NCH, 512], BF16)
        tmp = sb("m_tmp", [128, NCH, 512])
        hbf = sb("m_hbf", [128, NCH, 512], BF16)
        rstd = sb("m_rstd", [128, 512])
        ob = [sb("m_ob%d" % i, [128, 8, 512], BF16) for i in range(2)]
        dtb = sb("m_dtb", [64, 512])
        ssp = g.ps("m_ssp", [128, 512], F32, st)
        pp = [g.ps("m_pp%d" % i, [128, 512], F32, st) for i in range(3)]
        n = 0
        nb = 0
        for j in range(NT):
            x, xn = xt[j % 2], "m_xt%d" % (j % 2)
            cols = slice(j * 512, (j + 1) * 512)
            S.dma("sync", x[:], XTv[:, :, cols], reads=[("XT", j)], writes=[xn])
            normmod(g, l, 1, x, xn, sq, ssp, rstd, tmp, None, hbf, stream_of_tile(j), tag="M")
            for grp in range(6):
                o_, on = ob[nb % 2], "m_ob%d" % (nb % 2)
                nb += 1
                for ci in range(8):
                    oc = grp * 8 + ci
                    p_, pn = pp[n % 3], "m_pp%d" % (n % 3)
                    n += 1
                    for kc in range(NCH):
                        S.op("tensor", lambda e, kc=kc, oc=oc, p_=p_: e.matmul(p_[:], lhsT=w[:, kc, oc * 128:(oc + 1) * 128], rhs=hbf[:, kc, :],
                                                                           start=(kc == 0), stop=(kc == NCH - 1)), wtok + ["hbfM"], [pn])
                    act(o_[:, ci, :], p_[:], AF.Silu if grp < 2 else AF.Copy, [pn], [(on, ci)])
                rd = [(on, ci) for ci in range(8)]
                if grp < 2:
                    dstv = g.SZ.rearrange("(c p) t -> p c t", p=128)[:, grp * 8:(grp + 1) * 8, cols]
                else:
                    dstv = g.XBCp.rearrange("(c p) t -> p c t", p=128)[:, (grp - 2) * 8:(grp - 1) * 8, cols]
                S.dma("sync", dstv, o_[:], reads=rd, writes=[("inproj", j, grp)])
            p_, pn = pp[n % 3], "m_pp%d" % (n % 3)
            n += 1
            for kc in range(NCH):
                S.op("tensor", lambda e, kc=kc, p_=p_: e.matmul(p_[0:64, :], lhsT=w[:, kc, 6144:6208], rhs=hbf[:, kc, :], start=(kc == 0),
                                                             stop=(kc == NCH - 1)), wtok + ["hbfM"], [pn])
            act(dtb[:], p_[0:64, :], AF.Copy, [pn], ["dtb"])
            S.dma("sync", g.DTr[:, cols], dtb[:], reads=["dtb"], writes=[("dtr", j)])
        S.barrier()

    PBW = TCORE + 16
    seg = [(0, 256), (256, 256), (512, 4096), (4608, 4096)]
    segp = [2, 2 + 260, 2 + 520, 2 + 520 + 4100]
    with ExitStack() as st:
        sb = lambda n, s_, d=F32: g.sb(n, s_, d, st)
        pb_ = [sb("c_pb%d" % i, [128, PBW], BF16) for i in range(2)]
        acc = sb("c_acc", [128, PBW])
        cob = [sb("c_co%d" % i, [128, PBW], BF16) for i in range(2)]
        cw = sb("c_w", [128, 32, 6])
        tr_sb = [sb("c_tr%d" % i, [128, 4, 128], BF16) for i in range(2)]
        trp = [g.ps("c_trp%d" % i, [128, 4, 128], BF16, st) for i in range(2)]
        S.dma("sync", cw[:], g.ssd_cw[:, :, :], writes=["c_w"])
        for i in range(2):
            V(lambda e, i=i: e.memset(pb_[i][:], 0.0), [], ["c_pb%d" % i])
        L = PBW - 4
        ntr = 0
        for c in range(32):
            p_, pn = pb_[c % 2], "c_pb%d" % (c % 2)
            o_, on = cob[c % 2], "c_co%d" % (c % 2)
            for si, (gc, ln) in enumerate(seg):
                S.dma("sync", p_[:, segp[si]:segp[si] + ln], g.XBCp[c * 128:(c + 1) * 128, gc:gc + ln], writes=[pn])
            V(lambda e, c=c, p_=p_: e.tensor_scalar(out=acc[:, 0:L], in0=p_[:, 0:L], scalar1=cw[:, c, 0:1], scalar2=None, op0=ALU.mult), [pn, "c_w"], ["c_acc"])
            for k in range(1, 5):
                V(lambda e, c=c, k=k, p_=p_: e.scalar_tensor_tensor(out=acc[:, 0:L], in0=p_[:, k:k + L], scalar=cw[:, c, k:k + 1], in1=acc[:, 0:L],
                                                                   op0=ALU.mult, op1=ALU.add), [pn, "c_w", "c_acc"], ["c_acc"])
            act(o_[:, 2:2 + L], acc[:, 0:L], AF.Silu, ["c_acc", "c_w"], [on], bias=cw[:, c, 5:6], scale=1.0)
            for si, (gc, ln) in enumerate(seg):
                src = o_[:, segp[si]:segp[si] + ln]
                if c < 16:
                    S.dma("sync", g.XFo[c * 128:(c + 1) * 128, gc:gc + ln], src, reads=[on], writes=[("xfo", c, si)])
                else:
                    S.dma("sync", g.BCo[(c - 16) * 128:(c - 15) * 128, gc:gc + ln], src, reads=[on], writes=[("bco", c, si)])
            if c < 16:
                for si, (gc, ln) in enumerate(seg):
                    for t4 in range(ln // 512 if ln >= 512 else 1):
                        nsub = 4 if ln >= 512 else ln // 128
                        tp, tpn = trp[ntr % 2], "c_trp%d" % (ntr % 2)
                        ts_, tsn = tr_sb[ntr % 2], "c_tr%d" % (ntr % 2)
                        ntr += 1
                        for u in range(nsub):
                            a0 = segp[si] + t4 * 512 + u * 128
                            S.op("tensor", lambda e, tp=tp, u=u, a0=a0, o_=o_: e.transpose(tp[:, u, :], o_[:, a0:a0 + 128], g.ident_bf[:]),
                                 [on, "ident_bf"], [tpn])
                        act(ts_[:, 0:nsub, :], tp[:, 0:nsub, :], AF.Copy, [tpn], [tsn])
                        r0 = gc + t4 * 512
                        S.dma("sync", g.XTOK[r0:r0 + nsub * 128, c * 128:(c + 1) * 128].rearrange("(u p) v -> p u v", p=128), ts_[:, 0:nsub, :],
                              reads=[tsn], writes=[("xtok", c, si, t4)])
        S.barrier()

    with ExitStack() as st:
        sb = lambda n, s_, d=F32: g.sb(n, s_, d, st)
        hp = sb("k_hp", [64, 4])
        mk = sb("k_mk", [128, 2, 4, 512], BF16)
        S.dma("sync", hp[:], g.ssd_hp[:, :], writes=["k_hp"])
        S.op("gpsimd", lambda e: e.dma_start(out=mk[:], in_=g.ssd_mask[:, :, :, :]), [], ["k_mk"], dma=True)
        aneg = sb("k_aneg", [64, 1])
        act(aneg[:], hp[:, 1:2], AF.Exp, ["k_hp"], ["k_aneg"])
        V(lambda e: e.tensor_scalar(out=aneg[:], in0=aneg[:], scalar1=-1.0, scalar2=None, op0=ALU.mult), ["k_aneg"], ["k_aneg"])
        dt = sb("k_dt", [64, 4352]); psi = sb("k_psi", [64, 4352]); dta = sb("k_dta", [64, 4352])
        psiT = sb("k_psiT", [128, 34, 64]); dtT = sb("k_dtT", [128, 34, 64])
        sel = sb("k_sel", [64, 128])
        bcs = sb("k_bcs", [128, 8, 512])
        Bg = sb("k_B", [128, 4352], BF16); Cg = sb("k_C", [128, 4352], BF16)
        Xg = sb("k_X", [128, 34, 256], BF16)
        xdt = sb("k_xdt", [128, 34, 2, 256], BF16)
        Gs = [sb("k_G%d" % i, [128, 512], BF16) for i in range(2)]
        Gm = [sb("k_Gm%d" % i, [128, 512], BF16) for i in range(2)]
        Et = [sb("k_E%d" % i, [128, 512], BF16) for i in range(6)]
        Wt = [sb("k_W%d" % i, [128, 512], BF16) for i in range(6)]
        args_ = [sb("k_arg%d" % i, [128, 512]) for i in range(4)]
        yo = [sb("k_yo%d" % i, [64, 512], BF16) for i in range(2)]
        tps = g.ps("k_tps", [128, 4, 64], F32, st)
        bps = g.ps("k_bps", [128, 512], F32, st)
        gps = [g.ps("k_gps%d" % i, [128, 512], F32, st) for i in range(2)]
        yps = [g.ps("k_yps%d" % i, [64, 512], F32, st) for i in range(4)]
        ei = 0
        gi_ = 0
        yi = 0
        for sq_ in range(2):
            cc = slice(sq_ * 256, sq_ * 256 + 256)
            lc = slice(512 + sq_ * 4096, 512 + (sq_ + 1) * 4096)
            S.dma("sync", dt[:, 0:256], g.DTr[:, cc], writes=["k_dt"])
            S.dma("sync", dt[:, 256:4352], g.DTr[:, lc], writes=["k_dt"])
            act(dt[:], dt[:], AF.Exp, ["k_dt", "k_hp"], ["k_dt"], bias=hp[:, 0:1], scale=1.0)
            V(lambda e: e.tensor_scalar(out=dt[:], in0=dt[:], scalar1=1.0, scalar2=None, op0=ALU.add), ["k_dt"], ["k_dt"])
            act(dt[:], dt[:], AF.Ln, ["k_dt"], ["k_dt"])
            V(lambda e: e.tensor_scalar(out=dta[:], in0=dt[:], scalar1=aneg[:, 0:1], scalar2=None, op0=ALU.mult), ["k_dt", "k_aneg"], ["k_dta"])
            V(lambda e: e.tensor_tensor_scan(out=psi[0:32, :], data0=g.cst_t[0:32, 2, 0:1].to_broadcast([32, 4352]), data1=dta[0:32, :], initial=0.0, op0=ALU.mult, op1=ALU.add),
              ["k_dta", "cst"], ["k_psi"])
            V(lambda e: e.tensor_tensor_scan(out=mkap(psi[32:64, 255:256], -1, 256), data0=g.cst_t[32:64, 2, 0:1].to_broadcast([32, 256]),
                                             data1=mkap(dta[32:64, 255:256], -1, 256), initial=0.0, op0=ALU.mult, op1=ALU.add),
              ["k_dta", "cst"], ["k_psi"])
            V(lambda e: e.tensor_tensor_scan(out=mkap(psi[32:64, 4351:4352], -1, 4096), data0=g.cst_t[32:64, 2, 0:1].to_broadcast([32, 4096]),
                                             data1=mkap(dta[32:64, 4351:4352], -1, 4096), initial=0.0, op0=ALU.mult, op1=ALU.add),
              ["k_dta", "cst"], ["k_psi"])
            V(lambda e: e.tensor_scalar(out=psi[32:64, 256:4352], in0=psi[32:64, 256:4352], scalar1=psi[32:64, 0:1], scalar2=None, op0=ALU.add),
              ["k_psi"], ["k_psi"])
            for which, (src, dst, dn, scl) in enumerate(((psi, psiT, "k_psiT", -1.0), (dt, dtT, "k_dtT", 1.0))):
                srcn = "k_psi" if which == 0 else "k_dt"
                for k4 in range(9):
                    nk = 4 if k4 < 8 else 2
                    for u in range(nk):
                        kt = k4 * 4 + u
                        S.op("tensor", lambda e, u=u, kt=kt, src=src: e.transpose(tps[:, u, :], src[:, kt * 128:(kt + 1) * 128], g.cst_t[0:64, 0, 0:64]),
                             [srcn, "cst"], ["k_tps"])
                    act(dst[:, k4 * 4:k4 * 4 + nk, :], tps[:, 0:nk, :], AF.Copy, ["k_tps"], [dn], scale=scl)
            for grp in range(8):
                S.dma("sync", Bg[:, 0:256], g.BCo[grp * 128:(grp + 1) * 128, cc], writes=["k_B"])
                S.dma("sync", Bg[:, 256:4352], g.BCo[grp * 128:(grp + 1) * 128, lc], writes=["k_B"])
                S.dma("sync", Cg[:, 0:256], g.BCo[D + grp * 128:D + (grp + 1) * 128, cc], writes=["k_C"])
                S.dma("sync", Cg[:, 256:4352], g.BCo[D + grp * 128:D + (grp + 1) * 128, lc], writes=["k_C"])
                S.dma("sync", Xg[:, 0:2, :], g.XTOK[cc, grp * 256:(grp + 1) * 256].rearrange("(kt p) v -> p kt v", p=128), writes=["k_X"])
                S.dma("sync", Xg[:, 2:34, :], g.XTOK[lc, grp * 256:(grp + 1) * 256].rearrange("(kt p) v -> p kt v", p=128), writes=["k_X"])
                for d in range(2):
                    V(lambda e, d=d, grp=grp: e.tensor_tensor(
                        out=xdt[:, :, d, :].rearrange("p k (h v) -> p k h v", h=4), in0=Xg[:].rearrange("p k (h v) -> p k h v", h=4),
                        in1=bc(dtT[:, :, d * 32 + 4 * grp:d * 32 + 4 * grp + 4], [128, 34, 4, 64], 3), op=ALU.mult), ["k_X", "k_dtT"], ["k_xdt"])
                for qt in range(9):
                    if qt == 0:
                        q0, N, ktq = 0, 256, 0
                    else:
                        q0, N, ktq = 256 + (qt - 1) * 512, 512, 2 + 4 * (qt - 1)
                    nq = N // 128
                    for d in range(2):
                        for h in range(4):
                            dh = d * 32 + 4 * grp + h
                            V(lambda e, dh=dh: e.tensor_copy(out=sel[:], in_=g.cst_t[0:64, 0, dh:dh + 1].to_broadcast([64, 128])), ["cst"], ["k_sel"])
                            S.op("tensor", lambda e, q0=q0, N=N: e.matmul(bps[:, :N], lhsT=sel[:], rhs=psi[:, q0:q0 + N], start=True, stop=True),
                                 ["k_sel", "k_psi"], ["k_bps"])
                            act(bcs[:, d * 4 + h, :N], bps[:, :N], AF.Copy, ["k_bps"], [("k_bcs", d * 4 + h)])
                    work = []
                    for kt in range(34):
                        inq = ktq <= kt < ktq + nq
                        o = kt - ktq
                        if qt == 0:
                            if kt < 2:
                                work.append((kt, True, True, o))
                        else:
                            if kt < 2:
                                work.append((kt, True, True, None))
                            elif inq:
                                work.append((kt, True, True, o))
                            elif kt < ktq:
                                work.append((kt, True, False, None))
                            else:
                                work.append((kt, False, True, None))
                    nf = sum(1 for w_ in work if w_[1])
                    nbk = sum(1 for w_ in work if w_[2])
                    tot = nf + nbk
                    cnt = [0, 0, 0, 0]
                    ginfo = {}

                    def emitG(wi):
                        nonlocal gi_
                        kt = work[wi][0]
                        gp_, gpn = gps[gi_ % 2], "k_gps%d" % (gi_ % 2)
                        G_, Gn = Gs[gi_ % 2], "k_G%d" % (gi_ % 2)
                        gi_ += 1
                        S.op("tensor", lambda e, gp_=gp_, kt=kt, q0=q0, N=N: e.matmul(gp_[:, :N], lhsT=Bg[:, kt * 128:(kt + 1) * 128], rhs=Cg[:, q0:q0 + N],
                                                                                   start=True, stop=True), ["k_B", "k_C"], [gpn])
                        ginfo[wi] = (G_, Gn, gp_, gpn)

                    def emitGcast(wi):
                        G_, Gn, gp_, gpn = ginfo[wi]
                        V(lambda e, G_=G_, gp_=gp_, N=N: e.tensor_copy(out=G_[:, :N], in_=gp_[:, :N]), [gpn], [Gn])

                    def emitRest(wi):
                        nonlocal ei
                        (kt, fw, bw, o) = work[wi]
                        G_, Gn, _gp, _gpn = ginfo.pop(wi)
                        for d in range(2):
                            if not (fw if d == 0 else bw):
                                continue
                            if o is not None:
                                for h in range(4):
                                    dh = d * 32 + 4 * grp + h
                                    V(lambda e, d=d, h=h, kt=kt, dh=dh, N=N: e.tensor_scalar(out=args_[h][:, :N], in0=bcs[:, d * 4 + h, :N], scalar1=psiT[:, kt, dh:dh + 1],
                                                                                         scalar2=0.0, op0=ALU.add, op1=ALU.min),
                                      [("k_bcs", d * 4 + h), "k_psiT"], ["k_arg%d" % h])
                            if o is not None:
                                S.op("gpsimd", lambda e, d=d, o=o, G_=G_, N=N: e.tensor_tensor(out=Gm[d][:, :N], in0=G_[:, :N], in1=mk[:, d, o, :N], op=ALU.mult),
                                     [Gn, "k_mk"], ["k_Gm%d" % d])
                                Gsrc, Gsn = Gm[d], "k_Gm%d" % d
                            else:
                                Gsrc, Gsn = G_, Gn
                            for h in range(4):
                                dh = d * 32 + 4 * grp + h
                                E_, En = Et[ei % 6], "k_E%d" % (ei % 6)
                                W_, Wn = Wt[ei % 6], "k_W%d" % (ei % 6)
                                arg, argn = args_[h], "k_arg%d" % h
                                ei += 1
                                if o is not None:
                                    act(E_[:, :N], arg[:, :N], AF.Exp, [argn], [En])
                                else:
                                    act(E_[:, :N], bcs[:, d * 4 + h, :N], AF.Exp, [("k_bcs", d * 4 + h), "k_psiT"], [En], bias=psiT[:, kt, dh:dh + 1], scale=1.0)
                                weng = "vector"
                                S.op(weng, lambda e, W_=W_, E_=E_, Gsrc=Gsrc, N=N: e.tensor_tensor(out=W_[:, :N], in0=Gsrc[:, :N], in1=E_[:, :N], op=ALU.mult),
                                     [Gsn, En], [Wn])
                                S.op("tensor", lambda e, h=h, kt=kt, d=d, W_=W_, N=N, st_=(cnt[h] == 0), sp_=(cnt[h] == tot - 1): e.matmul(
                                    yps[h][:, :N], lhsT=xdt[:, kt, d, h * 64:(h + 1) * 64], rhs=W_[:, :N], start=st_, stop=sp_), ["k_xdt", Wn], ["k_yps%d" % h])
                                cnt[h] += 1

                    emitG(0)
                    emitGcast(0)
                    for wi in range(len(work)):
                        if wi + 1 < len(work):
                            emitG(wi + 1)
                        emitRest(wi)
                        if wi + 1 < len(work):
                            emitGcast(wi + 1)
                    for h in range(4):
                        y_, yn = yo[yi % 2], "k_yo%d" % (yi % 2)
                        yi += 1
                        act(y_[:, :N], yps[h][:, :N], AF.Copy, ["k_yps%d" % h], [yn])
                        r0 = (4 * grp + h) * 64
                        c0 = (sq_ * 256) if qt == 0 else (512 + sq_ * 4096 + (qt - 1) * 512)
                        S.dma("sync", g.YT[r0:r0 + 64, c0:c0 + N], y_[:, :N], reads=[yn], writes=[("yt", sq_, grp, qt, h)])
        S.barrier()

    with ExitStack() as st:
        sb = lambda n, s_, d=F32: g.sb(n, s_, d, st)
        fv = sb("f_v", [128, 16, 2])
        S.dma("sync", fv[:], g.ssd_fv[:, :, :], writes=["f_v"])
        yb = [sb("f_y%d" % i, [128, 16, 512], BF16) for i in range(2)]
        xb = [sb("f_x%d" % i, [128, 16, 512], BF16) for i in range(2)]
        zb = [sb("f_z%d" % i, [128, 16, 512], BF16) for i in range(2)]
        gy = sb("f_gy", [128, 16, 512])
        gsq = sb("f_gsq", [128, 16, 512], BF16)
        rr = sb("f_rr", [128, 512])
        go = [sb("f_go%d" % i, [128, 16, 512], BF16) for i in range(2)]
        nps = [g.ps("f_nps%d" % i, [128, 512], F32, st) for i in range(2)]
        YTv = g.YT.rearrange("(c p) t -> p c t", p=128)
        XFv = g.XFo.rearrange("(c p) t -> p c t", p=128)
        SZv = g.SZ.rearrange("(c p) t -> p c t", p=128)
        G2v = g.GT2.rearrange("(c p) t -> p c t", p=128)
        tiles = list(range(1 if last else 0, NT))
        for jn, j in enumerate(tiles):
            q = jn % 2
            cols = slice(j * 512, (j + 1) * 512)
            S.dma("sync", yb[q][:], YTv[:, :, cols], writes=["f_y%d" % q])
            S.dma("sync", xb[q][:], XFv[:, :, cols], writes=["f_x%d" % q])
            S.dma("sync", zb[q][:], SZv[:, :, cols], writes=["f_z%d" % q])
            for c in range(16):
                V(lambda e, c=c, q=q: e.scalar_tensor_tensor(out=gy[:, c, :], in0=xb[q][:, c, :], scalar=fv[:, c, 0:1], in1=yb[q][:, c, :],
                                                             op0=ALU.mult, op1=ALU.add), ["f_x%d" % q, "f_y%d" % q, "f_v"], [("f_gy", c)])
                S.op("gpsimd", lambda e, c=c, q=q: e.tensor_tensor(out=gy[:, c, :], in0=gy[:, c, :], in1=zb[q][:, c, :], op=ALU.mult),
                     [("f_gy", c), "f_z%d" % q], [("f_gy", c)])
                act(gsq[:, c, :], gy[:, c, :], AF.Square, [("f_gy", c)], [("f_gsq", c)])
            for grp in range(8):
                np_, npn = nps[grp % 2], "f_nps%d" % (grp % 2)
                for u in range(2):
                    S.op("tensor", lambda e, grp=grp, u=u, np_=np_: e.matmul(np_[:], lhsT=g.ones_bf[:], rhs=gsq[:, 2 * grp + u, :], start=(u == 0), stop=(u == 1)),
                         [("f_gsq", 2 * grp + u), "ones_bf"], [npn])
                V(lambda e, np_=np_: e.tensor_scalar(out=rr[:], in0=np_[:], scalar1=1.0 / 256, scalar2=EPS, op0=ALU.mult, op1=ALU.add), [npn], ["f_rr"])
                act(rr[:], rr[:], AF.Sqrt, ["f_rr"], ["f_rr"])
                V(lambda e: e.reciprocal(out=rr[:], in_=rr[:]), ["f_rr"], ["f_rr"])
                for u in range(2):
                    c = 2 * grp + u
                    V(lambda e, c=c, q=q: e.scalar_tensor_tensor(out=go[q][:, c, :], in0=gy[:, c, :], scalar=fv[:, c, 1:2], in1=rr[:],
                                                                 op0=ALU.mult, op1=ALU.mult), [("f_gy", c), "f_rr", "f_v"], [("f_go%d" % q, c)])
            S.dma("sync", G2v[:, :, cols], go[q][:], reads=[("f_go%d" % q, c) for c in range(16)], writes=[("gt2", j)])
        S.barrier()
    out_proj_residual(g, l, g.ssd_ow, DI, last, "so")


def da_layer(g, l):
    import math
    nc, S = g.nc, g.S
    last = (l == 3)
    lam_init = 0.8 - 0.6 * math.exp(-0.3 * l)
    XTv = g.XT.rearrange("(c p) t -> p c t", p=128)
    GTv = g.GT.rearrange("(c p) t -> p c t", p=128)

    def V(fn, r, w, eng="vector"):
        S.op(eng, fn, r, w)

    def tt(out, a, b, op, r, w, eng="vector"):
        S.op(eng, lambda e: e.tensor_tensor(out=out, in0=a, in1=b, op=op), r, w)

    def act(out, in_, func, r, w, **kw):
        S.op("scalar", lambda e: e.activation(out=out, in_=in_, func=func, **kw), r, w)

    with ExitStack() as sl:
        dac = g.sb("a_dac", [128, 3, 128], F32, sl)
        Rm = g.sb("a_Rm", [128, 128], BF16, sl)
        bones = g.sb("a_bones", [128, 128], BF16, sl)
        gv = g.sb("a_gv", [128, 4], F32, sl)
        lamt = g.sb("a_lamt", [64, 4], F32, sl)
        lpr = g.sb("a_lpr", [64, 2], F32, sl)
        lbc = g.sb("a_lbc", [128, 2], F32, sl)
        nlam = g.sb("a_nlam", [128, 1], F32, sl)
        S.dma("sync", dac[:], g.dac[:, :, :], writes=["dac"])
        S.dma("sync", gv[:], g.dagv[:, :], writes=["gv"])
        S.dma("sync", lamt[:], g.dalam[:, :], writes=["lamt"])
        V(lambda e: e.tensor_copy(out=Rm[:], in_=dac[:, 0, :]), ["dac"], ["Rm"])
        V(lambda e: e.tensor_copy(out=bones[:], in_=dac[:, 1, :]), ["dac"], ["bones"])
        V(lambda e: e.tensor_scalar(out=gv[:, 0:1], in0=gv[:, 0:1], scalar1=0.125, scalar2=None, op0=ALU.mult), ["gv"], ["gv"])
        V(lambda e: e.tensor_scalar(out=gv[:, 2:3], in0=gv[:, 2:3], scalar1=1.0 - lam_init, scalar2=None, op0=ALU.mult), ["gv"], ["gv"])
        tt(lpr[:, 0:1], lamt[:, 0:1], lamt[:, 1:2], ALU.mult, ["lamt"], ["lpr"])
        tt(lpr[:, 1:2], lamt[:, 2:3], lamt[:, 3:4], ALU.mult, ["lamt"], ["lpr"])
        with ExitStack() as s0:
            lps = g.ps("a_lps", [128, 2], F32, s0)
            S.op("tensor", lambda e: e.matmul(lps[:], lhsT=g.cst_t[0:64, 2, :], rhs=lpr[:], start=True, stop=True), ["lpr", "cst"], ["lps"])
            act(lbc[:], lps[:], AF.Exp, ["lps"], ["lbc"])
            V(lambda e: e.scalar_tensor_tensor(out=nlam[:], in0=lbc[:, 1:2], scalar=-lam_init, in1=lbc[:, 0:1], op0=ALU.add, op1=ALU.subtract),
              ["lbc"], ["nlam"])
            S.barrier()

        with ExitStack() as st:
            sb = lambda n, s_, d=F32: g.sb(n, s_, d, st)
            w = sb("a_w", [128, NCH, 3 * D], BF16)
            S.op("gpsimd", lambda e: e.dma_start(out=w[:, :, 0:1536], in_=g.daqkv.rearrange("(kc p) n -> p kc n", p=128)[:, :, 0:1536]), [], ["a_w0"], dma=True)
            S.op("gpsimd", lambda e: e.dma_start(out=w[:, :, 1536:3072], in_=g.daqkv.rearrange("(kc p) n -> p kc n", p=128)[:, :, 1536:3072]), [], ["a_w1"], dma=True)
            xt = [sb("a_xt%d" % i, [128, NCH, 512]) for i in range(2)]
            sq = sb("a_sq", [128, NCH, 512], BF16)
            tmp = sb("a_tmp", [128, NCH, 512])
            hbf = sb("a_hbf", [128, NCH, 512], BF16)
            rstd = sb("a_rstd", [128, 512])
            rc = sb("a_rc", [128, 512]); rs = sb("a_rs", [128, 512])
            qsq = sb("a_qsq", [128, 512], BF16)
            qr = sb("a_qr", [128, 512]); qn = sb("a_qn", [128, 512]); qnb = sb("a_qnb", [128, 512], BF16)
            t1 = sb("a_t1", [128, 512]); t2 = sb("a_t2", [128, 512])
            qo = [sb("a_qo%d" % i, [128, 512], BF16) for i in range(2)]
            vt = [sb("a_vt%d" % i, [128, D], BF16) for i in range(2)]
            ssp = g.ps("a_ssp", [128, 512], F32, st)
            pq = [g.ps("a_pq%d" % i, [128, 512], F32, st) for i in range(2)]
            pss = g.ps("a_pss", [128, 512], F32, st)
            prq = g.ps("a_prq", [128, 512], F32, st)
            pv = [g.ps("a_pv%d" % i, [128, 512], F32, st) for i in range(2)]
            n = 0
            nv = 0
            for j in range(NT):
                x, xn = xt[j % 2], "a_xt%d" % (j % 2)
                S.dma("sync", x[:], XTv[:, :, j * 512:(j + 1) * 512], reads=[("XT", j)], writes=[xn])
                normmod(g, l, 1, x, xn, sq, ssp, rstd, tmp, None, hbf, stream_of_tile(j), tag="DA")
                if j >= 1:
                    pos0 = ((j - 1) % 8) * 512
                    S.dma("sync", rc[:], g.ropeC[:, pos0:pos0 + 512], writes=["rc"])
                    S.dma("sync", rs[:], g.ropeS[:, pos0:pos0 + 512], writes=["rs"])
                for which in range(2):
                    dst = g.QT if which == 0 else g.KT
                    for hd in range(8):
                        p_, pn = pq[n % 2], "a_pq%d" % (n % 2)
                        o_, on = qo[n % 2], "a_qo%d" % (n % 2)
                        n += 1
                        c0 = which * D + hd * 128
                        for kc in range(NCH):
                            S.op("tensor", lambda e, kc=kc, c0=c0, p_=p_: e.matmul(p_[:], lhsT=w[:, kc, c0:c0 + 128], rhs=hbf[:, kc, :], start=(kc == 0),
                                                                               stop=(kc == NCH - 1)), ["a_w0", "a_w1", "hbfDA"], [pn])
                        act(qsq[:], p_[:], AF.Square, [pn], ["qsq"])
                        S.op("tensor", lambda e: e.matmul(pss[:], lhsT=bones[:], rhs=qsq[:], start=True, stop=True), ["qsq", "bones"], ["pss"])
                        V(lambda e: e.tensor_scalar(out=qr[:], in0=pss[:], scalar1=1.0 / 64, scalar2=EPS, op0=ALU.mult, op1=ALU.add), ["pss"], ["qr"])
                        act(qr[:], qr[:], AF.Sqrt, ["qr"], ["qr"])
                        V(lambda e: e.reciprocal(out=qr[:], in_=qr[:]), ["qr"], ["qr"])
                        if j == 0:
                            V(lambda e, which=which, p_=p_, o_=o_: e.scalar_tensor_tensor(out=o_[:], in0=p_[:], scalar=gv[:, which:which + 1], in1=qr[:],
                                                                                      op0=ALU.mult, op1=ALU.mult), [pn, "qr", "gv"], [on])
                        else:
                            V(lambda e, which=which, p_=p_: e.scalar_tensor_tensor(out=qn[:], in0=p_[:], scalar=gv[:, which:which + 1], in1=qr[:],
                                                                               op0=ALU.mult, op1=ALU.mult), [pn, "qr", "gv"], ["qn"])
                            S.op("gpsimd", lambda e: e.tensor_copy(out=qnb[:], in_=qn[:]), ["qn"], ["qnb"])
                            S.op("tensor", lambda e: e.matmul(prq[:], lhsT=Rm[:], rhs=qnb[:], start=True, stop=True), ["qnb", "Rm"], ["prq"])
                            S.op("gpsimd", lambda e: e.tensor_tensor(out=t1[:], in0=qn[:], in1=rc[:], op=ALU.mult), ["qn", "rc"], ["t1"])
                            tt(t2[:], prq[:], rs[:], ALU.mult, ["prq", "rs"], ["t2"])
                            S.op("gpsimd", lambda e, o_=o_: e.tensor_tensor(out=o_[:], in0=t1[:], in1=t2[:], op=ALU.add), ["t1", "t2"], [on])
                        S.dma("sync", dst[hd * 128:(hd + 1) * 128, j * 512:(j + 1) * 512], o_[:], reads=[on], writes=[("qk", which, hd, j)])
                for s_ in range(4):
                    v_, vn = vt[nv % 2], "a_vt%d" % (nv % 2)
                    nv += 1
                    for half in range(2):
                        for kc in range(NCH):
                            S.op("tensor", lambda e, kc=kc, half=half, s_=s_: e.matmul(
                                pv[half][:], lhsT=hbf[:, kc, s_ * 128:(s_ + 1) * 128], rhs=w[:, kc, 2 * D + half * 512:2 * D + (half + 1) * 512],
                                start=(kc == 0), stop=(kc == NCH - 1)), ["a_w0", "a_w1", "hbfDA"], ["a_pv%d" % half])
                        act(v_[:, half * 512:(half + 1) * 512], pv[half][:], AF.Copy, ["a_pv%d" % half], [(vn, half)])
                    r0 = j * 512 + s_ * 128
                    S.dma("sync", g.VTOK[r0:r0 + 128, :], v_[:], reads=[(vn, 0), (vn, 1)], writes=[("vtok", j, s_)])
            S.barrier()

        with ExitStack() as st:
            sb = lambda n, s_, d=F32: g.sb(n, s_, d, st)
            Kh = [sb("b_K%d" % i, [128, 4352], BF16) for i in range(2)]
            Qh = [sb("b_Q%d" % i, [128, 4352], BF16) for i in range(2)]
            Vh = [sb("b_V%d" % i, [128, 34, 128], BF16) for i in range(2)]
            pt = [sb("b_pt%d" % i, [128, 512], BF16) for i in range(3)]
            rsum = sb("b_rsum", [128, 512])
            oc_ = [sb("b_oc%d" % i, [128, 512]) for i in range(2)]
            osq = sb("b_osq", [128, 512], BF16)
            orr = sb("b_orr", [128, 512])
            ao = [sb("b_ao%d" % i, [128, 512], BF16) for i in range(2)]
            sps = [g.ps("b_sps%d" % i, [128, 512], F32, st) for i in range(3)]
            ops_ = [g.ps("b_ops%d" % i, [128, 512], F32, st) for i in range(2)]
            sums = [g.ps("b_sum%d" % i, [128, 512], F32, st) for i in range(2)]
            oss = g.ps("b_oss", [128, 512], F32, st)
            hn = 0
            ei = 0
            an = 0
            for sq_ in range(2):
                cc = slice(sq_ * 256, sq_ * 256 + 256)
                lc = slice(512 + sq_ * 4096, 512 + (sq_ + 1) * 4096)
                for hd in range(8):
                    K_, Q_, V_ = Kh[hn % 2], Qh[hn % 2], Vh[hn % 2]
                    Kn, Qn, Vn = "b_K%d" % (hn % 2), "b_Q%d" % (hn % 2), "b_V%d" % (hn % 2)
                    hn += 1
                    hr = slice(hd * 128, (hd + 1) * 128)
                    S.dma("sync", K_[:, 0:256], g.KT[hr, cc], writes=[Kn])
                    S.dma("sync", K_[:, 256:4352], g.KT[hr, lc], writes=[Kn])
                    S.dma("sync", Q_[:, 0:256], g.QT[hr, cc], writes=[Qn])
                    S.dma("sync", Q_[:, 256:4352], g.QT[hr, lc], writes=[Qn])
                    S.dma("sync", V_[:, 0:2, :], g.VTOK[cc, hr].rearrange("(kt p) v -> p kt v", p=128), writes=[Vn])
                    S.dma("sync", V_[:, 2:34, :], g.VTOK[lc, hr].rearrange("(kt p) v -> p kt v", p=128), writes=[Vn])
                    for qt in range(9):
                        if qt == 0:
                            q0, N, nkt = 0, 256, 2
                        else:
                            q0, N, nkt = 256 + (qt - 1) * 512, 512, 34
                        for comp in range(2):
                            cr = slice(comp * 64, (comp + 1) * 64)
                            o_ps, o_n = ops_[comp], "b_ops%d" % comp
                            s_ps, s_n = sums[comp], "b_sum%d" % comp
                            pend = {}
                            for it in range(nkt + 2):
                                if it < nkt:
                                    kt = it
                                    sp_, spn = sps[ei % 3], "b_sps%d" % (ei % 3)
                                    p_, ptn = pt[ei % 3], "b_pt%d" % (ei % 3)
                                    ei += 1
                                    S.op("tensor", lambda e, sp_=sp_, K_=K_, Q_=Q_, cr=cr, kt=kt, q0=q0, N=N: e.matmul(
                                        sp_[:, :N], lhsT=K_[cr, kt * 128:(kt + 1) * 128], rhs=Q_[cr, q0:q0 + N], start=True, stop=True), [Kn, Qn], [spn])
                                    act(p_[:, :N], sp_[:, :N], AF.Exp, [spn], [ptn])
                                    pend[kt] = (p_, ptn)
                                if it >= 2:
                                    kt = it - 2
                                    p_, ptn = pend.pop(kt)
                                    S.op("tensor", lambda e, o_ps=o_ps, V_=V_, kt=kt, p_=p_, N=N, nkt=nkt: e.matmul(
                                        o_ps[:, :N], lhsT=V_[:, kt, :], rhs=p_[:, :N], start=(kt == 0), stop=(kt == nkt - 1)), [Vn, ptn], [o_n])
                                    S.op("tensor", lambda e, s_ps=s_ps, p_=p_, N=N, kt=kt, nkt=nkt: e.matmul(
                                        s_ps[:, :N], lhsT=g.ones_bf[:], rhs=p_[:, :N], start=(kt == 0), stop=(kt == nkt - 1)), ["ones_bf", ptn], [s_n])
                            V(lambda e, s_ps=s_ps, N=N: e.reciprocal(out=rsum[:, :N], in_=s_ps[:, :N]), [s_n], ["rsum"])
                            tt(oc_[comp][:, :N], o_ps[:, :N], rsum[:, :N], ALU.mult, [o_n, "rsum"], ["b_oc%d" % comp])
                        V(lambda e, N=N: e.scalar_tensor_tensor(out=oc_[0][:, :N], in0=oc_[1][:, :N], scalar=nlam[:, 0:1], in1=oc_[0][:, :N],
                                                                op0=ALU.mult, op1=ALU.add), ["b_oc0", "b_oc1", "nlam"], ["b_oc0"])
                        act(osq[:, :N], oc_[0][:, :N], AF.Square, ["b_oc0"], ["osq"])
                        S.op("tensor", lambda e, N=N: e.matmul(oss[:, :N], lhsT=g.ones_bf[:], rhs=osq[:, :N], start=True, stop=True), ["osq", "ones_bf"], ["oss"])
                        V(lambda e, N=N: e.tensor_scalar(out=orr[:, :N], in0=oss[:, :N], scalar1=1.0 / 128, scalar2=EPS, op0=ALU.mult, op1=ALU.add),
                          ["oss"], ["orr"])
                        act(orr[:, :N], orr[:, :N], AF.Sqrt, ["orr"], ["orr"])
                        V(lambda e, N=N: e.reciprocal(out=orr[:, :N], in_=orr[:, :N]), ["orr"], ["orr"])
                        a_, a_n = ao[an % 2], "b_ao%d" % (an % 2)
                        an += 1
                        V(lambda e, N=N, a_=a_: e.scalar_tensor_tensor(out=a_[:, :N], in0=oc_[0][:, :N], scalar=gv[:, 2:3], in1=orr[:, :N],
                                                                      op0=ALU.mult, op1=ALU.mult), ["b_oc0", "orr", "gv"], [a_n])
                        if qt == 0:
                            S.dma("sync", g.GT[hr, cc], a_[:, :256], reads=[a_n], writes=[("AT", sq_, hd, qt)])
                        else:
                            c0 = 512 + sq_ * 4096 + (qt - 1) * 512
                            S.dma("sync", g.GT[hr, c0:c0 + 512], a_[:, :512], reads=[a_n], writes=[("AT", sq_, hd, qt)])
            S.barrier()

        out_proj_residual(g, l, g.daow, D, last, "o")


def out_proj_residual(g, l, wdram, kdim, last, tag):
    nc, S = g.nc, g.S
    XTv = g.XT.rearrange("(c p) t -> p c t", p=128)
    nk = kdim // 128
    src = g.GT if kdim == D else g.GT2
    SRCv = src.rearrange("(c p) t -> p c t", p=128)
    with ExitStack() as st:
        sb = lambda n, s_, d=F32: g.sb(n, s_, d, st)
        w = sb(tag + "_w", [128, nk, D], BF16)
        S.op("gpsimd", lambda e: e.dma_start(out=w[:], in_=wdram.rearrange("(kc p) n -> p kc n", p=128)), [], [tag + "_w"], dma=True)
        xt = [sb(tag + "_xt%d" % i, [128, NCH, 512]) for i in range(2)]
        ai = [sb(tag + "_ai%d" % i, [128, nk, 512], BF16) for i in range(2)]
        po = [g.ps(tag + "_po%d" % i, [128, 512], F32, st) for i in range(2)]
        tiles = list(range(1 if last else 0, NT))
        for jn, j in enumerate(tiles):
            x, xn = xt[jn % 2], tag + "_xt%d" % (jn % 2)
            a, an = ai[jn % 2], tag + "_ai%d" % (jn % 2)
            stream = stream_of_tile(j)
            S.dma("sync", x[:], XTv[:, :, j * 512:(j + 1) * 512], reads=[("XT", j)], writes=[xn])
            S.dma("sync", a[:], SRCv[:, :, j * 512:(j + 1) * 512], reads=[], writes=[an])
            for oc in range(NCH):
                q = oc % 2
                for kc in range(nk):
                    S.op("tensor", lambda e, oc=oc, kc=kc, q=q, a=a: e.matmul(po[q][:], lhsT=w[:, kc, oc * 128:(oc + 1) * 128], rhs=a[:, kc, :],
                                                                             start=(kc == 0), stop=(kc == nk - 1)), [tag + "_w", an], [tag + "_po%d" % q])
                S.op("vector", lambda e, oc=oc, q=q, x=x, stream=stream: e.scalar_tensor_tensor(
                    out=x[:, oc, :], in0=po[q][:], scalar=g.mods[l][:, 2, oc, stream:stream + 1], in1=x[:, oc, :], op0=ALU.mult, op1=ALU.add),
                    [tag + "_po%d" % q, xn, ("mods", l)], [xn])
            S.dma("sync", XTv[:, :, j * 512:(j + 1) * 512], x[:], reads=[xn], writes=[("XT", j)])
        S.barrier()


def host_consts():
    cst = np.zeros((128, 6, 128), np.float32)
    cst[:, 0, :] = np.eye(128)
    cst[:, 1, :] = np.triu(np.ones((128, 128)), 1)
    cst[:, 2, :] = 1.0
    cst[:, 3, :] = np.arange(128)[None, :]
    cst[:, 4, :] = np.arange(128)[:, None]
    cblk = np.zeros((128, 128), np.float32)
    cblk[:, :] = (np.arange(128) * BLK)[None, :]
    cblk[:, 100:118] = (np.arange(18) * BLK)[None, :]
    return cst, cblk


def fm(v):
    v = np.asarray(v, np.float32)
    lead = v.shape[:-1]
    n = v.shape[-1] // 128
    return np.ascontiguousarray(np.swapaxes(v.reshape(lead + (n, 128)), -1, -2))


def make_s5_inputs(inp):
    out = {}
    are, aim, ldt = inp["s5_a_re"], inp["s5_a_im"], inp["s5_log_dt"]
    ldtb = np.broadcast_to(ldt[..., None], are.shape)
    prm = np.stack([are, aim, ldtb], -1).astype(np.float32)
    pB = prm.reshape(2, 2, 32, 2, 64, 3).transpose(0, 3, 4, 1, 2, 5).reshape(2, 128, 2, 32, 3)
    out["s5B"] = np.ascontiguousarray(pB)
    pA = prm.reshape(2, 2, 8, 8, 64, 3)
    pA = np.broadcast_to(pA[:, :, :, :, None], (2, 2, 8, 8, 16, 64, 3))
    out["s5A"] = np.ascontiguousarray(pA.transpose(0, 3, 4, 1, 2, 5, 6).reshape(2, 128, 2, 8, 64, 3))
    b = np.stack([inp["s5_b_re"], inp["s5_b_im"]], 2).astype(np.float32)
    c = np.stack([inp["s5_c_re"], inp["s5_c_im"]], 2).astype(np.float32)
    BzB = np.zeros((2, 2, 64, 2, 2, 32, 128), np.float32)
    CzB = np.zeros_like(BzB)
    for q in range(32):
        for gp in range(2):
            c0 = 32 * (q % 4) + 16 * gp
            BzB[:, gp, :, :, :, q, c0:c0 + 16] = b[:, :, :, 2 * q + gp].transpose(0, 3, 1, 2, 4)
            CzB[:, gp, :, :, :, q, c0:c0 + 16] = c[:, :, :, 2 * q + gp].transpose(0, 4, 1, 2, 3)
    out["s5BzB"] = BzB.reshape(2, 128, 2, 2, 32, 128)
    out["s5CzB"] = CzB.reshape(2, 128, 2, 2, 32, 128)
    BzA = np.zeros((2, 8, 16, 2, 2, 8, 8, 64), np.float32)
    for ct in range(8):
        for g8 in range(8):
            BzA[:, g8, :, :, :, ct, g8, :] = b[:, :, :, 8 * ct + g8].transpose(0, 4, 1, 2, 3)
    out["s5BzA"] = BzA.reshape(2, 128, 2, 2, 8, 8, 64)
    out["s5d"] = fm(inp["s5_d"])
    out["s5gw"] = np.ascontiguousarray(inp["s5_glu_w"], dtype=np.float32)
    out["s5gb"] = fm(inp["s5_glu_b"])
    s5c = np.zeros((128, 16 + NBK), np.float32)
    s5c[:, 0:9] = np.arange(9)[None, :]
    s5c[:, 16:] = np.arange(NBK)[None, :]
    out["s5c"] = s5c
    return out


def make_ssd_inputs(inp):
    out = {}
    out["ssd_inw"] = np.ascontiguousarray(inp["ssd_in_w"][0], dtype=np.float32)
    cw = np.zeros((128, 32, 6), np.float32)
    w = inp["ssd_conv_w"][0]
    b = inp["ssd_conv_b"][0]
    cw[:, :, 0:5] = w.T.reshape(32, 128, 5).transpose(1, 0, 2)
    cw[:, :, 5] = b.reshape(32, 128).T
    out["ssd_cw"] = cw
    hp = np.zeros((64, 4), np.float32)
    hp[:, 0] = inp["ssd_dt_bias"][0].reshape(64)
    hp[:, 1] = inp["ssd_a_log"][0].reshape(64)
    out["ssd_hp"] = hp
    m = np.zeros((128, 2, 4, 512), np.float32)
    s_ = np.arange(128)[:, None]
    t = np.arange(512)[None, :]
    for o in range(4):
        m[:, 0, o, :] = (t >= 128 * o + s_)
        m[:, 1, o, :] = (t <= 128 * o + s_)
    out["ssd_mask"] = m
    fv = np.zeros((128, 16, 2), np.float32)
    dsk = np.repeat(inp["ssd_d"][0], 64)
    fv[:, :, 0] = dsk.reshape(16, 128).T
    fv[:, :, 1] = inp["ssd_norm_g"][0].reshape(16, 128).T
    out["ssd_fv"] = fv
    out["ssd_ow"] = np.ascontiguousarray(inp["ssd_out_w"][0], dtype=np.float32)
    return out


def make_da_inputs(inp):
    out = {}
    out["daqkv"] = np.ascontiguousarray(inp["da_qkv_w"][0], dtype=np.float32)
    out["daow"] = np.ascontiguousarray(inp["da_out_w"][0], dtype=np.float32)
    gv = np.zeros((128, 4), np.float32)
    gv[:, 0] = np.tile(inp["da_q_g"][0], 2)
    gv[:, 1] = np.tile(inp["da_k_g"][0], 2)
    gv[:, 2] = inp["da_sub_g"][0]
    out["dagv"] = gv
    out["dalam"] = np.ascontiguousarray(inp["da_lam"][0].T, dtype=np.float32)
    dac = np.zeros((128, 3, 128), np.float32)
    for m in range(128):
        if (m % 32) < 16:
            dac[m + 16, 0, m] = -1.0
        else:
            dac[m - 16, 0, m] = 1.0
    dac[:, 1, :] = (np.arange(128)[:, None] // 64 == np.arange(128)[None, :] // 64)
    out["dac"] = dac
    t = np.arange(4096)
    row, col = (t // 64).astype(np.float64), (t % 64).astype(np.float64)
    inv = (np.float32(10000.0) ** (-np.arange(16, dtype=np.float32) / np.float32(16))).astype(np.float64)
    ang = np.zeros((128, 4096))
    for p in range(128):
        dd = p % 64
        f = dd % 16
        ang[p] = (row if dd < 32 else col) * inv[f]
    out["ropeC"] = np.cos(ang).astype(np.float32)
    out["ropeS"] = np.sin(ang).astype(np.float32)
    return out


def make_in_maps(inp, layers, core_ids=range(8), stub_moe=False):
    L = list(layers)
    cst, cblk = host_consts()
    shared = {
        "mod_w": np.ascontiguousarray(inp["mod_w"][L]),
        "mod_bT": fm(inp["mod_b"][L]),
        "n1T": fm(inp["norm1_g"][L]),
        "n2T": fm(inp["norm2_g"][L]),
        "rwT": np.ascontiguousarray(inp["moe_router_w"][L].reshape(len(L), NCH, 128, NEXP).transpose(0, 2, 1, 3)),
        "rb_bc": np.ascontiguousarray(np.broadcast_to(inp["moe_router_b"][L][:, None, :], (len(L), 128, NEXP))),
        "gu_w": np.ascontiguousarray(inp["moe_gu_w"][L].reshape(len(L), NEXP * D, 2 * D)),
        "dn_w": np.ascontiguousarray(inp["moe_dn_w"][L].reshape(len(L), NEXP * D, D)),
        "gu_bT": np.ascontiguousarray(inp["moe_gu_b"][L].reshape(len(L), NEXP, 16, 128).transpose(0, 1, 3, 2).reshape(len(L), NEXP * 128, 16)),
        "dn_b": np.ascontiguousarray(inp["moe_dn_b"][L]),
        "cst": cst, "cblk": cblk,
    }
    if any(l % 3 == 0 for l in L):
        shared.update(make_s5_inputs(inp))
    if any(l % 3 == 2 for l in L):
        shared.update(make_da_inputs(inp))
    if any(l % 3 == 1 for l in L):
        shared.update(make_ssd_inputs(inp))
    if stub_moe:
        shared["gu_w"] = shared["gu_w"][:, :8].copy()
        shared["dn_w"] = shared["dn_w"][:, :8].copy()
    maps = []
    for c in core_ids:
        b0, b1 = 2 * c, 2 * c + 1
        x = inp["x"]
        ctx = inp["ctx"]
        xT = np.concatenate([ctx[b0].T, ctx[b1].T, x[b0].T, x[b1].T], axis=1)
        cvec = np.stack([inp["c_ctx"], inp["c"][b0], inp["c"][b1]], axis=-1)
        cT = np.ascontiguousarray(cvec.reshape(NCH, 128, 3).transpose(1, 0, 2))
        m = dict(shared)
        m["xT0"] = np.ascontiguousarray(xT, dtype=np.float32)
        m["cT"] = cT.astype(np.float32)
        maps.append(m)
    return maps


def kernel(**inputs):
    inp = {k: np.asarray(v) for k, v in inputs.items()}
    layers = [0, 1, 2, 3]
    nc = build_program(layers, 4)
    maps = make_in_maps(inp, layers)
    res = run_bass_kernel_spmd(nc, maps, core_ids=list(range(8)))
    out = np.zeros((16, 4096, D), np.float32)
    for c in range(8):
        o = res.results[c]["outT"]
        out[2 * c] = o[:, :4096].T
        out[2 * c + 1] = o[:, 4096:].T
    return out
```
